# Optimizing a Trainium2 kernel written in Bass

```python
import math
import jax, jax.numpy as jnp
from jax import lax
import numpy as np

D_MODEL = 1024
BATCH = 4
SEQ = 8192
DEPTH = 1

NSA_HEADS = 8
NSA_KV_GROUPS = 2
NSA_HPG = NSA_HEADS // NSA_KV_GROUPS
NSA_DK = 64
NSA_DV = 64
CMP_LEN = 32
CMP_STRIDE = 16
CMP_HIDDEN = 256
SLC_BLOCK = 64
SLC_TOPK = 16
WINDOW = 512
NSA_QCHUNK = 64
FORCED_SCORE = 1e4
ML_HEADS = 4
ML_DK = 128
ML_DV = 128
ML_CHUNK = 64
CONV_WIDTH = 4
D_FF = 4 * D_MODEL
REL_BUCKETS = 32
REL_MAX_DIST = 128
RMS_EPS = 1e-6

NSA_QW = NSA_HEADS * NSA_DK
NSA_KW = NSA_KV_GROUPS * NSA_DK
NSA_VW = NSA_KV_GROUPS * NSA_DV
NSA_OW = NSA_HEADS * NSA_DV
ML_QW = ML_HEADS * ML_DK
ML_VW = ML_HEADS * ML_DV
PROJ_WIDTHS = (NSA_QW, NSA_KW, NSA_VW, NSA_KW, NSA_VW, NSA_KW, NSA_VW, 3 * NSA_HEADS,
               ML_QW, ML_QW, ML_VW, ML_HEADS, ML_HEADS, ML_VW, 2 * D_MODEL)
D_PROJ = sum(PROJ_WIDTHS)

kernel_name = 'hybrid_nsa_mlstm_block'


def rms_norm(x, g):
    xf = x.astype(jnp.float32)
    y = xf * lax.rsqrt(jnp.mean(xf * xf, axis=-1, keepdims=True) + RMS_EPS)
    return (y * g.astype(jnp.float32)).astype(x.dtype)


def t5_bucket(dist):
    n = jnp.maximum(dist, 0)
    max_exact = REL_BUCKETS // 2
    nf = jnp.maximum(n, 1).astype(jnp.float32)
    large = max_exact + (jnp.log(nf / max_exact) / math.log(REL_MAX_DIST / max_exact)
                         * (REL_BUCKETS - max_exact)).astype(jnp.int32)
    return jnp.where(n < max_exact, n, jnp.minimum(large, REL_BUCKETS - 1))


def masked_softmax(s, mask):
    s = jnp.where(mask, s.astype(jnp.float32), -jnp.inf)
    m = jnp.max(s, axis=-1, keepdims=True)
    m = jnp.where(jnp.isfinite(m), m, 0.0)
    p = jnp.where(mask, jnp.exp(s - m), 0.0)
    return p / jnp.maximum(jnp.sum(p, axis=-1, keepdims=True), 1e-30)


def compress_blocks(kv, pos, w1, b1, w2, b2):
    bsz, s, g, d = kv.shape
    segs = kv.reshape(bsz, s // CMP_STRIDE, CMP_STRIDE, g, d)
    blocks = jnp.concatenate([segs[:, :-1], segs[:, 1:]], axis=2) + pos[:, None, :]
    flat = blocks.transpose(0, 1, 3, 2, 4).reshape(bsz, s // CMP_STRIDE - 1, g, CMP_LEN * d)
    return jax.nn.gelu(flat @ w1 + b1) @ w2 + b2


def nsa_mixer(q, kc, vc, ks, vs, kw, vw, gate_logits, q_gain, k_gain, cmp_k, cmp_v, rel_table):
    bsz, s = q.shape[:2]
    G, HPG = NSA_KV_GROUPS, NSA_HPG
    nc = s // CMP_STRIDE - 1
    ns = s // SLC_BLOCK
    topk = min(SLC_TOPK, ns)
    qg = (rms_norm(q, q_gain) * (NSA_DK ** -0.5)).reshape(bsz, s, G, HPG, NSA_DK)
    kcmp = rms_norm(compress_blocks(kc, *cmp_k), k_gain[0])
    vcmp = compress_blocks(vc, *cmp_v)
    ks_blk = rms_norm(ks, k_gain[1]).reshape(bsz, ns, SLC_BLOCK, G, NSA_DK)
    vs_blk = vs.reshape(bsz, ns, SLC_BLOCK, G, NSA_DV)
    pad = ((0, 0), (WINDOW, 0), (0, 0), (0, 0))
    kw_pad = jnp.pad(rms_norm(kw, k_gain[2]), pad)
    vw_pad = jnp.pad(vw, pad)
    gates = jax.nn.sigmoid(gate_logits.astype(jnp.float32)).astype(q.dtype).reshape(bsz, s, G, HPG, 3)
    table = rel_table.reshape(REL_BUCKETS, G, HPG)
    cmp_end = jnp.arange(nc) * CMP_STRIDE + (CMP_LEN - 1)
    ci = jnp.arange(nc)[:, None] * CMP_STRIDE
    sj = jnp.arange(ns)[None, :] * SLC_BLOCK
    cmp_to_slc = ((ci < sj + SLC_BLOCK) & (ci + CMP_LEN > sj)).astype(jnp.float32)
    blk_ids = jnp.arange(ns)
    b_i = jnp.arange(bsz)[:, None, None, None]
    g_i = jnp.arange(G)[None, None, :, None]
    g_i5 = jnp.arange(G)[None, None, :, None, None]

    def chunk(c):
        t0 = c * NSA_QCHUNK
        t = t0 + jnp.arange(NSA_QCHUNK)
        qc = lax.dynamic_slice_in_dim(qg, t0, NSA_QCHUNK, axis=1)
        dist_c = t[:, None] - cmp_end[None, :]
        bias_c = table[t5_bucket(dist_c)].transpose(0, 2, 3, 1)
        s_c = jnp.einsum('bqghd,bngd->bqghn', qc, kcmp).astype(jnp.float32) + bias_c
        p_c = masked_softmax(s_c, (dist_c >= 0)[:, None, None, :])
        o_c = jnp.einsum('bqghn,bngd->bqghd', p_c.astype(vcmp.dtype), vcmp)
        imp = jnp.sum(p_c, axis=3) @ cmp_to_slc
        cur = t // SLC_BLOCK
        valid = (blk_ids[None, :] * SLC_BLOCK) <= t[:, None]
        forced = (blk_ids[None, :] == 0) | (blk_ids[None, :] == cur[:, None]) | (blk_ids[None, :] == cur[:, None] - 1)
        score = jnp.where(forced[None, :, None, :], FORCED_SCORE,
                          jnp.where(valid[None, :, None, :], imp, -1.0))
        _, idx = lax.top_k(score, topk)
        k_sel = ks_blk[b_i, idx, :, g_i, :]
        v_sel = vs_blk[b_i, idx, :, g_i, :]
        pos_s = idx[..., None] * SLC_BLOCK + jnp.arange(SLC_BLOCK)
        dist_s = t[None, :, None, None, None] - pos_s
        bias_s = jnp.moveaxis(table[t5_bucket(dist_s), g_i5], -1, 3)
        s_s = (jnp.einsum('bqghd,bqgnkd->bqghnk', qc, k_sel).astype(jnp.float32) + bias_s)
        s_s = s_s.reshape(bsz, NSA_QCHUNK, G, HPG, topk * SLC_BLOCK)
        p_s = masked_softmax(s_s, (dist_s >= 0).reshape(bsz, NSA_QCHUNK, G, 1, topk * SLC_BLOCK))
        o_s = jnp.einsum('bqghm,bqgmd->bqghd', p_s.astype(v_sel.dtype),
                         v_sel.reshape(bsz, NSA_QCHUNK, G, topk * SLC_BLOCK, NSA_DV))
        kwc = lax.dynamic_slice_in_dim(kw_pad, t0, NSA_QCHUNK + WINDOW, axis=1)
        vwc = lax.dynamic_slice_in_dim(vw_pad, t0, NSA_QCHUNK + WINDOW, axis=1)
        pos_w = t0 - WINDOW + jnp.arange(NSA_QCHUNK + WINDOW)
        dist_w = t[:, None] - pos_w[None, :]
        mask_w = (dist_w >= 0) & (dist_w < WINDOW) & (pos_w >= 0)[None, :]
        bias_w = table[t5_bucket(dist_w)].transpose(0, 2, 3, 1)
        s_w = jnp.einsum('bqghd,bkgd->bqghk', qc, kwc).astype(jnp.float32) + bias_w
        p_w = masked_softmax(s_w, mask_w[:, None, None, :])
        o_w = jnp.einsum('bqghk,bkgd->bqghd', p_w.astype(vwc.dtype), vwc)
        gc = lax.dynamic_slice_in_dim(gates, t0, NSA_QCHUNK, axis=1)
        o = gc[..., 0:1] * o_c + gc[..., 1:2] * o_s + gc[..., 2:3] * o_w
        return o.reshape(bsz, NSA_QCHUNK, NSA_OW)

    out = lax.map(chunk, jnp.arange(s // NSA_QCHUNK))
    return out.transpose(1, 0, 2, 3).reshape(bsz, s, NSA_OW)


def causal_dwconv(x, w, b):
    s = x.shape[1]
    xp = jnp.pad(x, ((0, 0), (CONV_WIDTH - 1, 0), (0, 0)))
    y = b
    for j in range(CONV_WIDTH):
        y = y + xp[:, j:j + s] * w[j]
    return y


def mlstm_mixer(q, k, v, i_raw, f_raw, o_raw, conv_w, conv_b, i_bias, f_bias):
    bsz, s = q.shape[:2]
    nch = s // ML_CHUNK
    qk = jax.nn.silu(causal_dwconv(jnp.concatenate([q, k], axis=-1), conv_w, conv_b))
    q, k = jnp.split(qk, 2, axis=-1)

    def to_chunks(a, d):
        return a.astype(jnp.float32).reshape(bsz, nch, ML_CHUNK, ML_HEADS, d).transpose(1, 0, 3, 2, 4)

    def gate_chunks(a):
        return a.astype(jnp.float32).reshape(bsz, nch, ML_CHUNK, ML_HEADS).transpose(1, 0, 3, 2)

    qc = to_chunks(q * (ML_DK ** -0.5), ML_DK)
    kc = to_chunks(k, ML_DK)
    vc = to_chunks(v, ML_DV)
    li = gate_chunks(i_raw + i_bias)
    lf = jax.nn.log_sigmoid(gate_chunks(f_raw + f_bias))
    causal = jnp.tril(jnp.ones((ML_CHUNK, ML_CHUNK), dtype=bool))

    def step(carry, inp):
        c_mat, n_vec, m_prev = carry
        qb, kb, vb, lib, lfb = inp
        b = jnp.cumsum(lfb, axis=-1)
        g = b[..., -1]
        log_d = jnp.where(causal, b[..., :, None] - b[..., None, :] + lib[..., None, :], -jnp.inf)
        inter = b + m_prev[..., None]
        m_row = jnp.maximum(inter, jnp.max(log_d, axis=-1))
        w = jnp.einsum('bhid,bhjd->bhij', qb, kb) * jnp.exp(log_d - m_row[..., None])
        inter_scale = jnp.exp(inter - m_row)
        num = inter_scale[..., None] * jnp.einsum('bhid,bhde->bhie', qb, c_mat) + jnp.einsum('bhij,bhje->bhie', w, vb)
        den = inter_scale * jnp.einsum('bhid,bhd->bhi', qb, n_vec) + jnp.sum(w, axis=-1)
        h = num / jnp.maximum(jnp.abs(den), jnp.exp(-m_row))[..., None]
        log_src = g[..., None] - b + lib
        m_new = jnp.maximum(g + m_prev, jnp.max(log_src, axis=-1))
        decay = jnp.exp(g + m_prev - m_new)
        src = jnp.exp(log_src - m_new[..., None])
        c_new = decay[..., None, None] * c_mat + jnp.einsum('bhj,bhjd,bhje->bhde', src, kb, vb)
        n_new = decay[..., None] * n_vec + jnp.einsum('bhj,bhjd->bhd', src, kb)
        return (c_new, n_new, m_new), h

    init = (jnp.zeros((bsz, ML_HEADS, ML_DK, ML_DV), jnp.float32),
            jnp.zeros((bsz, ML_HEADS, ML_DK), jnp.float32),
            jnp.zeros((bsz, ML_HEADS), jnp.float32))
    _, h = lax.scan(step, init, (qc, kc, vc, li, lf))
    h = h.transpose(1, 0, 3, 2, 4).reshape(bsz, s, ML_VW)
    return (jax.nn.sigmoid(o_raw.astype(jnp.float32)) * h).astype(o_raw.dtype)


def setup_inputs(seed: int = 0) -> dict:
    key = jax.random.key(seed)
    keys = iter(jax.random.split(key, 40))

    def nrm(shape, scale):
        return jax.random.normal(next(keys), shape, jnp.float32) * scale

    L = DEPTH
    inp = {}
    inp['x'] = nrm((BATCH, SEQ, D_MODEL), 1.0)
    inp['norm1_g'] = 1.0 + nrm((L, D_MODEL), 0.01)
    inp['w_in'] = nrm((L, D_MODEL, D_PROJ), D_MODEL ** -0.5)
    inp['nsa_q_gain'] = 1.0 + nrm((L, NSA_DK), 0.01)
    inp['nsa_k_gain'] = 1.0 + nrm((L, 3, NSA_DK), 0.01)
    for name, d in (('k', NSA_DK), ('v', NSA_DV)):
        inp['cmp_' + name + '_pos'] = nrm((L, CMP_LEN, d), 0.1)
        inp['cmp_' + name + '_w1'] = nrm((L, CMP_LEN * d, CMP_HIDDEN), (CMP_LEN * d) ** -0.5)
        inp['cmp_' + name + '_b1'] = nrm((L, CMP_HIDDEN), 0.01)
        inp['cmp_' + name + '_w2'] = nrm((L, CMP_HIDDEN, d), CMP_HIDDEN ** -0.5)
        inp['cmp_' + name + '_b2'] = nrm((L, d), 0.01)
    inp['rel_table'] = nrm((REL_BUCKETS, NSA_HEADS), 0.5)
    inp['ml_conv_w'] = nrm((L, CONV_WIDTH, 2 * ML_QW), CONV_WIDTH ** -0.5)
    inp['ml_conv_b'] = nrm((L, 2 * ML_QW), 0.01)
    inp['ml_i_bias'] = nrm((L, ML_HEADS), 0.1)
    inp['ml_f_bias'] = jnp.linspace(3.0, 6.0, ML_HEADS, dtype=jnp.float32)[None, :] + nrm((L, ML_HEADS), 0.1)
    inp['w_branch_a'] = nrm((L, NSA_OW, D_MODEL), NSA_OW ** -0.5)
    inp['w_branch_b'] = nrm((L, ML_VW, D_MODEL), ML_VW ** -0.5)
    inp['w_out'] = nrm((L, D_MODEL, D_MODEL), D_MODEL ** -0.5)
    inp['norm2_g'] = 1.0 + nrm((L, D_MODEL), 0.01)
    inp['w_ff1'] = nrm((L, D_MODEL, D_FF), D_MODEL ** -0.5)
    inp['w_ff2'] = nrm((L, D_FF, D_MODEL), D_FF ** -0.5)
    return inp


def reference(x, norm1_g, w_in, nsa_q_gain, nsa_k_gain,
              cmp_k_pos, cmp_k_w1, cmp_k_b1, cmp_k_w2, cmp_k_b2,
              cmp_v_pos, cmp_v_w1, cmp_v_b1, cmp_v_w2, cmp_v_b2,
              rel_table, ml_conv_w, ml_conv_b, ml_i_bias, ml_f_bias,
              w_branch_a, w_branch_b, w_out, norm2_g, w_ff1, w_ff2):
    bsz, s = x.shape[:2]
    offsets = np.cumsum(PROJ_WIDTHS)[:-1].tolist()
    for l in range(DEPTH):
        h = rms_norm(x, norm1_g[l])
        proj = h @ w_in[l]
        (nq, nkc, nvc, nks, nvs, nkw, nvw, ngate,
         mq, mk, mv, mi, mf, mo, mgate) = jnp.split(proj, offsets, axis=-1)
        heads = lambda a, n: a.reshape(bsz, s, n, -1)
        y_a = nsa_mixer(heads(nq, NSA_HEADS),
                        heads(nkc, NSA_KV_GROUPS), heads(nvc, NSA_KV_GROUPS),
                        heads(nks, NSA_KV_GROUPS), heads(nvs, NSA_KV_GROUPS),
                        heads(nkw, NSA_KV_GROUPS), heads(nvw, NSA_KV_GROUPS),
                        ngate, nsa_q_gain[l], nsa_k_gain[l],
                        (cmp_k_pos[l], cmp_k_w1[l], cmp_k_b1[l], cmp_k_w2[l], cmp_k_b2[l]),
                        (cmp_v_pos[l], cmp_v_w1[l], cmp_v_b1[l], cmp_v_w2[l], cmp_v_b2[l]),
                        rel_table)
        y_b = mlstm_mixer(mq, mk, mv, mi, mf, mo, ml_conv_w[l], ml_conv_b[l], ml_i_bias[l], ml_f_bias[l])
        gate_a, gate_b = jnp.split(jax.nn.sigmoid(mgate), 2, axis=-1)
        mixed = gate_a * (y_a @ w_branch_a[l]) + gate_b * (y_b @ w_branch_b[l])
        x = x + mixed @ w_out[l]
        h2 = rms_norm(x, norm2_g[l])
        x = x + jnp.square(jax.nn.relu(h2 @ w_ff1[l])) @ w_ff2[l]
    return x
```

```python
import contextlib
import numpy as np
import ml_dtypes
import concourse.bass as bass
import concourse.mybir as mybir
from concourse.bass_utils import run_bass_kernel_spmd

F32 = mybir.dt.float32
BF16 = mybir.dt.bfloat16
ALU = mybir.AluOpType
AF = mybir.ActivationFunctionType
AX = mybir.AxisListType

D = 1024
DPROJ = 5408
NRES = 3360
NEG = -30000.0


class Prog:
    CE = ("pe", "act", "dve", "pool")
    NDS = 12

    def __init__(self, nc, es):
        self.nc = nc
        self.ops = []
        self.sem = {e: es.enter_context(nc.semaphore("sem_" + e)) for e in self.CE}
        self.cnt = {e: 0 for e in self.CE}
        self.dsem = {q: [es.enter_context(nc.semaphore("dsem_%s%d" % (q, i))) for i in range(self.NDS)]
                     for q in ("sp", "pool", "act")}
        self.dtot = {q: [0] * self.NDS for q in self.dsem}
        self.drr = {q: 0 for q in self.dsem}
        self.waited = {e: {} for e in ("pe", "act", "dve", "pool", "sp")}
        self.lastw = {}
        self.readers = {}
        self.done = {}
        self.nops = 0
        self.stage_first = True

    def op(self, eng, fn, reads=(), writes=(), dma=False):
        deps = set()
        for k in reads:
            if k in self.lastw:
                deps.add(self.lastw[k])
        for k in writes:
            relax = (not dma) and eng in ("dve", "act", "pe")
            if k in self.lastw and not (relax and not self.ops[self.lastw[k]]["dma"] and self.ops[self.lastw[k]]["eng"] == eng):
                deps.add(self.lastw[k])
            for r in self.readers.get(k, ()):
                if not (relax and not self.ops[r]["dma"] and self.ops[r]["eng"] == eng):
                    deps.add(r)
        i = len(self.ops)
        deps.discard(i)
        if eng == "pe":
            deps = {d for d in deps if self.ops[d]["eng"] != "pe" or self.ops[d]["dma"]}
        self.ops.append(dict(eng=eng, fn=fn, deps=deps, dma=dma, flag=dma))
        for k in reads:
            self.readers.setdefault(k, []).append(i)
        for k in writes:
            self.lastw[k] = i
            self.readers[k] = []
        return i

    def dma(self, q, out, in_, reads=(), writes=()):
        return self.op(q, lambda e: e.dma_start(out=out, in_=in_), reads, writes, dma=True)

    def flush(self):
        ops = self.ops
        for o in ops:
            for d in o["deps"]:
                ops[d]["flag"] = True
        last = {}
        for i, o in enumerate(ops):
            last[o["eng"]] = i
        for e, i in last.items():
            ops[i]["flag"] = True
        comp = {}
        pre_wait = {}
        for i, o in enumerate(ops):
            e = o["eng"]
            if o["dma"]:
                s = self.drr[e] % self.NDS
                self.drr[e] += 1
                pre_wait[i] = (self.dsem[e][s], self.dtot[e][s])
                self.dtot[e][s] += 16
                comp[i] = (self.dsem[e][s], self.dtot[e][s], 16)
            elif o["flag"]:
                self.cnt[e] += 1
                comp[i] = (self.sem[e], self.cnt[e], 1)
        barrier = None
        if not self.stage_first:
            barrier = self.barrier_vals
        byeng = {}
        for i, o in enumerate(ops):
            byeng.setdefault(o["eng"], []).append(i)

        def emit(eng_name, eobj):
            w = self.waited[eng_name]

            def wait(sem, val):
                if val <= 0:
                    return
                key = id(sem)
                if w.get(key, 0) >= val:
                    return
                w[key] = val
                eobj.wait_ge(sem, val)
            if barrier is not None:
                for sem, val in barrier:
                    wait(sem, val)
            for i in byeng.get(eng_name, []):
                o = ops[i]
                for d in sorted(o["deps"]):
                    sem, val, _ = comp[d]
                    wait(sem, val)
                if i in pre_wait:
                    wait(*pre_wait[i])
                inst = o["fn"](eobj)
                if i in comp:
                    sem, val, inc = comp[i]
                    inst.then_inc(sem, inc)
            if getattr(self, "final", False):
                for q in self.dsem:
                    for s in range(self.NDS):
                        wait(self.dsem[q][s], self.dtot[q][s])

        with self.nc.Block() as block:
            block.sync(lambda e: emit("sp", e))
            block.tensor(lambda e: emit("pe", e))
            block.scalar(lambda e: emit("act", e))
            block.vector(lambda e: emit("dve", e))
            block.gpsimd(lambda e: emit("pool", e))
        bv = [(self.sem[e], self.cnt[e]) for e in self.CE]
        for q in self.dsem:
            for s in range(self.NDS):
                bv.append((self.dsem[q][s], self.dtot[q][s]))
        self.barrier_vals = bv
        self.stage_first = False
        self.nops += len(ops)
        self.ops = []
        self.lastw = {}
        self.readers = {}


def _bc(ap, shape):
    return ap.to_broadcast(shape)


def build_program(NT, stages=("s1", "s2", "s3", "s4", "s5"), debug=False):
    NJ = NT // 2
    NTOK = NT * 128
    NOWN = NJ * 128
    nc = bass.Bass("TRN2", target_bir_lowering=False)

    def din(name, shape, dt=F32):
        return nc.dram_tensor(name, list(shape), dt, kind="ExternalInput").ap()

    def dscr(name, shape, dt=F32):
        return nc.dram_tensor(name, list(shape), dt, kind="ExternalOutput" if debug else "Internal").ap()

    x_loc = din("x_loc", [NTOK, D])
    w_in = din("w_in", [D, DPROJ])
    norm1_g = din("norm1_g", [D])
    ident_in = din("c_ident", [128, 128])
    out = nc.dram_tensor("out", [NOWN, D], F32, kind="ExternalOutput").ap()

    proj = dscr("proj", [NTOK, NRES])
    projT = dscr("projT", [1280, NTOK + 16])
    conv_w = din("ml_conv_w", [4, 1024])
    conv_b = din("ml_conv_b", [1024])
    i_bias = din("ml_i_bias", [4])
    f_bias = din("ml_f_bias", [4])
    kvalid_tm = din("kvalid_tm", [128, NT])
    tri_in = din("c_tri", [128, 128])
    yb = dscr("yb", [NOWN, 512], BF16)
    ya = dscr("ya", [NOWN, 512], BF16)
    norm2_g = din("norm2_g", [D])
    w_pa = din("w_branch_a", [512, D])
    w_pb = din("w_branch_b", [512, D])
    w_out = din("w_out", [D, D])
    w_ff1 = din("w_ff1", [D, 4 * D])
    w_ff2 = din("w_ff2", [4 * D, D])
    NCB = 8 * NT - 1
    CT = (NCB + 127) // 128
    cw1 = din("cmp_w1", [2, 2048, 256])
    cb1 = din("cmp_b1", [2, 256])
    cw2 = din("cmp_w2", [2, 256, 64])
    cb2 = din("cmp_b2", [2, 64])
    cpos = din("cmp_pos", [2, 32, 64])
    k_gain = din("nsa_k_gain", [3, 64])
    q_gain = din("nsa_q_gain", [64])
    cvalid = din("cvalid", [1, CT * 128])
    kvalid_row = din("kvalid_row", [1, NTOK])
    f0row = din("f0row", [128])
    rel31 = din("rel31", [8])
    Bg = din("c_Bg", [2, 128, 8, 128])
    Bcg = din("c_Bcg", [8, 128, 8, 128])
    Mc = din("c_Mc", [8, 128, 128])
    causal_in = din("c_causal", [128, 128])
    w4_in = din("c_w4", [128, 128])
    c2s_in = din("c_c2s", [CT * 128, 128])
    onehot_in = din("c_onehot", [128, NTOK])
    vmB_in = din("c_vmB", [128, 256])
    amB_in = din("c_amB", [128, 256])
    fzB_in = din("c_fzB", [128, 256])
    bc_scr = dscr("bc_scr", [8, 2, 128, 1024], BF16)
    kc_scr = dscr("kc_scr", [2, 65, CT * 128], BF16)
    vc_scr = dscr("vc_scr", [2, CT * 128, 65], BF16)

    es = contextlib.ExitStack()
    with es:
        es.enter_context(nc.allow_non_contiguous_dma(reason="small parameter vectors / layout loads"))
        P = Prog(nc, es)
        ident = es.enter_context(nc.sbuf_tensor("ident", [128, 128], BF16))
        identf = es.enter_context(nc.sbuf_tensor("identf", [128, 128], F32))
        P.dma("sp", identf[:], ident_in[:, :], writes=["identf"])
        P.dma("pool", ident[:], ident_in[:, :], writes=["ident"])

        if "s1" in stages:
            stage1(nc, P, NT, x_loc, w_in, norm1_g, proj, projT, ident)
        if "s2" in stages:
            stage2(nc, P, NT, proj, projT, conv_w, conv_b, i_bias, f_bias, kvalid_tm, yb, ident, identf, tri_in, tri_in)
        if "s3" in stages:
            stage3(nc, P, NT, projT, cw1, cb1, cw2, cb2, cpos, k_gain[0], cvalid, kc_scr, vc_scr, ident)
        if "s4" in stages:
            stage4(nc, P, NT, proj, kc_scr, vc_scr, q_gain, k_gain, kvalid_row, f0row, rel31, Bg, Bcg, Mc, causal_in, w4_in,
                   c2s_in, onehot_in, vmB_in, amB_in, fzB_in, ya, ident, identf, bc_scr)
        if "s5" in stages:
            stage5(nc, P, NT, x_loc, ya, yb, w_in, norm1_g, norm2_g, w_pa, w_pb, w_out, w_ff1, w_ff2, out, ident)
        P.final = True
        P.flush()
    return nc


def stage1(nc, P, NT, x_loc, w_in, norm1_g, proj, projT, ident):
    NB = NT // 4
    with contextlib.ExitStack() as es:
        W = es.enter_context(nc.sbuf_tensor("s1_W", [128, 8, NRES], BF16))
        g1T = es.enter_context(nc.sbuf_tensor("s1_g1T", [128, 8], F32))
        xt = [es.enter_context(nc.sbuf_tensor("s1_xt%d" % i, [128, D], F32)) for i in range(2)]
        junk = es.enter_context(nc.sbuf_tensor("s1_junk", [128, D], BF16))
        xn = [es.enter_context(nc.sbuf_tensor("s1_xn%d" % i, [128, D], BF16)) for i in range(2)]
        st = [es.enter_context(nc.sbuf_tensor("s1_st%d" % i, [128, 4], F32)) for i in range(2)]
        hT = [es.enter_context(nc.sbuf_tensor("s1_hT%d" % i, [128, 8, 512], BF16)) for i in range(2)]
        stg = [es.enter_context(nc.sbuf_tensor("s1_stg%d" % i, [128, 2080], F32)) for i in range(2)]
        stgT = [es.enter_context(nc.sbuf_tensor("s1_stgT%d" % i, [128, 512], F32)) for i in range(3)]
        zt = es.enter_context(nc.sbuf_tensor("s1_z", [128, 16], F32))
        pT = [es.enter_context(nc.psum_tensor("s1_pT%d" % i, [128, 8, 128], BF16)) for i in range(2)]
        pp = [es.enter_context(nc.psum_tensor("s1_pp%d" % i, [128, 512], F32)) for i in range(4)]

        for k in range(8):
            P.dma("pool", W[:, k, :], w_in[k * 128:(k + 1) * 128, 0:NRES], writes=[("W", k)])
        P.dma("sp", g1T[:], norm1_g.rearrange("(c p) -> p c", p=128), writes=["g1T"])
        P.op("dve", lambda e: e.memset(zt[:], 0.0), writes=["zt"])
        for r in range(10):
            P.dma("sp", projT[r * 128:(r + 1) * 128, 0:16], zt[:], reads=["zt"], writes=[("projT_z", r)])

        TM = [(0, 512), (768, 512), (1280, 24), (2328, 512), (2840, 8), (2848, 512)]
        tm_off = []
        o = 0
        for c0, wd in TM:
            tm_off.append(o)
            o += wd
        CM = [(512, 64, 0), (576, 64, 64), (640, 64, 128), (704, 64, 192)]
        for h in range(4):
            CM.append((1304 + 128 * h, 128, 256 + 128 * h))
        for h in range(4):
            CM.append((1816 + 128 * h, 128, 768 + 128 * h))

        ppi = 0
        evi = 0
        for b in range(NB):
            hb = hT[b % 2]
            for i in range(4):
                t = b * 4 + i
                xb = xt[t % 2]
                xnb = xn[t % 2]
                stb = st[t % 2]
                ptb = pT[t % 2]
                P.dma("sp", xb[:], x_loc[t * 128:(t + 1) * 128, :], writes=[("xt", t % 2)])
                P.op("act", lambda e, xb=xb, stb=stb: e.activation(out=junk[:], in_=xb[:], func=AF.Square, accum_out=stb[:, 0:1]),
                     reads=[("xt", t % 2)], writes=["junk", ("st0", t % 2)])
                P.op("act", lambda e, stb=stb: e.activation(out=stb[:, 1:2], in_=stb[:, 0:1], func=AF.Ln, scale=1.0 / D, bias=1e-6),
                     reads=[("st0", t % 2)], writes=[("st1", t % 2)])
                P.op("act", lambda e, stb=stb: e.activation(out=stb[:, 2:3], in_=stb[:, 1:2], func=AF.Exp, scale=-0.5),
                     reads=[("st1", t % 2)], writes=[("st2", t % 2)])
                P.op("dve", lambda e, xb=xb, xnb=xnb, stb=stb: e.tensor_scalar(out=xnb[:], in0=xb[:], scalar1=stb[:, 2:3], scalar2=None, op0=ALU.mult),
                     reads=[("xt", t % 2), ("st2", t % 2)], writes=[("xn", t % 2)])
                for c in range(8):
                    P.op("pe", lambda e, c=c, xnb=xnb, ptb=ptb: e.transpose(out=ptb[:, c, :], in_=xnb[:, c * 128:(c + 1) * 128], identity=ident[:]),
                         reads=[("xn", t % 2), "ident"], writes=[("pT", t % 2)])
                P.op("dve", lambda e, hb=hb, ptb=ptb, i=i: e.tensor_tensor(out=hb[:, :, i * 128:(i + 1) * 128], in0=ptb[:], in1=_bc(g1T[:].rearrange("p (c o) -> p c o", o=1), [128, 8, 128]), op=ALU.mult),
                     reads=[("pT", t % 2), "g1T"], writes=[("hT", b % 2, i)])
            for i in range(4):
                t = b * 4 + i
                sg = stg[t % 2]
                for gi, (c0, wd) in enumerate(TM):
                    if t % 2 == 0 and gi in (0, 5):
                        continue
                    pb = pp[ppi % 4]
                    pk = ("pp", ppi % 4)
                    ppi += 1
                    for k in range(8):
                        P.op("pe", lambda e, pb=pb, hb=hb, i=i, k=k, c0=c0, wd=wd: e.matmul(pb[:, 0:wd], lhsT=hb[:, k, i * 128:(i + 1) * 128], rhs=W[:, k, c0:c0 + wd], start=(k == 0), stop=(k == 7)),
                             reads=[("hT", b % 2, i), ("W", k)], writes=[pk])
                    eng = "dve" if evi % 2 == 0 else "act"
                    evi += 1
                    so = tm_off[gi]
                    if eng == "dve":
                        P.op("dve", lambda e, sg=sg, pb=pb, so=so, wd=wd: e.tensor_copy(out=sg[:, so:so + wd], in_=pb[:, 0:wd]),
                             reads=[pk], writes=[("stg", t % 2, gi)])
                    else:
                        P.op("act", lambda e, sg=sg, pb=pb, so=so, wd=wd: e.activation(out=sg[:, so:so + wd], in_=pb[:, 0:wd], func=AF.Copy),
                             reads=[pk], writes=[("stg", t % 2, gi)])
                    P.dma("sp", proj[t * 128:(t + 1) * 128, c0:c0 + wd], sg[:, so:so + wd], reads=[("stg", t % 2, gi)], writes=[("proj", t, gi)])
            for ci, (c0, M, r0) in enumerate(CM):
                pb = pp[ppi % 4]
                pk = ("pp", ppi % 4)
                ppi += 1
                sT = stgT[ci % 3]
                for k in range(8):
                    P.op("pe", lambda e, pb=pb, hb=hb, k=k, c0=c0, M=M: e.matmul(pb[0:M, :], lhsT=W[:, k, c0:c0 + M], rhs=hb[:, k, :], start=(k == 0), stop=(k == 7)),
                         reads=[("hT", b % 2, 0), ("hT", b % 2, 1), ("hT", b % 2, 2), ("hT", b % 2, 3), ("W", k)], writes=[pk])
                eng = "dve" if evi % 2 == 0 else "act"
                evi += 1
                if eng == "dve":
                    P.op("dve", lambda e, sT=sT, pb=pb, M=M: e.tensor_copy(out=sT[0:M, :], in_=pb[0:M, :]), reads=[pk], writes=[("stgT", ci % 3)])
                else:
                    P.op("act", lambda e, sT=sT, pb=pb, M=M: e.activation(out=sT[0:M, :], in_=pb[0:M, :], func=AF.Copy), reads=[pk], writes=[("stgT", ci % 3)])
                P.dma("sp", projT[r0:r0 + M, 16 + b * 512:16 + (b + 1) * 512], sT[0:M, :], reads=[("stgT", ci % 3)], writes=[("projT", ci, b)])
        P.flush()


def stage2(nc, P, NT, proj, projT, conv_w, conv_b, i_bias, f_bias, kvalid_tm, yb, ident, identf, tri_in, mask_in):
    NB = NT // 4
    with contextlib.ExitStack() as es:
        def sb(name, shape, dt=F32):
            return es.enter_context(nc.sbuf_tensor("s2_" + name, shape, dt))

        def ps(name, shape, dt=F32):
            return es.enter_context(nc.psum_tensor("s2_" + name, shape, dt))
        cw = sb("cw", [128, 4, 8])
        cb = sb("cb", [128, 8])
        ib = sb("ib", [128, 4])
        fb = sb("fb", [128, 4])
        kv = sb("kv", [128, NT])
        tri = sb("tri", [128, 128])
        maskT = sb("maskT", [128, 128])
        ctmp = sb("ctmp", [128, 512])
        qkT = [sb("qkT%d" % i, [128, 8, 515]) for i in range(2)]
        acc = [sb("acc%d" % i, [128, 8, 512]) for i in range(2)]
        sg = sb("sg", [128, 8, 512])
        gl = [sb("gl%d" % i, [128, 4, 8]) for i in range(2)]
        ga = [sb("ga%d" % i, [128, 3, 4, 4]) for i in range(2)]
        mv = sb("mv", [128, 4, 512])
        mo = [sb("mo%d" % i, [128, 2, 512]) for i in range(2)]
        vaug = [sb("vaug%d" % i, [128, 4, 4, 129], BF16) for i in range(2)]
        U4 = [sb("U4%d" % i, [128, 4, 128]) for i in range(2)]
        R4 = [sb("R4%d" % i, [128, 4, 128]) for i in range(2)]
        qp4 = [sb("qp4%d" % i, [128, 4, 128], BF16) for i in range(2)]
        kpT4 = [sb("kpT4%d" % i, [128, 4, 128], BF16) for i in range(2)]
        kp4 = [sb("kp4%d" % i, [128, 4, 128], BF16) for i in range(2)]
        wT4 = [sb("wT4%d" % i, [128, 4, 128], BF16) for i in range(2)]
        dn4 = sb("dn4", [128, 4, 2])
        ybt = [sb("ybt%d" % i, [128, 512], BF16) for i in range(2)]
        Cf4 = sb("Cf4", [128, 4, 129])
        Cb4 = sb("Cb4", [128, 4, 129], BF16)
        PC = ps("PC", [128, 512])
        PR = ps("PR", [128, 512])
        PS = ps("PS", [128, 512])
        pD = [ps("pD%d" % i, [128, 512]) for i in range(2)]
        pOb = [ps("pOb%d" % i, [128, 512]) for i in range(2)]
        pk = ps("pk", [128, 1024], BF16)

        for jj in range(4):
            P.dma("sp", cw[:, jj, :], conv_w[jj, :].rearrange("(c p) -> p c", p=128), writes=[("cw", jj)])
        cwk = [("cw", jj) for jj in range(4)]
        P.dma("sp", cb[:], conv_b.rearrange("(c p) -> p c", p=128), writes=["cb"])
        P.dma("sp", ib[:], bass.AP(i_bias.tensor, 0, [[0, 128], [1, 4]]), writes=["ib"])
        P.dma("sp", fb[:], bass.AP(f_bias.tensor, 0, [[0, 128], [1, 4]]), writes=["fb"])
        P.dma("sp", kv[:], kvalid_tm[:, :], writes=["kv"])
        P.dma("sp", tri[:], tri_in[:, :], writes=["tri"])
        P.dma("sp", maskT[:], mask_in[:, :], writes=["maskT"])
        P.op("dve", lambda e: e.memset(Cf4[:], 0.0), writes=["Cf4"])
        P.op("dve", lambda e: e.memset(Cb4[:], 0.0), writes=["Cb4"])
        for i in range(2):
            P.op("pool", lambda e, i=i: e.memset(vaug[i][:], 1.0), writes=[("vaug", i, ii) for ii in range(4)])

        def front(b):
            pb = b % 2
            c0 = 16 + b * 512 - 3
            rows = slice(b * 512, (b + 1) * 512)
            P.dma("sp", qkT[pb][:], projT[256:1280, c0:c0 + 515].rearrange("(c p) n -> p c n", p=128), writes=[("qkT", pb)])
            P.dma("sp", gl[pb][:], proj[rows, 2840:2848].rearrange("(i p) c -> p i c", p=128), writes=[("gl", pb)])
            P.dma("sp", mv[:], proj[rows, 2328:2840].rearrange("(i p) c -> p i c", p=128), writes=["mv"])
            for oi in range(2):
                t = b * 4 + 2 * oi + 1
                P.dma("sp", mo[pb][:, oi, :], proj[t * 128:(t + 1) * 128, 2848:3360], writes=[("mo", pb)])
            for c in range(8):
                eng = "dve"
                P.op(eng, lambda e, c=c, pb=pb: e.tensor_scalar(out=acc[pb][:, c, :], in0=qkT[pb][:, c, 0:512], scalar1=cw[:, 0, c:c + 1], scalar2=cb[:, c:c + 1], op0=ALU.mult, op1=ALU.add),
                     reads=[("qkT", pb), "cb"] + cwk, writes=[("acc", pb, c)])
                for jj in range(1, 4):
                    if eng == "dve":
                        P.op(eng, lambda e, c=c, pb=pb, jj=jj: e.scalar_tensor_tensor(out=acc[pb][:, c, :], in0=qkT[pb][:, c, jj:jj + 512], scalar=cw[:, jj, c:c + 1], in1=acc[pb][:, c, :], op0=ALU.mult, op1=ALU.add),
                             reads=[("qkT", pb), ("acc", pb, c)] + cwk, writes=[("acc", pb, c)])
                    else:
                        P.op(eng, lambda e, c=c, pb=pb, jj=jj: e.tensor_scalar(out=ctmp[:], in0=qkT[pb][:, c, jj:jj + 512], scalar1=cw[:, jj, c:c + 1], scalar2=None, op0=ALU.mult),
                             reads=[("qkT", pb)] + cwk, writes=["ctmp"])
                        P.op(eng, lambda e, c=c, pb=pb: e.tensor_tensor(out=acc[pb][:, c, :], in0=acc[pb][:, c, :], in1=ctmp[:], op=ALU.add),
                             reads=["ctmp", ("acc", pb, c)], writes=[("acc", pb, c)])
            acck = [("acc", pb, c) for c in range(8)]
            for half in range(2):
                hs = slice(half * 4, half * 4 + 4)
                hk = acck[half * 4:half * 4 + 4]
                P.op("act", lambda e, pb=pb, hs=hs: e.activation(out=sg[:, hs, :], in_=acc[pb][:, hs, :], func=AF.Exp, scale=-1.0), reads=hk, writes=[("sg", half)])
                P.op("act", lambda e, hs=hs: e.activation(out=sg[:, hs, :], in_=sg[:, hs, :], func=AF.Ln, bias=1.0), reads=[("sg", half)], writes=[("sg", half)])
                P.op("act", lambda e, hs=hs: e.activation(out=sg[:, hs, :], in_=sg[:, hs, :], func=AF.Exp, scale=-1.0), reads=[("sg", half)], writes=[("sg", half)])
                P.op("dve", lambda e, pb=pb, hs=hs: e.tensor_tensor(out=acc[pb][:, hs, :], in0=acc[pb][:, hs, :], in1=sg[:, hs, :], op=ALU.mult), reads=hk + [("sg", half)], writes=hk)
            P.op("dve", lambda e, pb=pb: e.tensor_tensor(out=ga[pb][:, 2], in0=gl[pb][:, :, 4:8], in1=_bc(fb[:].rearrange("p (o h) -> p o h", o=1), [128, 4, 4]), op=ALU.add), reads=[("gl", pb), "fb"], writes=[("ga2", pb)])
            P.op("act", lambda e, pb=pb: e.activation(out=ga[pb][:, 2], in_=ga[pb][:, 2], func=AF.Exp, scale=-1.0), reads=[("ga2", pb)], writes=[("ga2", pb)])
            P.op("act", lambda e, pb=pb: e.activation(out=ga[pb][:, 0], in_=ga[pb][:, 2], func=AF.Ln, bias=1.0), reads=[("ga2", pb)], writes=[("ga0", pb)])
            P.op("dve", lambda e, pb=pb: e.tensor_tensor(out=ga[pb][:, 1], in0=gl[pb][:, :, 0:4], in1=_bc(ib[:].rearrange("p (o h) -> p o h", o=1), [128, 4, 4]), op=ALU.add), reads=[("gl", pb), "ib"], writes=[("ga1", pb)])
            P.op("dve", lambda e, pb=pb, b=b: e.tensor_tensor(out=ga[pb][:, 1], in0=ga[pb][:, 1], in1=_bc(kv[:, 4 * b:4 * b + 4].rearrange("p (i o) -> p i o", o=1), [128, 4, 4]), op=ALU.add), reads=[("ga1", pb), "kv"], writes=[("ga1", pb)])
            for i in range(4):
                P.op("act", lambda e, pb=pb, i=i: e.activation(out=vaug[pb][:, i, :, 0:128], in_=mv[:, i, :].rearrange("p (h d) -> p h d", d=128), func=AF.Copy), reads=["mv"], writes=[("vaug", pb, i)])
            P.op("act", lambda e, pb=pb: e.activation(out=mo[pb][:], in_=mo[pb][:], func=AF.Exp, scale=-1.0), reads=[("mo", pb)], writes=[("mo", pb)])
            P.op("act", lambda e, pb=pb: e.activation(out=mo[pb][:], in_=mo[pb][:], func=AF.Ln, bias=1.0), reads=[("mo", pb)], writes=[("mo", pb)])
            P.op("act", lambda e, pb=pb: e.activation(out=mo[pb][:], in_=mo[pb][:], func=AF.Exp, scale=-1.0), reads=[("mo", pb)], writes=[("mo", pb)])

        s_ = 128.0 ** -0.5

        def levels(t):
            b, i = t // 4, t % 4
            pb = b % 2
            tp = t % 2
            own = (t % 2 == 1)
            j = t // 2
            oi = i // 2
            yp = j % 2
            ts_ = slice(i * 128, (i + 1) * 128)
            A = {}

            def A1():
                for h in range(4):
                    a_col = ga[pb][:, 0, i, h:h + 1]
                    li_col = ga[pb][:, 1, i, h:h + 1]
                    hs = slice(h * 128, (h + 1) * 128)
                    P.op("pe", lambda e, a_col=a_col, hs=hs: e.matmul(PC[:, hs], lhsT=_bc(a_col, [128, 128]), rhs=tri[:], start=True, stop=True), reads=[("ga0", pb), "tri"], writes=["PC"])
                    P.op("pe", lambda e, a_col=a_col, hs=hs: e.matmul(PR[:, hs], lhsT=_bc(a_col, [128, 128]), rhs=tri[:], start=True, stop=False), reads=[("ga0", pb), "tri"], writes=["PR"])
                    P.op("pe", lambda e, li_col=li_col, hs=hs: e.matmul(PR[:, hs], lhsT=_bc(li_col, [128, 128]), rhs=identf[:], start=False, stop=True), reads=[("ga1", pb), "identf"], writes=["PR"])

            def A2():
                P.op("act", lambda e: e.activation(out=U4[tp][:].rearrange("p h n -> p (h n)"), in_=PC[:, :], func=AF.Exp, scale=-1.0), reads=["PC"], writes=[("U4", tp)])
                P.op("act", lambda e: e.activation(out=R4[tp][:].rearrange("p h n -> p (h n)"), in_=PR[:, :], func=AF.Exp), reads=["PR"], writes=[("R4", tp)])

            def A3():
                P.op("dve", lambda e: e.scalar_tensor_tensor(out=qp4[tp][:], in0=acc[pb][:, 0:4, ts_], scalar=s_, in1=U4[tp][:], op0=ALU.mult, op1=ALU.mult),
                     reads=[("acc", pb, c) for c in range(4)] + [("U4", tp)], writes=[("qp4", tp)])
                P.op("dve", lambda e: e.tensor_tensor(out=kpT4[tp][:], in0=acc[pb][:, 4:8, ts_], in1=R4[tp][:], op=ALU.mult),
                     reads=[("acc", pb, c) for c in range(4, 8)] + [("R4", tp)], writes=[("kpT4", tp)])

            def A4():
                for h in range(4):
                    P.op("pe", lambda e, h=h: e.transpose(out=pk[:, h * 128:(h + 1) * 128], in_=kpT4[tp][:, h, :], identity=ident[:]), reads=[("kpT4", tp), "ident"], writes=["pk"])

            def A5():
                P.op("act", lambda e: e.activation(out=kp4[tp][:].rearrange("p h n -> p (h n)"), in_=pk[:, 0:512], func=AF.Copy), reads=["pk"], writes=[("kp4", tp)])

            def B6():
                for h in range(4):
                    pd = pD[h // 2][:, (h % 2) * 129:(h % 2) * 129 + 129]
                    P.op("pe", lambda e, h=h, pd=pd: e.matmul(pd, lhsT=kp4[tp][:, h, :], rhs=vaug[pb][:, i, h, :], start=True, stop=True), reads=[("kp4", tp), ("vaug", pb, i)], writes=[("pD", h // 2)])
                if own:
                    for h in range(4):
                        P.op("pe", lambda e, h=h: e.matmul(PS[:, h * 128:(h + 1) * 128], lhsT=kpT4[tp][:, h, :], rhs=qp4[tp][:, h, :], start=True, stop=True), reads=[("kpT4", tp), ("qp4", tp)], writes=["PS"])

            def B7():
                if own:
                    P.op("dve", lambda e: e.tensor_tensor(out=wT4[tp][:], in0=PS[:, :].rearrange("p (h n) -> p h n", n=128), in1=_bc(maskT[:].rearrange("p (o n) -> p o n", o=1), [128, 4, 128]), op=ALU.mult), reads=["PS", "maskT"], writes=[("wT4", tp)])

            def B8():
                if own:
                    for h in range(4):
                        pO = pOb[h // 2][:, (h % 2) * 129:(h % 2) * 129 + 129]
                        P.op("pe", lambda e, h=h, pO=pO: e.matmul(pO, lhsT=wT4[tp][:, h, :], rhs=vaug[pb][:, i, h, :], start=True, stop=False), reads=[("wT4", tp), ("vaug", pb, i)], writes=[("pO", h // 2)])
                        P.op("pe", lambda e, h=h, pO=pO: e.matmul(pO, lhsT=qp4[tp][:, h, :], rhs=Cb4[:, h, :], start=False, stop=True), reads=[("qp4", tp), "Cb4"], writes=[("pO", h // 2)])

            def B9():
                for k in range(2):
                    P.op("dve", lambda e, k=k: e.tensor_tensor(out=Cf4[:, 2 * k:2 * k + 2, :], in0=pD[k][:, 0:258].rearrange("p (h e) -> p h e", e=129), in1=Cf4[:, 2 * k:2 * k + 2, :], op=ALU.add), reads=[("pD", k), "Cf4"], writes=["Cf4"])
                P.op("dve", lambda e: e.tensor_tensor(out=Cf4[:], in0=Cf4[:], in1=_bc(U4[tp][:, :, 127:128], [128, 4, 129]), op=ALU.mult), reads=["Cf4", ("U4", tp)], writes=["Cf4"])
                if own:
                    for k in range(2):
                        den = pOb[k][:, 0:258].rearrange("p (h e) -> p h e", e=129)[:, :, 128:129]
                        dk_ = dn4[:, 2 * k:2 * k + 2, :]
                        P.op("dve", lambda e, den=den, dk_=dk_: e.tensor_scalar(out=dk_[:, :, 1:2], in0=den, scalar1=-1.0, scalar2=1.0, op0=ALU.mult, op1=ALU.max), reads=[("pO", k)], writes=[("dn1", k)])
                        P.op("dve", lambda e, den=den, dk_=dk_: e.scalar_tensor_tensor(out=dk_[:, :, 0:1], in0=den, scalar=1.0, in1=dk_[:, :, 1:2], op0=ALU.max, op1=ALU.max), reads=[("pO", k), ("dn1", k)], writes=[("dn0", k)])
                        P.op("dve", lambda e, dk_=dk_: e.reciprocal(out=dk_[:, :, 1:2], in_=dk_[:, :, 0:1]), reads=[("dn0", k)], writes=[("dn1", k)])
                    for h in range(4):
                        pO = pOb[h // 2][:, (h % 2) * 129:(h % 2) * 129 + 128]
                        P.op("dve", lambda e, h=h, pO=pO: e.scalar_tensor_tensor(out=ybt[yp][:, h * 128:(h + 1) * 128], in0=pO, scalar=dn4[:, h, 1:2], in1=mo[pb][:, oi, h * 128:(h + 1) * 128], op0=ALU.mult, op1=ALU.mult),
                             reads=[("pO", h // 2), ("dn1", h // 2), ("mo", pb)], writes=[("ybt", yp, h)])
                P.op("act", lambda e: e.activation(out=Cb4[:].rearrange("p h e -> p (h e)"), in_=Cf4[:].rearrange("p h e -> p (h e)"), func=AF.Copy), reads=["Cf4"], writes=["Cb4"])
                if own:
                    P.dma("sp", yb[j * 128:(j + 1) * 128, :], ybt[yp][:], reads=[("ybt", yp, h) for h in range(4)], writes=[("yb", j)])
            return [A1, A2, A3, A4, A5], [B6, B7, B8, B9]

        front(0)
        if NB > 1:
            front(1)
        lv = {0: levels(0)}
        for f in lv[0][0]:
            f()
        for t in range(NT):
            if t % 4 == 0 and t // 4 + 2 < NB and t > 0:
                pass
            if t + 1 < NT:
                lv[t + 1] = levels(t + 1)
                An = lv[t + 1][0]
            else:
                An = []
            Bc = lv[t][1]
            for k in range(5):
                if k < len(An):
                    An[k]()
                if k < len(Bc):
                    Bc[k]()
            if t % 4 == 3:
                nb_ = t // 4 + 2
                if nb_ < NB:
                    front(nb_)
            del lv[t]
        P.flush()


def stage5(nc, P, NT, x_loc, ya, yb, w_in, norm1_g, norm2_g, w_pa, w_pb, w_out, w_ff1, w_ff2, out, ident):
    NJ = NT // 2
    NBK = (NJ + 3) // 4
    with contextlib.ExitStack() as es:
        def sb(name, shape, dt=F32):
            return es.enter_context(nc.sbuf_tensor("s5_" + name, shape, dt))

        def ps(name, shape, dt=F32):
            return es.enter_context(nc.psum_tensor("s5_" + name, shape, dt))
        Wg = sb("Wg", [128, 8, 2048], BF16)
        PA = sb("PA", [128, 4, 1024], BF16)
        PB = sb("PB", [128, 4, 1024], BF16)
        Wo = sb("Wo", [128, 8, 1024], BF16)
        g1T = sb("g1T", [128, 8])
        g2T = sb("g2T", [128, 8])
        xk = [sb("xk%d" % i, [128, D]) for i in range(4)]
        junk = sb("junk", [128, D], BF16)
        xn = [sb("xn%d" % i, [128, D], BF16) for i in range(2)]
        st = [sb("st%d" % i, [128, 4]) for i in range(2)]
        yat = [sb("yat%d" % i, [128, 512], BF16) for i in range(2)]
        ybt = [sb("ybt%d" % i, [128, 512], BF16) for i in range(2)]
        hT = sb("hT", [128, 8, 512], BF16)
        yaT = sb("yaT", [128, 4, 512], BF16)
        ybT = sb("ybT", [128, 4, 512], BF16)
        mixT = sb("mixT", [128, 8, 512], BF16)
        sa = [sb("sa%d" % i, [128, 512]) for i in range(2)]
        sbb = [sb("sbb%d" % i, [128, 512]) for i in range(2)]
        W1c = [sb("W1c%d" % i, [128, 8, 512], BF16) for i in range(2)]
        W2c = [sb("W2c%d" % i, [128, 4, 1024], BF16) for i in range(2)]
        rl = [sb("rl%d" % i, [128, 512]) for i in range(2)]
        aT = [sb("aT%d" % i, [128, 4, 512], BF16) for i in range(2)]
        pT = [ps("pT%d" % i, [128, 8, 128], BF16) for i in range(2)]
        pq = [ps("pq%d" % i, [128, 512]) for i in range(6)]
        pqi = [0]

        def nextp():
            i = pqi[0] % 6
            pqi[0] += 1
            return pq[i], ("pq", i)

        for k in range(8):
            P.dma("pool", Wg[:, k, :], w_in[k * 128:(k + 1) * 128, NRES:DPROJ], writes=[("Wg", k)])
            P.dma("pool", Wo[:, k, :], w_out[k * 128:(k + 1) * 128, :], writes=[("Wo", k)])
        for k in range(4):
            P.dma("pool", PA[:, k, :], w_pa[k * 128:(k + 1) * 128, :], writes=[("PA", k)])
            P.dma("pool", PB[:, k, :], w_pb[k * 128:(k + 1) * 128, :], writes=[("PB", k)])
        P.dma("sp", g1T[:], norm1_g.rearrange("(c p) -> p c", p=128), writes=["g1T"])
        P.dma("sp", g2T[:], norm2_g.rearrange("(c p) -> p c", p=128), writes=["g2T"])
        Wgk = [("Wg", k) for k in range(8)]
        Wok = [("Wo", k) for k in range(8)]

        def norm_T(src, srckey, gT, gkey, dstT, dkey, i, q):
            P.op("act", lambda e: e.activation(out=junk[:], in_=src[:], func=AF.Square, accum_out=st[q][:, 0:1]), reads=[srckey], writes=["junk", ("st0", q)])
            P.op("act", lambda e: e.activation(out=st[q][:, 1:2], in_=st[q][:, 0:1], func=AF.Ln, scale=1.0 / D, bias=1e-6), reads=[("st0", q)], writes=[("st1", q)])
            P.op("act", lambda e: e.activation(out=st[q][:, 2:3], in_=st[q][:, 1:2], func=AF.Exp, scale=-0.5), reads=[("st1", q)], writes=[("st2", q)])
            P.op("dve", lambda e: e.tensor_scalar(out=xn[q][:], in0=src[:], scalar1=st[q][:, 2:3], scalar2=None, op0=ALU.mult), reads=[srckey, ("st2", q)], writes=[("xn", q)])
            for c in range(8):
                P.op("pe", lambda e, c=c: e.transpose(out=pT[q][:, c, :], in_=xn[q][:, c * 128:(c + 1) * 128], identity=ident[:]), reads=[("xn", q), "ident"], writes=[("pT", q)])
            P.op("dve", lambda e: e.tensor_tensor(out=dstT[:, :, i * 128:(i + 1) * 128], in0=pT[q][:], in1=_bc(gT[:].rearrange("p (c o) -> p c o", o=1), [128, 8, 128]), op=ALU.mult),
                 reads=[("pT", q), gkey], writes=[(dkey, i)])

        for bk in range(NBK):
            nti = min(4, NJ - bk * 4)
            NTK = nti * 128
            for i in range(nti):
                j = bk * 4 + i
                t = 2 * j + 1
                q = i % 2
                P.dma("sp", xk[i][:], x_loc[t * 128:(t + 1) * 128, :], writes=[("xk", i)])
                P.dma("sp", yat[q][:], ya[j * 128:(j + 1) * 128, :], writes=[("yat", q)])
                P.dma("sp", ybt[q][:], yb[j * 128:(j + 1) * 128, :], writes=[("ybt", q)])
                norm_T(xk[i], ("xk", i), g1T, "g1T", hT, "hT", i, q)
                for (srct, skey, dst, dkey) in ((yat[q], ("yat", q), yaT, "yaT"), (ybt[q], ("ybt", q), ybT, "ybT")):
                    for c in range(4):
                        P.op("pe", lambda e, c=c, srct=srct: e.transpose(out=pT[q][:, c, :], in_=srct[:, c * 128:(c + 1) * 128], identity=ident[:]), reads=[skey, "ident"], writes=[("pT", q)])
                    P.op("act", lambda e, dst=dst, i=i: e.activation(out=dst[:, :, i * 128:(i + 1) * 128], in_=pT[q][:, 0:4, :], func=AF.Copy), reads=[("pT", q)], writes=[(dkey, i)])
            hTk = [("hT", i) for i in range(nti)]
            yaTk = [("yaT", i) for i in range(nti)]
            ybTk = [("ybT", i) for i in range(nti)]
            for f in range(8):
                q = f % 2
                pga, kga = nextp()
                for k in range(8):
                    P.op("pe", lambda e, pga=pga, k=k, f=f: e.matmul(pga[:, 0:NTK], lhsT=Wg[:, k, f * 128:(f + 1) * 128], rhs=hT[:, k, 0:NTK], start=(k == 0), stop=(k == 7)), reads=hTk + Wgk, writes=[kga])
                pgb, kgb = nextp()
                for k in range(8):
                    P.op("pe", lambda e, pgb=pgb, k=k, f=f: e.matmul(pgb[:, 0:NTK], lhsT=Wg[:, k, 1024 + f * 128:1024 + (f + 1) * 128], rhs=hT[:, k, 0:NTK], start=(k == 0), stop=(k == 7)), reads=hTk + Wgk, writes=[kgb])
                P.op("act", lambda e, pga=pga, q=q: e.activation(out=sa[q][:, 0:NTK], in_=pga[:, 0:NTK], func=AF.Exp, scale=-1.0), reads=[kga], writes=[("sa", q)])
                P.op("act", lambda e, pgb=pgb, q=q: e.activation(out=sbb[q][:, 0:NTK], in_=pgb[:, 0:NTK], func=AF.Exp, scale=-1.0), reads=[kgb], writes=[("sbb", q)])
                pa_, kpa = nextp()
                for k in range(4):
                    P.op("pe", lambda e, pa_=pa_, k=k, f=f: e.matmul(pa_[:, 0:NTK], lhsT=PA[:, k, f * 128:(f + 1) * 128], rhs=yaT[:, k, 0:NTK], start=(k == 0), stop=(k == 3)), reads=yaTk + [("PA", kk) for kk in range(4)], writes=[kpa])
                pb_, kpb = nextp()
                for k in range(4):
                    P.op("pe", lambda e, pb_=pb_, k=k, f=f: e.matmul(pb_[:, 0:NTK], lhsT=PB[:, k, f * 128:(f + 1) * 128], rhs=ybT[:, k, 0:NTK], start=(k == 0), stop=(k == 3)), reads=ybTk + [("PB", kk) for kk in range(4)], writes=[kpb])
                for (sx, skey) in ((sa[q], ("sa", q)), (sbb[q], ("sbb", q))):
                    P.op("act", lambda e, sx=sx: e.activation(out=sx[:, 0:NTK], in_=sx[:, 0:NTK], func=AF.Ln, bias=1.0), reads=[skey], writes=[skey])
                    P.op("act", lambda e, sx=sx: e.activation(out=sx[:, 0:NTK], in_=sx[:, 0:NTK], func=AF.Exp, scale=-1.0), reads=[skey], writes=[skey])
                P.op("dve", lambda e, q=q, pa_=pa_: e.tensor_tensor(out=sa[q][:, 0:NTK], in0=pa_[:, 0:NTK], in1=sa[q][:, 0:NTK], op=ALU.mult), reads=[kpa, ("sa", q)], writes=[("sa", q)])
                P.op("dve", lambda e, q=q, pb_=pb_: e.tensor_tensor(out=sbb[q][:, 0:NTK], in0=pb_[:, 0:NTK], in1=sbb[q][:, 0:NTK], op=ALU.mult), reads=[kpb, ("sbb", q)], writes=[("sbb", q)])
                P.op("dve", lambda e, q=q, f=f: e.tensor_tensor(out=mixT[:, f, 0:NTK], in0=sa[q][:, 0:NTK], in1=sbb[q][:, 0:NTK], op=ALU.add), reads=[("sa", q), ("sbb", q)], writes=[("mixT", f)])
            mixk = [("mixT", f) for f in range(8)]
            def wout(i):
                for hh in range(2):
                    po, kpo = nextp()
                    for f in range(8):
                        P.op("pe", lambda e, po=po, f=f, i=i, hh=hh: e.matmul(po[:, :], lhsT=mixT[:, f, i * 128:(i + 1) * 128], rhs=Wo[:, f, hh * 512:(hh + 1) * 512], start=(f == 0), stop=(f == 7)), reads=mixk + Wok, writes=[kpo])
                    P.op("dve", lambda e, po=po, i=i, hh=hh: e.tensor_tensor(out=xk[i][:, hh * 512:(hh + 1) * 512], in0=po[:, :], in1=xk[i][:, hh * 512:(hh + 1) * 512], op=ALU.add), reads=[kpo, ("xk", i)], writes=[("xk", i)])
            wout(0)
            for i in range(nti):
                if i + 1 < nti:
                    wout(i + 1)
                norm_T(xk[i], ("xk", i), g2T, "g2T", hT, "hT", i, i % 2)
            for fc in range(8):
                wq = fc % 2
                P.dma("pool", W1c[wq][:], w_ff1[:, fc * 512:(fc + 1) * 512].rearrange("(k p) n -> p k n", p=128), writes=[("W1c", wq)])
                P.dma("pool", W2c[wq][:], w_ff2[fc * 512:(fc + 1) * 512, :].rearrange("(k p) n -> p k n", p=128), writes=[("W2c", wq)])
                for m in range(4):
                    pf, kpf = nextp()
                    for k in range(8):
                        P.op("pe", lambda e, pf=pf, k=k, m=m, wq=wq: e.matmul(pf[:, 0:NTK], lhsT=W1c[wq][:, k, m * 128:(m + 1) * 128], rhs=hT[:, k, 0:NTK], start=(k == 0), stop=(k == 7)), reads=hTk + [("W1c", wq)], writes=[kpf])
                    rq = m % 2
                    P.op("act", lambda e, pf=pf, rq=rq: e.activation(out=rl[rq][:, 0:NTK], in_=pf[:, 0:NTK], func=AF.Relu), reads=[kpf], writes=[("rl", rq)])
                    P.op("dve", lambda e, rq=rq, m=m, wq=wq: e.tensor_tensor(out=aT[wq][:, m, 0:NTK], in0=rl[rq][:, 0:NTK], in1=rl[rq][:, 0:NTK], op=ALU.mult), reads=[("rl", rq)], writes=[("aT", wq, m)])
                aTk = [("aT", wq, m) for m in range(4)]
                for i in range(nti):
                    for hh in range(2):
                        po, kpo = nextp()
                        for m in range(4):
                            P.op("pe", lambda e, po=po, m=m, i=i, hh=hh, wq=wq: e.matmul(po[:, :], lhsT=aT[wq][:, m, i * 128:(i + 1) * 128], rhs=W2c[wq][:, m, hh * 512:(hh + 1) * 512], start=(m == 0), stop=(m == 3)), reads=aTk + [("W2c", wq)], writes=[kpo])
                        P.op("dve", lambda e, po=po, i=i, hh=hh: e.tensor_tensor(out=xk[i][:, hh * 512:(hh + 1) * 512], in0=po[:, :], in1=xk[i][:, hh * 512:(hh + 1) * 512], op=ALU.add), reads=[kpo, ("xk", i)], writes=[("xk", i)])
            for i in range(nti):
                j = bk * 4 + i
                P.dma("sp", out[j * 128:(j + 1) * 128, :], xk[i][:], reads=[("xk", i)], writes=[("out", j)])
        P.flush()


def stage3(nc, P, NT, projT, cw1, cb1, cw2, cb2, cpos, k_gain, cvalid, kc_scr, vc_scr, ident):
    NTOK = NT * 128
    NCB = 8 * NT - 1
    CT = (NCB + 127) // 128
    with contextlib.ExitStack() as es:
        def sb(name, shape, dt=F32):
            return es.enter_context(nc.sbuf_tensor("s3_" + name, shape, dt))

        def ps(name, shape, dt=F32):
            return es.enter_context(nc.psum_tensor("s3_" + name, shape, dt))
        xT = sb("xT", [64, NTOK + 16], BF16)
        w1 = sb("w1", [64, 32, 256], BF16)
        posT = sb("posT", [64, 32], BF16)
        b1 = sb("b1", [128, 2])
        b1e = sb("b1e", [128, 2])
        w2 = sb("w2", [128, 2, 64], BF16)
        b2 = sb("b2", [128, 64])
        gk = sb("gk", [128, 64])
        hx = [sb("hx%d" % i, [128, 512]) for i in range(2)]
        hu = [sb("hu%d" % i, [128, 512]) for i in range(2)]
        hidT = sb("hidT", [128, 2, CT * 128], BF16)
        o = [sb("o%d" % i, [128, 64]) for i in range(2)]
        junk = sb("junk", [128, 64])
        st = [sb("st%d" % i, [128, 4]) for i in range(2)]
        kn = [sb("kn%d" % i, [128, 64], BF16) for i in range(2)]
        kct = [sb("kct%d" % i, [65, 128], BF16) for i in range(2)]
        vct = [sb("vct%d" % i, [128, 65], BF16) for i in range(2)]
        pb = ps("pb", [128, 512])
        ph = [ps("ph%d" % i, [128, 512]) for i in range(2)]
        po = [ps("po%d" % i, [128, 512]) for i in range(2)]
        pt = [ps("pt%d" % i, [128, 1024], BF16) for i in range(2)]
        P.dma("sp", gk[:], bass.AP(k_gain.tensor, k_gain.offset, [[0, 128], [1, 64]]), writes=["gk"])
        P.op("dve", lambda e: e.memset(hidT[:], 0.0), writes=["hidT"])
        for i in range(2):
            P.op("pool", lambda e, i=i: e.memset(vct[i][:], 1.0), writes=[("vct", i)])
        it = 0
        for kind in range(2):
            for g in range(2):
                r0 = kind * 128 + g * 64
                P.dma("pool", xT[:, :], projT[r0:r0 + 64, 16:16 + NTOK + 16] if False else projT[r0:r0 + 64, 0:NTOK + 16], writes=["xT"])
                if g == 0:
                    P.dma("pool", w1[:], cw1[kind].rearrange("(l d) h -> d l h", d=64), writes=["w1"])
                    for l in range(32):
                        pass
                    P.dma("pool", posT[:], cpos[kind].rearrange("l d -> d l"), writes=["posT"])
                    P.dma("sp", b1[:], cb1[kind].rearrange("(c p) -> p c", p=128), writes=["b1"])
                    P.dma("pool", w2[:], cw2[kind].rearrange("(c p) d -> p c d", p=128), writes=["w2"])
                    P.dma("sp", b2[:], bass.AP(cb2[kind].tensor, cb2[kind].offset, [[0, 128], [1, 64]]), writes=["b2"])
                    for c in range(2):
                        for l in range(32):
                            P.op("pe", lambda e, c=c, l=l: e.matmul(pb[:, c:c + 1], lhsT=w1[:, l, c * 128:(c + 1) * 128], rhs=posT[:, l:l + 1], start=(l == 0), stop=(l == 31)), reads=["w1", "posT"], writes=["pb"])
                    P.op("dve", lambda e: e.tensor_tensor(out=b1e[:], in0=pb[:, 0:2], in1=b1[:], op=ALU.add), reads=["pb", "b1"], writes=["b1e"])
                xv = xT[:, :].rearrange("p (n r) -> p n r", r=16)
                for n0 in range(0, NCB, 512):
                    nn = min(512, NCB - n0)
                    for c in range(2):
                        q = it % 2
                        it += 1
                        for l in range(32):
                            if l < 16:
                                rhs = xv[:, 1 + n0:1 + n0 + nn, l]
                            else:
                                rhs = xv[:, 2 + n0:2 + n0 + nn, l - 16]
                            P.op("pe", lambda e, c=c, l=l, rhs=rhs, q=q: e.matmul(ph[q][:, 0:nn], lhsT=w1[:, l, c * 128:(c + 1) * 128], rhs=rhs, start=(l == 0), stop=(l == 31)), reads=["w1", "xT"], writes=[("ph", q)])
                        P.op("dve", lambda e, c=c, q=q: e.tensor_scalar(out=hx[q][:, 0:nn], in0=ph[q][:, 0:nn], scalar1=b1e[:, c:c + 1], scalar2=None, op0=ALU.add), reads=[("ph", q), "b1e"], writes=[("hx", q)])
                        P.op("dve", lambda e, q=q: e.tensor_tensor(out=hu[q][:, 0:nn], in0=hx[q][:, 0:nn], in1=hx[q][:, 0:nn], op=ALU.mult), reads=[("hx", q)], writes=[("hu", q)])
                        P.op("dve", lambda e, q=q: e.tensor_scalar(out=hu[q][:, 0:nn], in0=hu[q][:, 0:nn], scalar1=0.044715, scalar2=1.0, op0=ALU.mult, op1=ALU.add), reads=[("hu", q)], writes=[("hu", q)])
                        P.op("dve", lambda e, q=q: e.tensor_tensor(out=hu[q][:, 0:nn], in0=hu[q][:, 0:nn], in1=hx[q][:, 0:nn], op=ALU.mult), reads=[("hu", q), ("hx", q)], writes=[("hu", q)])
                        P.op("act", lambda e, q=q: e.activation(out=hu[q][:, 0:nn], in_=hu[q][:, 0:nn], func=AF.Exp, scale=-1.5957691216057308), reads=[("hu", q)], writes=[("hu", q)])
                        P.op("dve", lambda e, q=q: e.tensor_scalar(out=hu[q][:, 0:nn], in0=hu[q][:, 0:nn], scalar1=1.0, scalar2=None, op0=ALU.add), reads=[("hu", q)], writes=[("hu", q)])
                        P.op("dve", lambda e, q=q: e.reciprocal(out=hu[q][:, 0:nn], in_=hu[q][:, 0:nn]), reads=[("hu", q)], writes=[("hu", q)])
                        P.op("dve", lambda e, q=q, c=c, n0=n0: e.tensor_tensor(out=hidT[:, c, n0:n0 + nn], in0=hx[q][:, 0:nn], in1=hu[q][:, 0:nn], op=ALU.mult), reads=[("hu", q), ("hx", q)], writes=["hidT"])
                for ct in range(CT):
                    q = ct % 2
                    for c in range(2):
                        P.op("pe", lambda e, c=c, ct=ct, q=q: e.matmul(po[q][:, 0:64], lhsT=hidT[:, c, ct * 128:(ct + 1) * 128], rhs=w2[:, c, :], start=(c == 0), stop=(c == 1)), reads=["hidT", "w2"], writes=[("po", q)])
                    P.op("dve", lambda e, q=q: e.tensor_tensor(out=o[q][:], in0=po[q][:, 0:64], in1=b2[:], op=ALU.add), reads=[("po", q), "b2"], writes=[("o", q)])
                    if kind == 0:
                        P.op("act", lambda e, q=q: e.activation(out=junk[:], in_=o[q][:], func=AF.Square, accum_out=st[q][:, 0:1]), reads=[("o", q)], writes=["junk", ("st0", q)])
                        P.op("act", lambda e, q=q: e.activation(out=st[q][:, 1:2], in_=st[q][:, 0:1], func=AF.Ln, scale=1.0 / 64, bias=1e-6), reads=[("st0", q)], writes=[("st1", q)])
                        P.op("act", lambda e, q=q: e.activation(out=st[q][:, 2:3], in_=st[q][:, 1:2], func=AF.Exp, scale=-0.5), reads=[("st1", q)], writes=[("st2", q)])
                        P.op("dve", lambda e, q=q: e.scalar_tensor_tensor(out=kn[q][:], in0=o[q][:], scalar=st[q][:, 2:3], in1=gk[:], op0=ALU.mult, op1=ALU.mult), reads=[("o", q), ("st2", q), "gk"], writes=[("kn", q)])
                        P.op("pe", lambda e, q=q: e.transpose(out=pt[q][0:64, 0:128], in_=kn[q][:], identity=ident[:]), reads=[("kn", q), "ident"], writes=[("pt", q)])
                        P.op("act", lambda e, q=q: e.activation(out=kct[q][0:64, :], in_=pt[q][0:64, 0:128], func=AF.Copy), reads=[("pt", q)], writes=[("kct", q)])
                        P.dma("pool", kct[q][64:65, :], cvalid[0:1, ct * 128:(ct + 1) * 128], reads=[("kct", q)], writes=[("kct", q)])
                        P.dma("sp", kc_scr[g, :, ct * 128:(ct + 1) * 128], kct[q][:], reads=[("kct", q)], writes=[("kc_scr", g, ct)])
                    else:
                        P.op("dve", lambda e, q=q: e.tensor_copy(out=vct[q][:, 0:64], in_=o[q][:]), reads=[("o", q)], writes=[("vct", q)])
                        P.dma("sp", vc_scr[g, ct * 128:(ct + 1) * 128, :], vct[q][:], reads=[("vct", q)], writes=[("vc_scr", g, ct)])
        P.flush()


def _brow(ap, n):
    return bass.AP(ap.tensor, ap.offset, [[0, 128], [1, n]])


def stage4(nc, P, NT, proj, kc_scr, vc_scr, q_gain, k_gain, kvalid_row, f0row, rel31, Bg, Bcg, Mc, causal_in, w4_in,
           c2s_in, onehot_in, vmB_in, amB_in, fzB_in, ya, ident, identf, bc_scr):
    NJ = NT // 2
    NTOK = NT * 128
    NCB = 8 * NT - 1
    CT = (NCB + 127) // 128
    with contextlib.ExitStack() as es:
        def sb(name, shape, dt=F32):
            return es.enter_context(nc.sbuf_tensor("s4_" + name, shape, dt))

        def ps(name, shape, dt=F32):
            return es.enter_context(nc.psum_tensor("s4_" + name, shape, dt))
        KsT = sb("KsT", [65, 2, NTOK], BF16)
        KwT = sb("KwT", [65, 2, NTOK], BF16)
        Vs = sb("Vs", [128, NT, 2, 65], BF16)
        Vw = sb("Vw", [128, NT, 2, 65], BF16)
        KcT = sb("KcT", [65, 2, CT * 128], BF16)
        Vc = sb("Vc", [128, CT, 2, 65], BF16)
        c2s = sb("c2s", [128, CT, 128], BF16)
        onehot = sb("onehot", [128, NTOK], BF16)
        B01 = sb("B01", [128, 2, 8, 128])
        W4 = sb("W4", [128, 128])
        causal = sb("causal", [128, 128])
        ch = sb("ch", [128, 8])
        gq = sb("gq", [128, 64])
        gks = sb("gks", [128, 64])
        gkw = sb("gkw", [128, 64])
        f0 = sb("f0", [128, 128])
        vmB = sb("vmB", [128, 256])
        amB = sb("amB", [128, 256])
        fzB = sb("fzB", [128, 256])
        kvt = [sb("kvt%d" % i, [128, 512]) for i in range(2)]
        sqk = [sb("sqk%d" % i, [128, 256]) for i in range(2)]
        tmpk = [sb("tmpk%d" % i, [128, 256]) for i in range(2)]
        ssk = [sb("ssk%d" % i, [128, 8]) for i in range(2)]
        kb = [sb("kb%d" % i, [128, 256], BF16) for i in range(2)]
        qt = sb("qt", [128, 512])
        sqq = sb("sqq", [128, 512])
        tmpq = sb("tmpq", [128, 512])
        ssq = sb("ssq", [128, 16])
        qb_ = sb("qb", [128, 512], BF16)
        QT = [sb("QT%d" % i, [65, 8, 128], BF16) for i in range(2)]
        gsig = [sb("gsig%d" % i, [128, 24]) for i in range(2)]
        bct = sb("bct", [128, 8, 128])
        bctH = sb("bctH", [128, 8, 128], BF16)
        bctL = sb("bctL", [128, 8, 128], BF16)
        B01H = sb("B01H", [128, 2, 8, 128], BF16)
        B01L = sb("B01L", [128, 2, 8, 128], BF16)
        W4H = sb("W4H", [128, 4, 128], BF16)
        mct = sb("mct", [128, 128])
        sfp = [sb("sfp%d" % i, [128, 512]) for i in range(2)]
        PT = [sb("PT%d" % i, [128, 512], BF16) for i in range(4)]
        PTc = [sb("PTc%d" % i, [128, 512], BF16) for i in range(2)]
        rcM = sb("rcM", [128, 16])
        rcC = sb("rcC", [128, 16])
        imp = sb("imp", [128, 128])
        sc2 = sb("sc2", [128, 128])
        m8 = sb("m8", [128, 16])
        mneg = sb("mneg", [128, 128], BF16)
        mnegT = [sb("mnegT%d" % i, [128, 4, 128], BF16) for i in range(2)]
        yat = [sb("yat%d" % i, [128, 512]) for i in range(2)]
        yab = [sb("yab%d" % i, [128, 512], BF16) for i in range(2)]
        oTsM = sb("oTsM", [65, 512])
        oTsC = sb("oTsC", [65, 512])
        iTs = sb("iTs", [128, 512])
        psc = [ps("psc%d" % i, [128, 512]) for i in range(3)]
        povT = ps("povT", [128, 512])
        pov = ps("pov", [128, 512])
        povT2 = ps("povT2", [128, 512])
        pimT = ps("pimT", [128, 512])
        ptr = ps("ptr", [128, 1024], BF16)
        pch = ptr.bitcast(F32)

        P.dma("pool", onehot[:], onehot_in[:, :], writes=["onehot"])
        P.dma("pool", c2s[:], c2s_in.rearrange("(c p) b -> p c b", p=128), writes=["c2s"])
        P.dma("sp", W4[:], w4_in[:, :], writes=["W4"])
        P.dma("sp", causal[:], causal_in[:, :], writes=["causal"])
        P.dma("sp", ch[:], _brow(rel31, 8), writes=["ch"])
        P.dma("sp", gq[:], _brow(q_gain, 64), writes=["gq"])
        P.dma("sp", gks[:], _brow(k_gain[1], 64), writes=["gks"])
        P.dma("sp", gkw[:], _brow(k_gain[2], 64), writes=["gkw"])
        P.dma("sp", f0[:], _brow(f0row, 128), writes=["f0"])
        P.dma("sp", vmB[:], vmB_in[:, :], writes=["vmB"])
        P.dma("sp", amB[:], amB_in[:, :], writes=["amB"])
        P.dma("sp", fzB[:], fzB_in[:, :], writes=["fzB"])
        for d in range(2):
            P.dma("sp", B01[:, d], Bg[d], writes=[("B01", d)])
            P.op("dve", lambda e, d=d: e.tensor_tensor(out=B01[:, d], in0=B01[:, d], in1=_bc(ch[:].rearrange("p (h o) -> p h o", o=1), [128, 8, 128]), op=ALU.subtract), reads=[("B01", d), "ch"], writes=[("B01", d)])
        P.op("dve", lambda e: e.tensor_tensor(out=B01[:, 0], in0=B01[:, 0], in1=_bc(causal[:].rearrange("p (o q) -> p o q", o=1), [128, 8, 128]), op=ALU.add), reads=[("B01", 0), "causal"], writes=[("B01", 0)])
        for d in range(2):
            P.op("dve", lambda e, d=d: e.tensor_copy(out=B01H[:, d], in_=B01[:, d]), reads=[("B01", d)], writes=[("B01H", d)])
            P.op("dve", lambda e, d=d: e.tensor_tensor(out=B01[:, d], in0=B01[:, d], in1=B01H[:, d], op=ALU.subtract), reads=[("B01", d), ("B01H", d)], writes=[("B01", d)])
            P.op("dve", lambda e, d=d: e.tensor_copy(out=B01L[:, d], in_=B01[:, d]), reads=[("B01", d)], writes=[("B01L", d)])
        P.op("dve", lambda e: e.tensor_copy(out=W4H[:], in_=_bc(W4[:].rearrange("p (o q) -> p o q", o=1), [128, 4, 128])), reads=["W4"], writes=["W4H"])
        for v in range(8):
            P.dma("sp", bct[:], Bcg[v], writes=["bct"])
            P.dma("sp", mct[:], Mc[v], writes=["mct"])
            P.op("dve", lambda e: e.tensor_tensor(out=bct[:], in0=bct[:], in1=_bc(ch[:].rearrange("p (h o) -> p h o", o=1), [128, 8, 128]), op=ALU.subtract), reads=["bct", "ch"], writes=["bct"])
            P.op("dve", lambda e: e.tensor_tensor(out=bct[:], in0=bct[:], in1=_bc(mct[:].rearrange("p (o q) -> p o q", o=1), [128, 8, 128]), op=ALU.add), reads=["bct", "mct"], writes=["bct"])
            P.op("dve", lambda e: e.tensor_copy(out=bctH[:], in_=bct[:]), reads=["bct"], writes=["bctH"])
            P.op("dve", lambda e: e.tensor_tensor(out=bct[:], in0=bct[:], in1=bctH[:], op=ALU.subtract), reads=["bct", "bctH"], writes=["bct"])
            P.op("dve", lambda e: e.tensor_copy(out=bctL[:], in_=bct[:]), reads=["bct"], writes=["bctL"])
            P.dma("sp", bc_scr[v, 0], bctH[:].rearrange("p h q -> p (h q)"), reads=["bctH"], writes=[("bc_scr", v, 0)])
            P.dma("sp", bc_scr[v, 1], bctL[:].rearrange("p h q -> p (h q)"), reads=["bctL"], writes=[("bc_scr", v, 1)])
        for g in range(2):
            P.dma("pool", KsT[64:65, g, :], kvalid_row[0:1, :], writes=[("KsTv", g)])
            P.dma("pool", KwT[64:65, g, :], kvalid_row[0:1, :], writes=[("KwTv", g)])
            P.dma("sp", KcT[:, g, :], kc_scr[g], writes=[("KcT", g)])
            P.dma("sp", Vc[:, :, g, :], vc_scr[g].rearrange("(c p) e -> p c e", p=128), writes=[("Vc", g)])
        P.op("pool", lambda e: e.memset(Vs[:], 1.0), writes=["Vs_init"])
        P.op("pool", lambda e: e.memset(Vw[:], 1.0), writes=["Vw_init"])
        for i in range(2):
            P.op("pool", lambda e, i=i: e.memset(QT[i][:], 1.0), writes=[("QT", i)])

        v3 = lambda ap, b=64: ap.rearrange("p (a b) -> p a b", b=b)

        def kvprep_levels(t):
            q = t % 2
            L = [[] for _ in range(7)]
            L[0].append(lambda: P.dma("sp", kvt[q][:], proj[t * 128:(t + 1) * 128, 768:1280], writes=[("kvt", q)]))
            for ci, c0 in enumerate((0, 256)):
                L[1].append(lambda ci=ci, c0=c0: P.op("dve", lambda e: e.tensor_tensor(out=v3(sqk[q][:, ci * 128:(ci + 1) * 128]), in0=v3(kvt[q][:, c0:c0 + 128]), in1=v3(kvt[q][:, c0:c0 + 128]), op=ALU.mult), reads=[("kvt", q)], writes=[("sqk", q, ci)]))
            L[1].append(lambda: P.op("dve", lambda e: e.tensor_reduce(out=ssk[q][:, 0:4], in_=v3(sqk[q][:]), axis=AX.X, op=ALU.add), reads=[("sqk", q, 0), ("sqk", q, 1)], writes=[("ssk0", q)]))
            L[1].append(lambda: P.op("pool", lambda e: e.tensor_copy(out=Vs[:, t, :, 0:64], in_=v3(kvt[q][:, 128:256])), reads=[("kvt", q), "Vs_init"], writes=[("Vs", t)]))
            L[1].append(lambda: P.op("pool", lambda e: e.tensor_copy(out=Vw[:, t, :, 0:64], in_=v3(kvt[q][:, 384:512])), reads=[("kvt", q), "Vw_init"], writes=[("Vw", t)]))
            L[2].append(lambda: P.op("act", lambda e: e.activation(out=ssk[q][:, 0:4], in_=ssk[q][:, 0:4], func=AF.Ln, scale=1.0 / 64, bias=1e-6), reads=[("ssk0", q)], writes=[("ssk0", q)]))
            L[2].append(lambda: P.op("act", lambda e: e.activation(out=ssk[q][:, 4:8], in_=ssk[q][:, 0:4], func=AF.Exp, scale=-0.5), reads=[("ssk0", q)], writes=[("ssk4", q)]))
            for ci, (c0, gt, gk_) in enumerate(((0, gks, "gks"), (256, gkw, "gkw"))):
                L[3].append(lambda ci=ci, c0=c0: P.op("dve", lambda e: e.tensor_tensor(out=v3(tmpk[q][:, ci * 128:(ci + 1) * 128]), in0=v3(kvt[q][:, c0:c0 + 128]), in1=_bc(ssk[q][:, 4 + 2 * ci:6 + 2 * ci].rearrange("p (a o) -> p a o", o=1), [128, 2, 64]), op=ALU.mult), reads=[("kvt", q), ("ssk4", q)], writes=[("tmpk", q, ci)]))
                L[3].append(lambda ci=ci, gt=gt, gk_=gk_: P.op("dve", lambda e: e.tensor_tensor(out=v3(kb[q][:, ci * 128:(ci + 1) * 128]), in0=v3(tmpk[q][:, ci * 128:(ci + 1) * 128]), in1=_bc(gt[:].rearrange("p (o b) -> p o b", o=1), [128, 2, 64]), op=ALU.mult), reads=[("tmpk", q, ci), gk_], writes=[("kb", q, ci)]))
            for a4 in range(4):
                L[4].append(lambda a4=a4: P.op("pe", lambda e: e.transpose(out=ptr[0:64, q * 512 + a4 * 128:q * 512 + (a4 + 1) * 128], in_=kb[q][:, a4 * 64:(a4 + 1) * 64], identity=ident[:]), reads=[("kb", q, 0), ("kb", q, 1), "ident"], writes=["ptr"]))
            L[5].append(lambda: P.op("act", lambda e: e.activation(out=KsT[0:64, :, t * 128:(t + 1) * 128], in_=ptr[0:64, q * 512:q * 512 + 256].rearrange("p (g n) -> p g n", n=128), func=AF.Copy), reads=["ptr"], writes=[("KsT", t)]))
            L[5].append(lambda: P.op("act", lambda e: e.activation(out=KwT[0:64, :, t * 128:(t + 1) * 128], in_=ptr[0:64, q * 512 + 256:q * 512 + 512].rearrange("p (g n) -> p g n", n=128), func=AF.Copy), reads=["ptr"], writes=[("KwT", t)]))
            return L

        def qprep_levels(Q, qb):
            L = [[] for _ in range(7)]
            v = ((Q % 16) - 1) // 2
            L[0].append(lambda: P.dma("sp", qt[:], proj[Q * 128:(Q + 1) * 128, 0:512], writes=["qt"]))
            L[0].append(lambda: P.dma("sp", gsig[qb][:], proj[Q * 128:(Q + 1) * 128, 1280:1304], writes=[("gsig", qb)]))
            L[0].append(lambda: P.dma("sp", bctH[:].rearrange("p h q -> p (h q)"), bc_scr[v, 0], reads=[("bc_scr", v, 0)], writes=["bctH"]))
            L[0].append(lambda: P.dma("sp", bctL[:].rearrange("p h q -> p (h q)"), bc_scr[v, 1], reads=[("bc_scr", v, 1)], writes=["bctL"]))
            L[1].append(lambda: P.op("dve", lambda e: e.tensor_tensor(out=v3(sqq[:]), in0=v3(qt[:]), in1=v3(qt[:]), op=ALU.mult), reads=["qt"], writes=["sqq"]))
            L[1].append(lambda: P.op("dve", lambda e: e.tensor_reduce(out=ssq[:, 0:8], in_=v3(sqq[:]), axis=AX.X, op=ALU.add), reads=["sqq"], writes=["ssq0"]))
            L[2].append(lambda: P.op("act", lambda e: e.activation(out=ssq[:, 0:8], in_=ssq[:, 0:8], func=AF.Ln, scale=1.0 / 64, bias=1e-6), reads=["ssq0"], writes=["ssq0"]))
            L[2].append(lambda: P.op("act", lambda e: e.activation(out=ssq[:, 8:16], in_=ssq[:, 0:8], func=AF.Exp, scale=-0.5), reads=["ssq0"], writes=["ssq8"]))
            L[2].append(lambda: P.op("act", lambda e: e.activation(out=gsig[qb][:], in_=gsig[qb][:], func=AF.Exp, scale=-1.0), reads=[("gsig", qb)], writes=[("gsig", qb)]))
            L[2].append(lambda: P.op("act", lambda e: e.activation(out=gsig[qb][:], in_=gsig[qb][:], func=AF.Ln, bias=1.0), reads=[("gsig", qb)], writes=[("gsig", qb)]))
            L[2].append(lambda: P.op("act", lambda e: e.activation(out=gsig[qb][:], in_=gsig[qb][:], func=AF.Exp, scale=-1.0), reads=[("gsig", qb)], writes=[("gsig", qb)]))
            L[3].append(lambda: P.op("dve", lambda e: e.tensor_tensor(out=v3(tmpq[:]), in0=v3(qt[:]), in1=_bc(ssq[:, 8:16].rearrange("p (a o) -> p a o", o=1), [128, 8, 64]), op=ALU.mult), reads=["qt", "ssq8"], writes=["tmpq"]))
            L[3].append(lambda: P.op("dve", lambda e: e.scalar_tensor_tensor(out=v3(qb_[:]), in0=v3(tmpq[:]), scalar=0.125, in1=_bc(gq[:].rearrange("p (o b) -> p o b", o=1), [128, 8, 64]), op0=ALU.mult, op1=ALU.mult), reads=["tmpq", "gq"], writes=["qb"]))
            for h in range(8):
                L[4].append(lambda h=h: P.op("pe", lambda e: e.transpose(out=ptr[0:64, h * 128:(h + 1) * 128], in_=qb_[:, h * 64:(h + 1) * 64], identity=ident[:]), reads=["qb", "ident"], writes=["ptr"]))
            L[5].append(lambda: P.op("act", lambda e: e.activation(out=QT[qb][0:64, :, :], in_=ptr[0:64, :].rearrange("p (h n) -> p h n", n=128), func=AF.Copy), reads=["ptr"], writes=[("QT", qb)]))
            return L

        pti = [0]
        sci = [0]

        def qkm_op(QTg, qtkey, kt, kT_of, use_mask, mT, pq=None, bias=None):
            if pq is None:
                pq = sci[0]
            pscb = psc[pq]
            ksrc, kkeys = kT_of(kt)
            extra = []
            if use_mask:
                extra.append((onehot[:, kt * 128:(kt + 1) * 128], mT[0][:].rearrange("p h q -> p (h q)"), ["onehot", mT[1]]))
            if bias is not None:
                for bap in bias[0]:
                    extra.append((ident[:], bap.rearrange("p h q -> p (h q)"), ["ident"] + bias[1]))
            P.op("pe", lambda e: e.matmul(pscb[:, :], lhsT=ksrc, rhs=QTg, start=True, stop=(len(extra) == 0)), reads=kkeys + [qtkey], writes=[("psc", pq)])
            for xi, (l_, r_, k_) in enumerate(extra):
                P.op("pe", lambda e, l_=l_, r_=r_, xi=xi: e.matmul(pscb[:, :], lhsT=l_, rhs=r_, start=False, stop=(xi == len(extra) - 1)), reads=k_, writes=[("psc", pq)])
            return pq

        def exp_op(pq, has_bias=False):
            pi_ = pti[0] % 4
            pti[0] += 1
            P.op("act", lambda e: e.activation(out=PT[pi_][:], in_=psc[pq][:, :], func=AF.Exp), reads=[("psc", pq)], writes=[("PT", pi_)])
            return pi_

        def pv_op(pi_, vsrc, vkeys, acc, acckey, first, last):
            P.op("pe", lambda e: e.matmul(acc[0:65, :], lhsT=vsrc, rhs=PT[pi_][:], start=first, stop=last), reads=[("PT", pi_)] + vkeys, writes=[acckey])

        def norm_ops(po, pok, rc, rckey, gs, gskey, g, br, first_branch, yt, ytk):
            pov3 = po[:, 0:260].rearrange("p (h e) -> p h e", e=65)
            P.op("dve", lambda e: e.tensor_scalar(out=rc[:, 0:4].rearrange("p (h o) -> p h o", o=1), in0=pov3[:, :, 64:65], scalar1=1e-30, scalar2=None, op0=ALU.max), reads=[pok], writes=[rckey + "0"])
            P.op("dve", lambda e: e.reciprocal(out=rc[:, 4:8], in_=rc[:, 0:4]), reads=[rckey + "0"], writes=[rckey + "4"])
            gv = gs[:, g * 12:(g + 1) * 12].rearrange("p (h b) -> p h b", b=3)[:, :, br:br + 1]
            P.op("dve", lambda e: e.tensor_tensor(out=rc[:, 8:12].rearrange("p (h o) -> p h o", o=1), in0=rc[:, 4:8].rearrange("p (h o) -> p h o", o=1), in1=gv, op=ALU.mult), reads=[rckey + "4", gskey], writes=[rckey + "8"])
            for h in range(4):
                dst = yt[:, (g * 4 + h) * 64:(g * 4 + h + 1) * 64]
                if first_branch:
                    P.op("dve", lambda e, h=h, dst=dst: e.tensor_scalar(out=dst, in0=pov3[:, h, 0:64], scalar1=rc[:, 8 + h:9 + h], scalar2=None, op0=ALU.mult), reads=[pok, rckey + "8"], writes=[(ytk, g, h)])
                else:
                    P.op("dve", lambda e, h=h, dst=dst: e.scalar_tensor_tensor(out=dst, in0=pov3[:, h, 0:64], scalar=rc[:, 8 + h:9 + h], in1=dst, op0=ALU.mult, op1=ALU.add), reads=[pok, rckey + "8", (ytk, g, h)], writes=[(ytk, g, h)])

        def chain_levels(Q, g):
            j = Q // 2
            qb = j % 2
            QTg = QT[qb][:, 4 * g:4 * g + 4, :].rearrange("p h q -> p (h q)")
            ctb = (8 * Q + 6) // 128
            n = ctb + 1
            L = []
            st = {}
            for idx in range(n):
                kt = idx
                hb = (kt == ctb)
                def lev_a(idx=idx, kt=kt, hb=hb):
                    qkm_op(QTg, ("QT", qb), kt, lambda kt_: (KcT[:, g, kt_ * 128:(kt_ + 1) * 128], [("KcT", g)]), False, None, pq=None,
                           bias=(([bctH[:, 4 * g:4 * g + 4, :], bctL[:, 4 * g:4 * g + 4, :]], ["bctH", "bctL"]) if hb else None))
                    pqc = sci[0]
                    P.op("act", lambda e: e.activation(out=PTc[idx % 2][:], in_=psc[pqc][:, :], func=AF.Exp), reads=[("psc", pqc)], writes=[("PTc", idx % 2)])

                def lev_b(idx=idx, kt=kt):
                    P.op("pe", lambda e: e.matmul(povT2[0:65, :], lhsT=Vc[:, kt, g, :], rhs=PTc[idx % 2][:], start=(idx == 0), stop=(idx == n - 1)), reads=[("PTc", idx % 2), ("Vc", g)], writes=["povT2"])
                    P.op("pe", lambda e: e.matmul(pimT[:, :], lhsT=c2s[:, kt, :], rhs=PTc[idx % 2][:], start=(idx == 0), stop=(idx == n - 1)), reads=[("PTc", idx % 2), "c2s"], writes=["pimT"])
                L.append([lev_a])
                L.append([lev_b])
            L.append([lambda: P.op("dve", lambda e: e.tensor_copy(out=oTsC[0:65, :], in_=povT2[0:65, :]), reads=["povT2"], writes=["oTsC"]),
                      lambda: P.op("dve", lambda e: e.tensor_copy(out=iTs[:, :], in_=pimT[:, :]), reads=["pimT"], writes=["iTs"])])
            L.append([lambda h=h: P.op("pe", lambda e: e.transpose(out=pch[:, h * 65:(h + 1) * 65], in_=oTsC[0:65, h * 128:(h + 1) * 128], identity=identf[0:65, 0:65]), reads=["oTsC", "identf"], writes=["ptr"]) for h in range(4)])
            L.append([lambda: norm_ops(pch, "ptr", rcC, "rcC", gsig[qb], ("gsig", qb), g, 0, True, yat[qb], "yat%d" % qb)])
            L += [[] for _ in range(4)]
            L.append([lambda h=h: P.op("pe", lambda e: e.transpose(out=pch[:, h * 128:(h + 1) * 128], in_=iTs[:, h * 128:(h + 1) * 128], identity=identf[:, :]), reads=["iTs", "identf"], writes=["ptr"]) for h in range(4)])
            o0 = 128 - 2 * Q

            def topk():
                for h in range(4):
                    if h == 0:
                        P.op("dve", lambda e: e.tensor_scalar(out=imp[:], in0=pch[:, 0:128], scalar1=rcC[:, 4:5], scalar2=None, op0=ALU.mult), reads=["ptr", "rcC4"], writes=["imp"])
                    else:
                        P.op("dve", lambda e, h=h: e.scalar_tensor_tensor(out=imp[:], in0=pch[:, h * 128:(h + 1) * 128], scalar=rcC[:, 4 + h:5 + h], in1=imp[:], op0=ALU.mult, op1=ALU.add), reads=["ptr", "rcC4", "imp"], writes=["imp"])
                P.op("dve", lambda e: e.tensor_tensor(out=imp[:], in0=imp[:], in1=vmB[:, o0:o0 + 128], op=ALU.mult), reads=["imp", "vmB"], writes=["imp"])
                P.op("dve", lambda e: e.tensor_tensor(out=imp[:], in0=imp[:], in1=amB[:, o0:o0 + 128], op=ALU.add), reads=["imp", "amB"], writes=["imp"])
                P.op("dve", lambda e: e.tensor_tensor(out=imp[:], in0=imp[:], in1=fzB[:, o0:o0 + 128], op=ALU.max), reads=["imp", "fzB"], writes=["imp"])
                P.op("dve", lambda e: e.tensor_tensor(out=imp[:], in0=imp[:], in1=f0[:], op=ALU.max), reads=["imp", "f0"], writes=["imp"])
                P.op("dve", lambda e: e.max(out=m8[:, 0:8], in_=imp[:]), reads=["imp"], writes=["m8a"])
                P.op("dve", lambda e: e.match_replace(out=sc2[:], in_to_replace=m8[:, 0:8], in_values=imp[:], imm_value=-3.0), reads=["imp", "m8a"], writes=["sc2"])
                P.op("dve", lambda e: e.max(out=m8[:, 8:16], in_=sc2[:]), reads=["sc2"], writes=["m8b"])
                P.op("dve", lambda e: e.tensor_scalar(out=mneg[:], in0=imp[:], scalar1=m8[:, 15:16], scalar2=NEG, op0=ALU.is_lt, op1=ALU.mult), reads=["imp", "m8b"], writes=["mneg"])
            L.append([topk])
            L += [[] for _ in range(8)]
            mi = (2 * j + g) % 2
            L.append([lambda: P.op("pe", lambda e: e.transpose(out=ptr[:, 0:128], in_=mneg[:], identity=ident[:]), reads=["mneg", "ident"], writes=["ptr"])])
            L.append([lambda: P.op("act", lambda e: e.activation(out=mnegT[mi][:], in_=_bc(ptr[:, 0:128].rearrange("p (o q) -> p o q", o=1), [128, 4, 128]), func=AF.Copy), reads=["ptr"], writes=[("mnegT", mi)])])
            return L

        def zip_levels(*lists):
            n = max(len(l) for l in lists)
            out_ = []
            for i in range(n):
                lv = []
                for l in lists:
                    if i < len(l):
                        lv += l[i]
                out_.append(lv)
            return out_

        def spaced(L):
            gaps = {0: 1, 1: 2, 2: 1, 3: 2, 4: 1}
            out_ = []
            for i, lv in enumerate(L):
                out_.append(lv)
                out_ += [[] for _ in range(gaps.get(i, 0))]
            return out_

        def run_levels(levels):
            for lv in levels:
                for f in lv:
                    f()

        def main_unit(Q, g, filler, carry):
            j = Q // 2
            qb = j % 2
            mi = (2 * j + g) % 2
            QTg = QT[qb][:, 4 * g:4 * g + 4, :].rearrange("p h q -> p (h q)")
            sel_kts = list(range(Q + 1))
            win_kts = list(range(max(0, Q - 4), Q + 1))
            total_it = len(sel_kts) + len(win_kts)
            done_it = [0]
            fpos = [0]

            def pull():
                done_it[0] += 1
                rem_it = max(1, total_it - done_it[0] + 1 - 8)
                rem_f = len(filler) - fpos[0]
                k = -(-rem_f // rem_it) if rem_it > 0 else rem_f
                for _ in range(k):
                    if fpos[0] < len(filler):
                        for f in filler[fpos[0]]:
                            f()
                        fpos[0] += 1

            class Branch:
                def __init__(self, kts, kT_of, v_of, bias_of, use_mask, br):
                    self.kts, self.kT_of, self.v_of, self.bias_of, self.use_mask, self.br = kts, kT_of, v_of, bias_of, use_mask, br
                    self.slots = {}
                    self.started = False

                def qk(self, idx):
                    self.slots[idx] = qkm_op(QTg, ("QT", qb), self.kts[idx], self.kT_of, self.use_mask, (mnegT[mi], ("mnegT", mi)), pq=idx % 3, bias=self.bias_of(self.kts[idx]))

                def start(self):
                    if not self.started:
                        self.qk(0)
                        if len(self.kts) > 1:
                            self.qk(1)
                        self.started = True

                def loop(self, after=None):
                    n = len(self.kts)
                    self.start()
                    for idx in range(n):
                        if idx + 2 < n:
                            self.qk(idx + 2)
                        sci[0] = idx % 3
                        pi_ = exp_op(self.slots[idx])
                        vsrc, vkeys = self.v_of(self.kts[idx])
                        pv_op(pi_, vsrc, vkeys, povT, "povT", idx == 0, idx == n - 1)
                        if after is not None and (idx == after[0] or (idx == n - 1 and after[0] >= n)):
                            for f in after[1]:
                                f()
                        pull()

                def epi_copy(self):
                    P.op("dve", lambda e: e.tensor_copy(out=oTsM[0:65, :], in_=povT[0:65, :]), reads=["povT"], writes=["oTsM"])

                def epi_rest(self):
                    for h in range(4):
                        P.op("pe", lambda e, h=h: e.transpose(out=pov[:, h * 65:(h + 1) * 65], in_=oTsM[0:65, h * 128:(h + 1) * 128], identity=identf[0:65, 0:65]), reads=["oTsM", "identf"], writes=["pov"])
                    norm_ops(pov, "pov", rcM, "rcM", gsig[qb], ("gsig", qb), g, self.br, False, yat[qb], "yat%d" % qb)

            def sbias(kt):
                dl = Q - kt
                if dl <= 1:
                    return ([B01H[:, dl, 4 * g:4 * g + 4, :], B01L[:, dl, 4 * g:4 * g + 4, :]], [("B01H", dl), ("B01L", dl)])
                return None

            def wbias(kt):
                dl = Q - kt
                if dl <= 1:
                    return sbias(kt)
                if dl == 4:
                    return ([W4H[:]], ["W4H"])
                return None
            selB = Branch(sel_kts,
                          lambda kt: (KsT[:, g, kt * 128:(kt + 1) * 128], [("KsT", kt), ("KsTv", g)]),
                          lambda kt: (Vs[:, kt, g, :], [("Vs", kt)]), sbias, (2 * Q + 2 > 16), 1)
            winB = Branch(win_kts,
                          lambda kt: (KwT[:, g, kt * 128:(kt + 1) * 128], [("KwT", kt), ("KwTv", g)]),
                          lambda kt: (Vw[:, kt, g, :], [("Vw", kt)]), wbias, False, 2)
            selB.loop(after=(1, carry))
            winB.start()
            selB.epi_copy()
            winB.loop(after=(1, [selB.epi_rest]))
            while fpos[0] < len(filler):
                for f in filler[fpos[0]]:
                    f()
                fpos[0] += 1
            winB.epi_copy()

            def finalize():
                if g == 1:
                    P.op("dve", lambda e: e.tensor_copy(out=yab[qb][:], in_=yat[qb][:]), reads=[("yat%d" % qb, gg, h) for gg in range(2) for h in range(4)], writes=[("yab", qb)])
                    P.dma("sp", ya[j * 128:(j + 1) * 128, :], yab[qb][:], reads=[("yab", qb)], writes=[("ya", j)])
            return [winB.epi_rest, finalize]

        units = [(2 * j + 1, g) for j in range(NJ) for g in range(2)]
        run_levels(zip_levels(kvprep_levels(0), kvprep_levels(1)))
        run_levels(qprep_levels(1, 0))
        run_levels(chain_levels(1, 0))
        carry = []
        for ui, (Q, g) in enumerate(units):
            filler = []
            if ui + 1 < len(units):
                nQ, ng = units[ui + 1]
                if nQ != Q:
                    jn = nQ // 2
                    filler += zip_levels(spaced(kvprep_levels(2 * jn)), spaced(kvprep_levels(2 * jn + 1)),
                                         [[] for _ in range(4)] + spaced(qprep_levels(nQ, jn % 2)))
                filler += chain_levels(nQ, ng)
            carry = main_unit(Q, g, filler, carry)
        for f in carry:
            f()
        P.flush()


def _t5_bucket_np(dist):
    n = np.maximum(dist, 0)
    nf = np.maximum(n, 1).astype(np.float32)
    large = 16 + (np.log(nf / np.float32(16)) / np.float32(np.log(8.0)) * np.float32(16)).astype(np.int32)
    return np.where(n < 16, n, np.minimum(large, 31)).astype(np.int64)


def make_consts(NT):
    NTOK = NT * 128
    NCB = 8 * NT - 1
    CT = (NCB + 127) // 128
    c = {}
    c["c_ident"] = np.eye(128, dtype=np.float32)
    c["c_tri"] = np.triu(np.ones((128, 128), np.float32))
    k = np.arange(128)[:, None]
    q = np.arange(128)[None, :]
    c["c_causal"] = np.where(k > q, NEG, 0.0).astype(np.float32)
    c["c_w4"] = np.where(k <= q, NEG, 0.0).astype(np.float32)
    n = np.arange(CT * 128)[:, None]
    blk = np.arange(128)[None, :]
    c2s = ((16 * n < 64 * blk + 64) & (16 * n + 32 > 64 * blk) & (n < NCB)).astype(np.float32)
    c["c_c2s"] = c2s
    key = np.arange(NTOK)[None, :]
    c["c_onehot"] = (key // 64 == np.arange(128)[:, None]).astype(np.float32)
    qq = np.arange(128)[:, None]
    rb = np.arange(256)[None, :] - 128
    cur = (qq >= 64).astype(np.int64)
    vm = (rb <= cur).astype(np.float32)
    c["c_vmB"] = vm
    c["c_amB"] = vm - 1.0
    c["c_fzB"] = np.where(rb == cur, 10003.0, np.where(rb == cur - 1, 10002.0, -1.0)).astype(np.float32)
    idx = {}
    idx["Bg"] = np.stack([_t5_bucket_np(128 * d + q - k) for d in range(2)])
    r = np.arange(128)[:, None]
    dists = np.stack([16 * 8 * (2 * v + 1) + q - 16 * r - 31 for v in range(8)])
    idx["Bcg"] = _t5_bucket_np(dists)
    c["c_Mc"] = np.where(dists < 0, NEG, 0.0).astype(np.float32)
    return c, idx


def core_inputs(inputs, b, par, NT, consts, idx):
    NTOK = NT * 128
    NCB = 8 * NT - 1
    CT = (NCB + 127) // 128
    x = inputs["x"][b]
    m = dict(consts)
    if par == 1:
        m["x_loc"] = np.ascontiguousarray(x[:NTOK])
    else:
        m["x_loc"] = np.concatenate([np.zeros((128, D), np.float32), x[:NTOK - 128]], axis=0)
    kval = np.zeros(NTOK, np.float32)
    cval = np.zeros(CT * 128, np.float32)
    cval[NCB:] = NEG
    f0 = np.full(128, -1.0, np.float32)
    if par == 0:
        kval[:128] = NEG
        cval[:8] = NEG
        f0[2] = 10001.0
    else:
        f0[0] = 10001.0
    m["kvalid_row"] = kval[None, :]
    m["kvalid_tm"] = np.ascontiguousarray(kval.reshape(NT, 128).T)
    m["cvalid"] = cval[None, :]
    m["f0row"] = f0
    for k_ in ("w_in", "norm1_g", "norm2_g", "w_branch_a", "w_branch_b", "w_out", "w_ff1", "w_ff2",
               "ml_conv_w", "ml_conv_b", "ml_i_bias", "ml_f_bias", "nsa_k_gain", "nsa_q_gain"):
        m[k_] = np.ascontiguousarray(inputs[k_][0])
    for nm in ("w1", "b1", "w2", "b2", "pos"):
        m["cmp_" + nm] = np.stack([inputs["cmp_k_" + nm][0], inputs["cmp_v_" + nm][0]])
    tab = inputs["rel_table"]
    m["rel31"] = np.ascontiguousarray(tab[31])
    m["c_Bg"] = np.ascontiguousarray(tab[idx["Bg"]].transpose(0, 1, 3, 2))
    m["c_Bcg"] = np.ascontiguousarray(tab[idx["Bcg"]].transpose(0, 1, 3, 2))
    return m


_CACHE = {}


def kernel(**inputs):
    inputs = {k: np.asarray(v) for k, v in inputs.items()}
    NT = 64
    if "nc" not in _CACHE:
        _CACHE["nc"] = build_program(NT)
        _CACHE["consts"] = make_consts(NT)
    nc = _CACHE["nc"]
    consts, idx = _CACHE["consts"]
    in_maps = []
    for core in range(8):
        b, par = core // 2, core % 2
        in_maps.append(core_inputs(inputs, b, par, NT, consts, idx))
    res = run_bass_kernel_spmd(nc, in_maps, core_ids=list(range(8)))
    B, S = inputs["x"].shape[:2]
    outp = np.zeros((B, S, D), np.float32)
    for core in range(8):
        b, par = core // 2, core % 2
        o = np.asarray(res.results[core]["out"]).reshape(NT // 2, 128, D)
        for j in range(NT // 2):
            gt = 2 * j + par
            outp[b, gt * 128:(gt + 1) * 128] = o[j]
    return outp
```

```python
import contextlib
import numpy as np
import ml_dtypes
import concourse.bass as bass
import concourse.mybir as mybir
from concourse.bass_utils import run_bass_kernel_spmd

F32 = mybir.dt.float32
BF16 = mybir.dt.bfloat16
ALU = mybir.AluOpType
AF = mybir.ActivationFunctionType
AX = mybir.AxisListType

D = 1024
DPROJ = 5408
NRES = 3360
NEG = -30000.0


class Prog:
    CE = ("pe", "act", "dve", "pool")
    NDS = 12

    def __init__(self, nc, es):
        self.nc = nc
        self.ops = []
        self.sem = {e: es.enter_context(nc.semaphore("sem_" + e)) for e in self.CE}
        self.cnt = {e: 0 for e in self.CE}
        self.dsem = {q: [es.enter_context(nc.semaphore("dsem_%s%d" % (q, i))) for i in range(self.NDS)]
                     for q in ("sp", "pool", "act")}
        self.dtot = {q: [0] * self.NDS for q in self.dsem}
        self.drr = {q: 0 for q in self.dsem}
        self.waited = {e: {} for e in ("pe", "act", "dve", "pool", "sp")}
        self.lastw = {}
        self.readers = {}
        self.done = {}
        self.nops = 0
        self.stage_first = True

    def op(self, eng, fn, reads=(), writes=(), dma=False):
        deps = set()
        for k in reads:
            if k in self.lastw:
                deps.add(self.lastw[k])
        for k in writes:
            relax = (not dma) and eng in ("dve", "act", "pe")
            if k in self.lastw and not (relax and not self.ops[self.lastw[k]]["dma"] and self.ops[self.lastw[k]]["eng"] == eng):
                deps.add(self.lastw[k])
            for r in self.readers.get(k, ()):
                if not (relax and not self.ops[r]["dma"] and self.ops[r]["eng"] == eng):
                    deps.add(r)
        i = len(self.ops)
        deps.discard(i)
        if eng == "pe":
            deps = {d for d in deps if self.ops[d]["eng"] != "pe" or self.ops[d]["dma"]}
        self.ops.append(dict(eng=eng, fn=fn, deps=deps, dma=dma, flag=dma))
        for k in reads:
            self.readers.setdefault(k, []).append(i)
        for k in writes:
            self.lastw[k] = i
            self.readers[k] = []
        return i

    def dma(self, q, out, in_, reads=(), writes=()):
        return self.op(q, lambda e: e.dma_start(out=out, in_=in_), reads, writes, dma=True)

    def flush(self):
        ops = self.ops
        for o in ops:
            for d in o["deps"]:
                ops[d]["flag"] = True
        last = {}
        for i, o in enumerate(ops):
            last[o["eng"]] = i
        for e, i in last.items():
            ops[i]["flag"] = True
        comp = {}
        pre_wait = {}
        for i, o in enumerate(ops):
            e = o["eng"]
            if o["dma"]:
                s = self.drr[e] % self.NDS
                self.drr[e] += 1
                pre_wait[i] = (self.dsem[e][s], self.dtot[e][s])
                self.dtot[e][s] += 16
                comp[i] = (self.dsem[e][s], self.dtot[e][s], 16)
            elif o["flag"]:
                self.cnt[e] += 1
                comp[i] = (self.sem[e], self.cnt[e], 1)
        barrier = None
        if not self.stage_first:
            barrier = self.barrier_vals
        byeng = {}
        for i, o in enumerate(ops):
            byeng.setdefault(o["eng"], []).append(i)

        def emit(eng_name, eobj):
            w = self.waited[eng_name]

            def wait(sem, val):
                if val <= 0:
                    return
                key = id(sem)
                if w.get(key, 0) >= val:
                    return
                w[key] = val
                eobj.wait_ge(sem, val)
            if barrier is not None:
                for sem, val in barrier:
                    wait(sem, val)
            for i in byeng.get(eng_name, []):
                o = ops[i]
                for d in sorted(o["deps"]):
                    sem, val, _ = comp[d]
                    wait(sem, val)
                if i in pre_wait:
                    wait(*pre_wait[i])
                inst = o["fn"](eobj)
                if i in comp:
                    sem, val, inc = comp[i]
                    inst.then_inc(sem, inc)
            if getattr(self, "final", False):
                for q in self.dsem:
                    for s in range(self.NDS):
                        wait(self.dsem[q][s], self.dtot[q][s])

        with self.nc.Block() as block:
            block.sync(lambda e: emit("sp", e))
            block.tensor(lambda e: emit("pe", e))
            block.scalar(lambda e: emit("act", e))
            block.vector(lambda e: emit("dve", e))
            block.gpsimd(lambda e: emit("pool", e))
        bv = [(self.sem[e], self.cnt[e]) for e in self.CE]
        for q in self.dsem:
            for s in range(self.NDS):
                bv.append((self.dsem[q][s], self.dtot[q][s]))
        self.barrier_vals = bv
        self.stage_first = False
        self.nops += len(ops)
        self.ops = []
        self.lastw = {}
        self.readers = {}


def _bc(ap, shape):
    return ap.to_broadcast(shape)


def build_program(NT, stages=("s1", "s2", "s3", "s4", "s5"), debug=False):
    NJ = NT // 2
    NTOK = NT * 128
    NOWN = NJ * 128
    nc = bass.Bass("TRN2", target_bir_lowering=False)

    def din(name, shape, dt=F32):
        return nc.dram_tensor(name, list(shape), dt, kind="ExternalInput").ap()

    def dscr(name, shape, dt=F32):
        return nc.dram_tensor(name, list(shape), dt, kind="ExternalOutput" if debug else "Internal").ap()

    x_loc = din("x_loc", [NTOK, D])
    w_in = din("w_in", [D, DPROJ])
    norm1_g = din("norm1_g", [D])
    ident_in = din("c_ident", [128, 128])
    out = nc.dram_tensor("out", [NOWN, D], F32, kind="ExternalOutput").ap()

    proj = dscr("proj", [NTOK, NRES])
    projT = dscr("projT", [1280, NTOK + 16])
    conv_w = din("ml_conv_w", [4, 1024])
    conv_b = din("ml_conv_b", [1024])
    i_bias = din("ml_i_bias", [4])
    f_bias = din("ml_f_bias", [4])
    kvalid_tm = din("kvalid_tm", [128, NT])
    tri_in = din("c_tri", [128, 128])
    yb = dscr("yb", [NOWN, 512], BF16)
    ya = dscr("ya", [NOWN, 512], BF16)
    norm2_g = din("norm2_g", [D])
    w_pa = din("w_branch_a", [512, D])
    w_pb = din("w_branch_b", [512, D])
    w_out = din("w_out", [D, D])
    w_ff1 = din("w_ff1", [D, 4 * D])
    w_ff2 = din("w_ff2", [4 * D, D])
    NCB = 8 * NT - 1
    CT = (NCB + 127) // 128
    cw1 = din("cmp_w1", [2, 2048, 256])
    cb1 = din("cmp_b1", [2, 256])
    cw2 = din("cmp_w2", [2, 256, 64])
    cb2 = din("cmp_b2", [2, 64])
    cpos = din("cmp_pos", [2, 32, 64])
    k_gain = din("nsa_k_gain", [3, 64])
    q_gain = din("nsa_q_gain", [64])
    cvalid = din("cvalid", [1, CT * 128])
    kvalid_row = din("kvalid_row", [1, NTOK])
    f0row = din("f0row", [128])
    rel31 = din("rel31", [8])
    Bg = din("c_Bg", [2, 128, 8, 128])
    Bcg = din("c_Bcg", [8, 128, 8, 128])
    Mc = din("c_Mc", [8, 128, 128])
    causal_in = din("c_causal", [128, 128])
    w4_in = din("c_w4", [128, 128])
    c2s_in = din("c_c2s", [CT * 128, 128])
    onehot_in = din("c_onehot", [128, NTOK])
    vmB_in = din("c_vmB", [128, 256])
    amB_in = din("c_amB", [128, 256])
    fzB_in = din("c_fzB", [128, 256])
    bc_scr = dscr("bc_scr", [8, 2, 128, 1024], BF16)
    kc_scr = dscr("kc_scr", [2, 65, CT * 128], BF16)
    vc_scr = dscr("vc_scr", [2, CT * 128, 65], BF16)

    es = contextlib.ExitStack()
    with es:
        es.enter_context(nc.allow_non_contiguous_dma(reason="small parameter vectors / layout loads"))
        P = Prog(nc, es)
        ident = es.enter_context(nc.sbuf_tensor("ident", [128, 128], BF16))
        identf = es.enter_context(nc.sbuf_tensor("identf", [128, 128], F32))
        P.dma("sp", identf[:], ident_in[:, :], writes=["identf"])
        P.dma("pool", ident[:], ident_in[:, :], writes=["ident"])

        if "s1" in stages:
            stage1(nc, P, NT, x_loc, w_in, norm1_g, proj, projT, ident)
        if "s2" in stages:
            stage2(nc, P, NT, proj, projT, conv_w, conv_b, i_bias, f_bias, kvalid_tm, yb, ident, identf, tri_in, tri_in)
        if "s3" in stages:
            stage3(nc, P, NT, projT, cw1, cb1, cw2, cb2, cpos, k_gain[0], cvalid, kc_scr, vc_scr, ident)
        if "s4" in stages:
            stage4(nc, P, NT, proj, kc_scr, vc_scr, q_gain, k_gain, kvalid_row, f0row, rel31, Bg, Bcg, Mc, causal_in, w4_in,
                   c2s_in, onehot_in, vmB_in, amB_in, fzB_in, ya, ident, identf, bc_scr)
        if "s5" in stages:
            stage5(nc, P, NT, x_loc, ya, yb, w_in, norm1_g, norm2_g, w_pa, w_pb, w_out, w_ff1, w_ff2, out, ident)
        P.final = True
        P.flush()
    return nc


def stage1(nc, P, NT, x_loc, w_in, norm1_g, proj, projT, ident):
    NB = NT // 4
    with contextlib.ExitStack() as es:
        W = es.enter_context(nc.sbuf_tensor("s1_W", [128, 8, NRES], BF16))
        g1T = es.enter_context(nc.sbuf_tensor("s1_g1T", [128, 8], F32))
        xt = [es.enter_context(nc.sbuf_tensor("s1_xt%d" % i, [128, D], F32)) for i in range(4)]
        junk = es.enter_context(nc.sbuf_tensor("s1_junk", [128, D], BF16))
        xn = [es.enter_context(nc.sbuf_tensor("s1_xn%d" % i, [128, D], BF16)) for i in range(4)]
        st = [es.enter_context(nc.sbuf_tensor("s1_st%d" % i, [128, 4], F32)) for i in range(4)]
        hT = [es.enter_context(nc.sbuf_tensor("s1_hT%d" % i, [128, 8, 512], BF16)) for i in range(2)]
        stg = [es.enter_context(nc.sbuf_tensor("s1_stg%d" % i, [128, 2080], F32)) for i in range(2)]
        stgT = [es.enter_context(nc.sbuf_tensor("s1_stgT%d" % i, [128, 512], F32)) for i in range(3)]
        zt = es.enter_context(nc.sbuf_tensor("s1_z", [128, 16], F32))
        pT = [es.enter_context(nc.psum_tensor("s1_pT%d" % i, [128, 8, 128], BF16)) for i in range(2)]
        pp = [es.enter_context(nc.psum_tensor("s1_pp%d" % i, [128, 512], F32)) for i in range(4)]

        for k in range(8):
            P.dma("pool", W[:, k, :], w_in[k * 128:(k + 1) * 128, 0:NRES], writes=[("W", k)])
        P.dma("sp", g1T[:], norm1_g.rearrange("(c p) -> p c", p=128), writes=["g1T"])
        P.op("dve", lambda e: e.memset(zt[:], 0.0), writes=["zt"])
        for r in range(10):
            P.dma("sp", projT[r * 128:(r + 1) * 128, 0:16], zt[:], reads=["zt"], writes=[("projT_z", r)])

        TM = [(0, 512), (768, 512), (1280, 24), (2328, 512), (2840, 8), (2848, 512)]
        tm_off = []
        o = 0
        for c0, wd in TM:
            tm_off.append(o)
            o += wd
        CM = [(512, 64, 0), (576, 64, 64), (640, 64, 128), (704, 64, 192)]
        for h in range(4):
            CM.append((1304 + 128 * h, 128, 256 + 128 * h))
        for h in range(4):
            CM.append((1816 + 128 * h, 128, 768 + 128 * h))

        ppi = 0
        evi = 0
        def prep_a(b):
            for i in range(4):
                t = b * 4 + i
                xb, xnb, stb = xt[i], xn[i], st[i]
                P.dma("sp", xb[:], x_loc[t * 128:(t + 1) * 128, :], writes=[("xt", i)])
                P.op("act", lambda e, xb=xb, stb=stb: e.activation(out=junk[:], in_=xb[:], func=AF.Square, accum_out=stb[:, 0:1]),
                     reads=[("xt", i)], writes=["junk", ("st0", i)])
                P.op("act", lambda e, stb=stb: e.activation(out=stb[:, 1:2], in_=stb[:, 0:1], func=AF.Ln, scale=1.0 / D, bias=1e-6),
                     reads=[("st0", i)], writes=[("st1", i)])
                P.op("act", lambda e, stb=stb: e.activation(out=stb[:, 2:3], in_=stb[:, 1:2], func=AF.Exp, scale=-0.5),
                     reads=[("st1", i)], writes=[("st2", i)])
                P.op("dve", lambda e, xb=xb, xnb=xnb, stb=stb: e.tensor_scalar(out=xnb[:], in0=xb[:], scalar1=stb[:, 2:3], scalar2=None, op0=ALU.mult),
                     reads=[("xt", i), ("st2", i)], writes=[("xn", i)])

        def prep_b(b):
            hb = hT[b % 2]
            for i in range(4):
                xnb, ptb = xn[i], pT[i % 2]
                for c in range(8):
                    P.op("pe", lambda e, c=c, xnb=xnb, ptb=ptb: e.transpose(out=ptb[:, c, :], in_=xnb[:, c * 128:(c + 1) * 128], identity=ident[:]),
                         reads=[("xn", i), "ident"], writes=[("pT", i % 2)])
                P.op("dve", lambda e, hb=hb, ptb=ptb, i=i: e.tensor_tensor(out=hb[:, :, i * 128:(i + 1) * 128], in0=ptb[:], in1=_bc(g1T[:].rearrange("p (c o) -> p c o", o=1), [128, 8, 128]), op=ALU.mult),
                     reads=[("pT", i % 2), "g1T"], writes=[("hT", b % 2, i)])

        def proj_tok(b):
            nonlocal ppi, evi
            hb = hT[b % 2]
            for i in range(4):
                t = b * 4 + i
                sg = stg[t % 2]
                for gi, (c0, wd) in enumerate(TM):
                    if t % 2 == 0 and gi in (0, 5):
                        continue
                    pb = pp[ppi % 4]
                    pk = ("pp", ppi % 4)
                    ppi += 1
                    for k in range(8):
                        P.op("pe", lambda e, pb=pb, hb=hb, i=i, k=k, c0=c0, wd=wd: e.matmul(pb[:, 0:wd], lhsT=hb[:, k, i * 128:(i + 1) * 128], rhs=W[:, k, c0:c0 + wd], start=(k == 0), stop=(k == 7)),
                             reads=[("hT", b % 2, i), ("W", k)], writes=[pk])
                    eng = "dve" if evi % 2 == 0 else "act"
                    evi += 1
                    so = tm_off[gi]
                    if eng == "dve":
                        P.op("dve", lambda e, sg=sg, pb=pb, so=so, wd=wd: e.tensor_copy(out=sg[:, so:so + wd], in_=pb[:, 0:wd]),
                             reads=[pk], writes=[("stg", t % 2, gi)])
                    else:
                        P.op("act", lambda e, sg=sg, pb=pb, so=so, wd=wd: e.activation(out=sg[:, so:so + wd], in_=pb[:, 0:wd], func=AF.Copy),
                             reads=[pk], writes=[("stg", t % 2, gi)])
                    P.dma("sp", proj[t * 128:(t + 1) * 128, c0:c0 + wd], sg[:, so:so + wd], reads=[("stg", t % 2, gi)], writes=[("proj", t, gi)])

        def proj_chan(b):
            nonlocal ppi, evi
            hb = hT[b % 2]
            for ci, (c0, M, r0) in enumerate(CM):
                pb = pp[ppi % 4]
                pk = ("pp", ppi % 4)
                ppi += 1
                sT = stgT[ci % 3]
                for k in range(8):
                    P.op("pe", lambda e, pb=pb, hb=hb, k=k, c0=c0, M=M: e.matmul(pb[0:M, :], lhsT=W[:, k, c0:c0 + M], rhs=hb[:, k, :], start=(k == 0), stop=(k == 7)),
                         reads=[("hT", b % 2, 0), ("hT", b % 2, 1), ("hT", b % 2, 2), ("hT", b % 2, 3), ("W", k)], writes=[pk])
                eng = "dve" if evi % 2 == 0 else "act"
                evi += 1
                if eng == "dve":
                    P.op("dve", lambda e, sT=sT, pb=pb, M=M: e.tensor_copy(out=sT[0:M, :], in_=pb[0:M, :]), reads=[pk], writes=[("stgT", ci % 3)])
                else:
                    P.op("act", lambda e, sT=sT, pb=pb, M=M: e.activation(out=sT[0:M, :], in_=pb[0:M, :], func=AF.Copy), reads=[pk], writes=[("stgT", ci % 3)])
                P.dma("sp", projT[r0:r0 + M, 16 + b * 512:16 + (b + 1) * 512], sT[0:M, :], reads=[("stgT", ci % 3)], writes=[("projT", ci, b)])

        prep_a(0)
        prep_b(0)
        for b in range(NB):
            if b + 1 < NB:
                prep_a(b + 1)
            proj_tok(b)
            if b + 1 < NB:
                prep_b(b + 1)
            proj_chan(b)
        P.flush()


def stage2(nc, P, NT, proj, projT, conv_w, conv_b, i_bias, f_bias, kvalid_tm, yb, ident, identf, tri_in, mask_in):
    NB = NT // 4
    with contextlib.ExitStack() as es:
        def sb(name, shape, dt=F32):
            return es.enter_context(nc.sbuf_tensor("s2_" + name, shape, dt))

        def ps(name, shape, dt=F32):
            return es.enter_context(nc.psum_tensor("s2_" + name, shape, dt))
        cw = sb("cw", [128, 4, 8])
        cb = sb("cb", [128, 8])
        ib = sb("ib", [128, 4])
        fb = sb("fb", [128, 4])
        kv = sb("kv", [128, NT])
        tri = sb("tri", [128, 128])
        maskT = sb("maskT", [128, 128])
        ctmp = sb("ctmp", [128, 512])
        qkT = [sb("qkT%d" % i, [128, 8, 515]) for i in range(2)]
        acc = [sb("acc%d" % i, [128, 8, 512]) for i in range(2)]
        sg = sb("sg", [128, 8, 512])
        gl = [sb("gl%d" % i, [128, 4, 8]) for i in range(2)]
        ga = [sb("ga%d" % i, [128, 3, 4, 4]) for i in range(2)]
        mv = sb("mv", [128, 4, 512])
        mo = [sb("mo%d" % i, [128, 2, 512]) for i in range(2)]
        vaug = [sb("vaug%d" % i, [128, 4, 4, 129], BF16) for i in range(2)]
        U4 = [sb("U4%d" % i, [128, 4, 128]) for i in range(2)]
        R4 = [sb("R4%d" % i, [128, 4, 128]) for i in range(2)]
        qp4 = [sb("qp4%d" % i, [128, 4, 128], BF16) for i in range(2)]
        kpT4 = [sb("kpT4%d" % i, [128, 4, 128], BF16) for i in range(2)]
        kp4 = [sb("kp4%d" % i, [128, 4, 128], BF16) for i in range(2)]
        wT4 = [sb("wT4%d" % i, [128, 4, 128], BF16) for i in range(2)]
        dn4 = sb("dn4", [128, 4, 2])
        ybt = [sb("ybt%d" % i, [128, 512], BF16) for i in range(2)]
        Cf4 = sb("Cf4", [128, 4, 129])
        Cb4 = sb("Cb4", [128, 4, 129], BF16)
        PC = ps("PC", [128, 512])
        PR = ps("PR", [128, 512])
        PS = ps("PS", [128, 512])
        pD = [ps("pD%d" % i, [128, 512]) for i in range(2)]
        pOb = [ps("pOb%d" % i, [128, 512]) for i in range(2)]
        pk = ps("pk", [128, 1024], BF16)

        for jj in range(4):
            P.dma("sp", cw[:, jj, :], conv_w[jj, :].rearrange("(c p) -> p c", p=128), writes=[("cw", jj)])
        cwk = [("cw", jj) for jj in range(4)]
        P.dma("sp", cb[:], conv_b.rearrange("(c p) -> p c", p=128), writes=["cb"])
        P.dma("sp", ib[:], bass.AP(i_bias.tensor, 0, [[0, 128], [1, 4]]), writes=["ib"])
        P.dma("sp", fb[:], bass.AP(f_bias.tensor, 0, [[0, 128], [1, 4]]), writes=["fb"])
        P.dma("sp", kv[:], kvalid_tm[:, :], writes=["kv"])
        P.dma("sp", tri[:], tri_in[:, :], writes=["tri"])
        P.dma("sp", maskT[:], mask_in[:, :], writes=["maskT"])
        P.op("dve", lambda e: e.memset(Cf4[:], 0.0), writes=["Cf4"])
        P.op("dve", lambda e: e.memset(Cb4[:], 0.0), writes=["Cb4"])
        for i in range(2):
            P.op("pool", lambda e, i=i: e.memset(vaug[i][:], 1.0), writes=[("vaug", i, ii) for ii in range(4)])

        def front(b):
            pb = b % 2
            c0 = 16 + b * 512 - 3
            rows = slice(b * 512, (b + 1) * 512)
            P.dma("sp", qkT[pb][:], projT[256:1280, c0:c0 + 515].rearrange("(c p) n -> p c n", p=128), writes=[("qkT", pb)])
            P.dma("sp", gl[pb][:], proj[rows, 2840:2848].rearrange("(i p) c -> p i c", p=128), writes=[("gl", pb)])
            P.dma("sp", mv[:], proj[rows, 2328:2840].rearrange("(i p) c -> p i c", p=128), writes=["mv"])
            for oi in range(2):
                t = b * 4 + 2 * oi + 1
                P.dma("sp", mo[pb][:, oi, :], proj[t * 128:(t + 1) * 128, 2848:3360], writes=[("mo", pb)])
            for c in range(8):
                eng = "dve"
                P.op(eng, lambda e, c=c, pb=pb: e.tensor_scalar(out=acc[pb][:, c, :], in0=qkT[pb][:, c, 0:512], scalar1=cw[:, 0, c:c + 1], scalar2=cb[:, c:c + 1], op0=ALU.mult, op1=ALU.add),
                     reads=[("qkT", pb), "cb"] + cwk, writes=[("acc", pb, c)])
                for jj in range(1, 4):
                    if eng == "dve":
                        P.op(eng, lambda e, c=c, pb=pb, jj=jj: e.scalar_tensor_tensor(out=acc[pb][:, c, :], in0=qkT[pb][:, c, jj:jj + 512], scalar=cw[:, jj, c:c + 1], in1=acc[pb][:, c, :], op0=ALU.mult, op1=ALU.add),
                             reads=[("qkT", pb), ("acc", pb, c)] + cwk, writes=[("acc", pb, c)])
                    else:
                        P.op(eng, lambda e, c=c, pb=pb, jj=jj: e.tensor_scalar(out=ctmp[:], in0=qkT[pb][:, c, jj:jj + 512], scalar1=cw[:, jj, c:c + 1], scalar2=None, op0=ALU.mult),
                             reads=[("qkT", pb)] + cwk, writes=["ctmp"])
                        P.op(eng, lambda e, c=c, pb=pb: e.tensor_tensor(out=acc[pb][:, c, :], in0=acc[pb][:, c, :], in1=ctmp[:], op=ALU.add),
                             reads=["ctmp", ("acc", pb, c)], writes=[("acc", pb, c)])
            acck = [("acc", pb, c) for c in range(8)]
            for half in range(2):
                hs = slice(half * 4, half * 4 + 4)
                hk = acck[half * 4:half * 4 + 4]
                P.op("act", lambda e, pb=pb, hs=hs: e.activation(out=sg[:, hs, :], in_=acc[pb][:, hs, :], func=AF.Exp, scale=-1.0), reads=hk, writes=[("sg", half)])
                P.op("act", lambda e, hs=hs: e.activation(out=sg[:, hs, :], in_=sg[:, hs, :], func=AF.Ln, bias=1.0), reads=[("sg", half)], writes=[("sg", half)])
                P.op("act", lambda e, hs=hs: e.activation(out=sg[:, hs, :], in_=sg[:, hs, :], func=AF.Exp, scale=-1.0), reads=[("sg", half)], writes=[("sg", half)])
                P.op("dve", lambda e, pb=pb, hs=hs: e.tensor_tensor(out=acc[pb][:, hs, :], in0=acc[pb][:, hs, :], in1=sg[:, hs, :], op=ALU.mult), reads=hk + [("sg", half)], writes=hk)
            P.op("dve", lambda e, pb=pb: e.tensor_tensor(out=ga[pb][:, 2], in0=gl[pb][:, :, 4:8], in1=_bc(fb[:].rearrange("p (o h) -> p o h", o=1), [128, 4, 4]), op=ALU.add), reads=[("gl", pb), "fb"], writes=[("ga2", pb)])
            P.op("act", lambda e, pb=pb: e.activation(out=ga[pb][:, 2], in_=ga[pb][:, 2], func=AF.Exp, scale=-1.0), reads=[("ga2", pb)], writes=[("ga2", pb)])
            P.op("act", lambda e, pb=pb: e.activation(out=ga[pb][:, 0], in_=ga[pb][:, 2], func=AF.Ln, bias=1.0), reads=[("ga2", pb)], writes=[("ga0", pb)])
            P.op("dve", lambda e, pb=pb: e.tensor_tensor(out=ga[pb][:, 1], in0=gl[pb][:, :, 0:4], in1=_bc(ib[:].rearrange("p (o h) -> p o h", o=1), [128, 4, 4]), op=ALU.add), reads=[("gl", pb), "ib"], writes=[("ga1", pb)])
            P.op("dve", lambda e, pb=pb, b=b: e.tensor_tensor(out=ga[pb][:, 1], in0=ga[pb][:, 1], in1=_bc(kv[:, 4 * b:4 * b + 4].rearrange("p (i o) -> p i o", o=1), [128, 4, 4]), op=ALU.add), reads=[("ga1", pb), "kv"], writes=[("ga1", pb)])
            for i in range(4):
                P.op("act", lambda e, pb=pb, i=i: e.activation(out=vaug[pb][:, i, :, 0:128], in_=mv[:, i, :].rearrange("p (h d) -> p h d", d=128), func=AF.Copy), reads=["mv"], writes=[("vaug", pb, i)])
            P.op("act", lambda e, pb=pb: e.activation(out=mo[pb][:], in_=mo[pb][:], func=AF.Exp, scale=-1.0), reads=[("mo", pb)], writes=[("mo", pb)])
            P.op("act", lambda e, pb=pb: e.activation(out=mo[pb][:], in_=mo[pb][:], func=AF.Ln, bias=1.0), reads=[("mo", pb)], writes=[("mo", pb)])
            P.op("act", lambda e, pb=pb: e.activation(out=mo[pb][:], in_=mo[pb][:], func=AF.Exp, scale=-1.0), reads=[("mo", pb)], writes=[("mo", pb)])

        s_ = 128.0 ** -0.5

        def levels(t):
            b, i = t // 4, t % 4
            pb = b % 2
            tp = t % 2
            own = (t % 2 == 1)
            j = t // 2
            oi = i // 2
            yp = j % 2
            ts_ = slice(i * 128, (i + 1) * 128)
            A = {}

            def A1():
                for h in range(4):
                    a_col = ga[pb][:, 0, i, h:h + 1]
                    li_col = ga[pb][:, 1, i, h:h + 1]
                    hs = slice(h * 128, (h + 1) * 128)
                    P.op("pe", lambda e, a_col=a_col, hs=hs: e.matmul(PC[:, hs], lhsT=_bc(a_col, [128, 128]), rhs=tri[:], start=True, stop=True), reads=[("ga0", pb), "tri"], writes=["PC"])
                    P.op("pe", lambda e, a_col=a_col, hs=hs: e.matmul(PR[:, hs], lhsT=_bc(a_col, [128, 128]), rhs=tri[:], start=True, stop=False), reads=[("ga0", pb), "tri"], writes=["PR"])
                    P.op("pe", lambda e, li_col=li_col, hs=hs: e.matmul(PR[:, hs], lhsT=_bc(li_col, [128, 128]), rhs=identf[:], start=False, stop=True), reads=[("ga1", pb), "identf"], writes=["PR"])

            def A2():
                P.op("act", lambda e: e.activation(out=U4[tp][:].rearrange("p h n -> p (h n)"), in_=PC[:, :], func=AF.Exp, scale=-1.0), reads=["PC"], writes=[("U4", tp)])
                P.op("act", lambda e: e.activation(out=R4[tp][:].rearrange("p h n -> p (h n)"), in_=PR[:, :], func=AF.Exp), reads=["PR"], writes=[("R4", tp)])

            def A3():
                P.op("dve", lambda e: e.scalar_tensor_tensor(out=qp4[tp][:], in0=acc[pb][:, 0:4, ts_], scalar=s_, in1=U4[tp][:], op0=ALU.mult, op1=ALU.mult),
                     reads=[("acc", pb, c) for c in range(4)] + [("U4", tp)], writes=[("qp4", tp)])
                P.op("dve", lambda e: e.tensor_tensor(out=kpT4[tp][:], in0=acc[pb][:, 4:8, ts_], in1=R4[tp][:], op=ALU.mult),
                     reads=[("acc", pb, c) for c in range(4, 8)] + [("R4", tp)], writes=[("kpT4", tp)])

            def A4():
                for h in range(4):
                    P.op("pe", lambda e, h=h: e.transpose(out=pk[:, h * 128:(h + 1) * 128], in_=kpT4[tp][:, h, :], identity=ident[:]), reads=[("kpT4", tp), "ident"], writes=["pk"])

            def A5():
                P.op("act", lambda e: e.activation(out=kp4[tp][:].rearrange("p h n -> p (h n)"), in_=pk[:, 0:512], func=AF.Copy), reads=["pk"], writes=[("kp4", tp)])

            def B6():
                for h in range(4):
                    pd = pD[h // 2][:, (h % 2) * 129:(h % 2) * 129 + 129]
                    P.op("pe", lambda e, h=h, pd=pd: e.matmul(pd, lhsT=kp4[tp][:, h, :], rhs=vaug[pb][:, i, h, :], start=True, stop=True), reads=[("kp4", tp), ("vaug", pb, i)], writes=[("pD", h // 2)])
                if own:
                    for h in range(4):
                        P.op("pe", lambda e, h=h: e.matmul(PS[:, h * 128:(h + 1) * 128], lhsT=kpT4[tp][:, h, :], rhs=qp4[tp][:, h, :], start=True, stop=True), reads=[("kpT4", tp), ("qp4", tp)], writes=["PS"])

            def B7():
                if own:
                    P.op("dve", lambda e: e.tensor_tensor(out=wT4[tp][:], in0=PS[:, :].rearrange("p (h n) -> p h n", n=128), in1=_bc(maskT[:].rearrange("p (o n) -> p o n", o=1), [128, 4, 128]), op=ALU.mult), reads=["PS", "maskT"], writes=[("wT4", tp)])

            def B8():
                if own:
                    for h in range(4):
                        pO = pOb[h // 2][:, (h % 2) * 129:(h % 2) * 129 + 129]
                        P.op("pe", lambda e, h=h, pO=pO: e.matmul(pO, lhsT=wT4[tp][:, h, :], rhs=vaug[pb][:, i, h, :], start=True, stop=False), reads=[("wT4", tp), ("vaug", pb, i)], writes=[("pO", h // 2)])
                        P.op("pe", lambda e, h=h, pO=pO: e.matmul(pO, lhsT=qp4[tp][:, h, :], rhs=Cb4[:, h, :], start=False, stop=True), reads=[("qp4", tp), "Cb4"], writes=[("pO", h // 2)])

            def B9():
                for k in range(2):
                    P.op("dve", lambda e, k=k: e.tensor_tensor(out=Cf4[:, 2 * k:2 * k + 2, :], in0=pD[k][:, 0:258].rearrange("p (h e) -> p h e", e=129), in1=Cf4[:, 2 * k:2 * k + 2, :], op=ALU.add), reads=[("pD", k), "Cf4"], writes=["Cf4"])
                P.op("dve", lambda e: e.tensor_tensor(out=Cf4[:], in0=Cf4[:], in1=_bc(U4[tp][:, :, 127:128], [128, 4, 129]), op=ALU.mult), reads=["Cf4", ("U4", tp)], writes=["Cf4"])
                if own:
                    for k in range(2):
                        den = pOb[k][:, 0:258].rearrange("p (h e) -> p h e", e=129)[:, :, 128:129]
                        dk_ = dn4[:, 2 * k:2 * k + 2, :]
                        P.op("dve", lambda e, den=den, dk_=dk_: e.tensor_scalar(out=dk_[:, :, 1:2], in0=den, scalar1=-1.0, scalar2=1.0, op0=ALU.mult, op1=ALU.max), reads=[("pO", k)], writes=[("dn1", k)])
                        P.op("dve", lambda e, den=den, dk_=dk_: e.scalar_tensor_tensor(out=dk_[:, :, 0:1], in0=den, scalar=1.0, in1=dk_[:, :, 1:2], op0=ALU.max, op1=ALU.max), reads=[("pO", k), ("dn1", k)], writes=[("dn0", k)])
                        P.op("dve", lambda e, dk_=dk_: e.reciprocal(out=dk_[:, :, 1:2], in_=dk_[:, :, 0:1]), reads=[("dn0", k)], writes=[("dn1", k)])
                    for h in range(4):
                        pO = pOb[h // 2][:, (h % 2) * 129:(h % 2) * 129 + 128]
                        P.op("dve", lambda e, h=h, pO=pO: e.scalar_tensor_tensor(out=ybt[yp][:, h * 128:(h + 1) * 128], in0=pO, scalar=dn4[:, h, 1:2], in1=mo[pb][:, oi, h * 128:(h + 1) * 128], op0=ALU.mult, op1=ALU.mult),
                             reads=[("pO", h // 2), ("dn1", h // 2), ("mo", pb)], writes=[("ybt", yp, h)])
                P.op("act", lambda e: e.activation(out=Cb4[:].rearrange("p h e -> p (h e)"), in_=Cf4[:].rearrange("p h e -> p (h e)"), func=AF.Copy), reads=["Cf4"], writes=["Cb4"])
                if own:
                    P.dma("sp", yb[j * 128:(j + 1) * 128, :], ybt[yp][:], reads=[("ybt", yp, h) for h in range(4)], writes=[("yb", j)])
            return [A1, A2, A3, A4, A5], [B6, B7, B8, B9]

        front(0)
        if NB > 1:
            front(1)
        lv = {0: levels(0)}
        for f in lv[0][0]:
            f()
        for t in range(NT):
            if t % 4 == 0 and t // 4 + 2 < NB and t > 0:
                pass
            if t + 1 < NT:
                lv[t + 1] = levels(t + 1)
                An = lv[t + 1][0]
            else:
                An = []
            Bc = lv[t][1]
            for k in range(5):
                if k < len(An):
                    An[k]()
                if k < len(Bc):
                    Bc[k]()
            if t % 4 == 3:
                nb_ = t // 4 + 2
                if nb_ < NB:
                    front(nb_)
            del lv[t]
        P.flush()


def stage5(nc, P, NT, x_loc, ya, yb, w_in, norm1_g, norm2_g, w_pa, w_pb, w_out, w_ff1, w_ff2, out, ident):
    NJ = NT // 2
    NBK = (NJ + 3) // 4
    with contextlib.ExitStack() as es:
        def sb(name, shape, dt=F32):
            return es.enter_context(nc.sbuf_tensor("s5_" + name, shape, dt))

        def ps(name, shape, dt=F32):
            return es.enter_context(nc.psum_tensor("s5_" + name, shape, dt))
        Wg = sb("Wg", [128, 8, 2048], BF16)
        PA = sb("PA", [128, 4, 1024], BF16)
        PB = sb("PB", [128, 4, 1024], BF16)
        Wo = sb("Wo", [128, 8, 1024], BF16)
        g1T = sb("g1T", [128, 8])
        g2T = sb("g2T", [128, 8])
        xk = [sb("xk%d" % i, [128, D]) for i in range(4)]
        junk = sb("junk", [128, D], BF16)
        xn = [sb("xn%d" % i, [128, D], BF16) for i in range(2)]
        st = [sb("st%d" % i, [128, 4]) for i in range(2)]
        yat = [sb("yat%d" % i, [128, 512], BF16) for i in range(2)]
        ybt = [sb("ybt%d" % i, [128, 512], BF16) for i in range(2)]
        hT = sb("hT", [128, 8, 512], BF16)
        yaT = sb("yaT", [128, 4, 512], BF16)
        ybT = sb("ybT", [128, 4, 512], BF16)
        mixT = sb("mixT", [128, 8, 512], BF16)
        sa = [sb("sa%d" % i, [128, 512]) for i in range(2)]
        sbb = [sb("sbb%d" % i, [128, 512]) for i in range(2)]
        W1c = [sb("W1c%d" % i, [128, 8, 512], BF16) for i in range(2)]
        W2c = [sb("W2c%d" % i, [128, 4, 1024], BF16) for i in range(2)]
        rl = [sb("rl%d" % i, [128, 512]) for i in range(2)]
        aT = [sb("aT%d" % i, [128, 4, 512], BF16) for i in range(2)]
        pT = [ps("pT%d" % i, [128, 8, 128], BF16) for i in range(2)]
        pq = [ps("pq%d" % i, [128, 512]) for i in range(6)]
        pqi = [0]

        def nextp():
            i = pqi[0] % 6
            pqi[0] += 1
            return pq[i], ("pq", i)

        for k in range(8):
            P.dma("pool", Wg[:, k, :], w_in[k * 128:(k + 1) * 128, NRES:DPROJ], writes=[("Wg", k)])
            P.dma("pool", Wo[:, k, :], w_out[k * 128:(k + 1) * 128, :], writes=[("Wo", k)])
        for k in range(4):
            P.dma("pool", PA[:, k, :], w_pa[k * 128:(k + 1) * 128, :], writes=[("PA", k)])
            P.dma("pool", PB[:, k, :], w_pb[k * 128:(k + 1) * 128, :], writes=[("PB", k)])
        P.dma("sp", g1T[:], norm1_g.rearrange("(c p) -> p c", p=128), writes=["g1T"])
        P.dma("sp", g2T[:], norm2_g.rearrange("(c p) -> p c", p=128), writes=["g2T"])
        Wgk = [("Wg", k) for k in range(8)]
        Wok = [("Wo", k) for k in range(8)]

        def norm_T(src, srckey, gT, gkey, dstT, dkey, i, q):
            P.op("act", lambda e: e.activation(out=junk[:], in_=src[:], func=AF.Square, accum_out=st[q][:, 0:1]), reads=[srckey], writes=["junk", ("st0", q)])
            P.op("act", lambda e: e.activation(out=st[q][:, 1:2], in_=st[q][:, 0:1], func=AF.Ln, scale=1.0 / D, bias=1e-6), reads=[("st0", q)], writes=[("st1", q)])
            P.op("act", lambda e: e.activation(out=st[q][:, 2:3], in_=st[q][:, 1:2], func=AF.Exp, scale=-0.5), reads=[("st1", q)], writes=[("st2", q)])
            P.op("dve", lambda e: e.tensor_scalar(out=xn[q][:], in0=src[:], scalar1=st[q][:, 2:3], scalar2=None, op0=ALU.mult), reads=[srckey, ("st2", q)], writes=[("xn", q)])
            for c in range(8):
                P.op("pe", lambda e, c=c: e.transpose(out=pT[q][:, c, :], in_=xn[q][:, c * 128:(c + 1) * 128], identity=ident[:]), reads=[("xn", q), "ident"], writes=[("pT", q)])
            P.op("dve", lambda e: e.tensor_tensor(out=dstT[:, :, i * 128:(i + 1) * 128], in0=pT[q][:], in1=_bc(gT[:].rearrange("p (c o) -> p c o", o=1), [128, 8, 128]), op=ALU.mult),
                 reads=[("pT", q), gkey], writes=[(dkey, i)])

        for bk in range(NBK):
            nti = min(4, NJ - bk * 4)
            NTK = nti * 128
            for i in range(nti):
                j = bk * 4 + i
                t = 2 * j + 1
                q = i % 2
                P.dma("sp", xk[i][:], x_loc[t * 128:(t + 1) * 128, :], writes=[("xk", i)])
                P.dma("sp", yat[q][:], ya[j * 128:(j + 1) * 128, :], writes=[("yat", q)])
                P.dma("sp", ybt[q][:], yb[j * 128:(j + 1) * 128, :], writes=[("ybt", q)])
                norm_T(xk[i], ("xk", i), g1T, "g1T", hT, "hT", i, q)
                for (srct, skey, dst, dkey) in ((yat[q], ("yat", q), yaT, "yaT"), (ybt[q], ("ybt", q), ybT, "ybT")):
                    for c in range(4):
                        P.op("pe", lambda e, c=c, srct=srct: e.transpose(out=pT[q][:, c, :], in_=srct[:, c * 128:(c + 1) * 128], identity=ident[:]), reads=[skey, "ident"], writes=[("pT", q)])
                    P.op("act", lambda e, dst=dst, i=i: e.activation(out=dst[:, :, i * 128:(i + 1) * 128], in_=pT[q][:, 0:4, :], func=AF.Copy), reads=[("pT", q)], writes=[(dkey, i)])
            hTk = [("hT", i) for i in range(nti)]
            yaTk = [("yaT", i) for i in range(nti)]
            ybTk = [("ybT", i) for i in range(nti)]
            for f in range(8):
                q = f % 2
                pga, kga = nextp()
                for k in range(8):
                    P.op("pe", lambda e, pga=pga, k=k, f=f: e.matmul(pga[:, 0:NTK], lhsT=Wg[:, k, f * 128:(f + 1) * 128], rhs=hT[:, k, 0:NTK], start=(k == 0), stop=(k == 7)), reads=hTk + Wgk, writes=[kga])
                pgb, kgb = nextp()
                for k in range(8):
                    P.op("pe", lambda e, pgb=pgb, k=k, f=f: e.matmul(pgb[:, 0:NTK], lhsT=Wg[:, k, 1024 + f * 128:1024 + (f + 1) * 128], rhs=hT[:, k, 0:NTK], start=(k == 0), stop=(k == 7)), reads=hTk + Wgk, writes=[kgb])
                P.op("act", lambda e, pga=pga, q=q: e.activation(out=sa[q][:, 0:NTK], in_=pga[:, 0:NTK], func=AF.Exp, scale=-1.0), reads=[kga], writes=[("sa", q)])
                P.op("act", lambda e, pgb=pgb, q=q: e.activation(out=sbb[q][:, 0:NTK], in_=pgb[:, 0:NTK], func=AF.Exp, scale=-1.0), reads=[kgb], writes=[("sbb", q)])
                pa_, kpa = nextp()
                for k in range(4):
                    P.op("pe", lambda e, pa_=pa_, k=k, f=f: e.matmul(pa_[:, 0:NTK], lhsT=PA[:, k, f * 128:(f + 1) * 128], rhs=yaT[:, k, 0:NTK], start=(k == 0), stop=(k == 3)), reads=yaTk + [("PA", kk) for kk in range(4)], writes=[kpa])
                pb_, kpb = nextp()
                for k in range(4):
                    P.op("pe", lambda e, pb_=pb_, k=k, f=f: e.matmul(pb_[:, 0:NTK], lhsT=PB[:, k, f * 128:(f + 1) * 128], rhs=ybT[:, k, 0:NTK], start=(k == 0), stop=(k == 3)), reads=ybTk + [("PB", kk) for kk in range(4)], writes=[kpb])
                for (sx, skey) in ((sa[q], ("sa", q)), (sbb[q], ("sbb", q))):
                    P.op("act", lambda e, sx=sx: e.activation(out=sx[:, 0:NTK], in_=sx[:, 0:NTK], func=AF.Ln, bias=1.0), reads=[skey], writes=[skey])
                    P.op("act", lambda e, sx=sx: e.activation(out=sx[:, 0:NTK], in_=sx[:, 0:NTK], func=AF.Exp, scale=-1.0), reads=[skey], writes=[skey])
                P.op("dve", lambda e, q=q, pa_=pa_: e.tensor_tensor(out=sa[q][:, 0:NTK], in0=pa_[:, 0:NTK], in1=sa[q][:, 0:NTK], op=ALU.mult), reads=[kpa, ("sa", q)], writes=[("sa", q)])
                P.op("dve", lambda e, q=q, pb_=pb_: e.tensor_tensor(out=sbb[q][:, 0:NTK], in0=pb_[:, 0:NTK], in1=sbb[q][:, 0:NTK], op=ALU.mult), reads=[kpb, ("sbb", q)], writes=[("sbb", q)])
                P.op("dve", lambda e, q=q, f=f: e.tensor_tensor(out=mixT[:, f, 0:NTK], in0=sa[q][:, 0:NTK], in1=sbb[q][:, 0:NTK], op=ALU.add), reads=[("sa", q), ("sbb", q)], writes=[("mixT", f)])
            mixk = [("mixT", f) for f in range(8)]
            def wout(i):
                for hh in range(2):
                    po, kpo = nextp()
                    for f in range(8):
                        P.op("pe", lambda e, po=po, f=f, i=i, hh=hh: e.matmul(po[:, :], lhsT=mixT[:, f, i * 128:(i + 1) * 128], rhs=Wo[:, f, hh * 512:(hh + 1) * 512], start=(f == 0), stop=(f == 7)), reads=mixk + Wok, writes=[kpo])
                    P.op("dve", lambda e, po=po, i=i, hh=hh: e.tensor_tensor(out=xk[i][:, hh * 512:(hh + 1) * 512], in0=po[:, :], in1=xk[i][:, hh * 512:(hh + 1) * 512], op=ALU.add), reads=[kpo, ("xk", i)], writes=[("xk", i)])
            wout(0)
            for i in range(nti):
                if i + 1 < nti:
                    wout(i + 1)
                norm_T(xk[i], ("xk", i), g2T, "g2T", hT, "hT", i, i % 2)
            for fc in range(8):
                wq = fc % 2
                P.dma("pool", W1c[wq][:], w_ff1[:, fc * 512:(fc + 1) * 512].rearrange("(k p) n -> p k n", p=128), writes=[("W1c", wq)])
                P.dma("pool", W2c[wq][:], w_ff2[fc * 512:(fc + 1) * 512, :].rearrange("(k p) n -> p k n", p=128), writes=[("W2c", wq)])
                for m in range(4):
                    pf, kpf = nextp()
                    for k in range(8):
                        P.op("pe", lambda e, pf=pf, k=k, m=m, wq=wq: e.matmul(pf[:, 0:NTK], lhsT=W1c[wq][:, k, m * 128:(m + 1) * 128], rhs=hT[:, k, 0:NTK], start=(k == 0), stop=(k == 7)), reads=hTk + [("W1c", wq)], writes=[kpf])
                    rq = m % 2
                    P.op("act", lambda e, pf=pf, rq=rq: e.activation(out=rl[rq][:, 0:NTK], in_=pf[:, 0:NTK], func=AF.Relu), reads=[kpf], writes=[("rl", rq)])
                    P.op("dve", lambda e, rq=rq, m=m, wq=wq: e.tensor_tensor(out=aT[wq][:, m, 0:NTK], in0=rl[rq][:, 0:NTK], in1=rl[rq][:, 0:NTK], op=ALU.mult), reads=[("rl", rq)], writes=[("aT", wq, m)])
                aTk = [("aT", wq, m) for m in range(4)]
                for i in range(nti):
                    for hh in range(2):
                        po, kpo = nextp()
                        for m in range(4):
                            P.op("pe", lambda e, po=po, m=m, i=i, hh=hh, wq=wq: e.matmul(po[:, :], lhsT=aT[wq][:, m, i * 128:(i + 1) * 128], rhs=W2c[wq][:, m, hh * 512:(hh + 1) * 512], start=(m == 0), stop=(m == 3)), reads=aTk + [("W2c", wq)], writes=[kpo])
                        P.op("dve", lambda e, po=po, i=i, hh=hh: e.tensor_tensor(out=xk[i][:, hh * 512:(hh + 1) * 512], in0=po[:, :], in1=xk[i][:, hh * 512:(hh + 1) * 512], op=ALU.add), reads=[kpo, ("xk", i)], writes=[("xk", i)])
            for i in range(nti):
                j = bk * 4 + i
                P.dma("sp", out[j * 128:(j + 1) * 128, :], xk[i][:], reads=[("xk", i)], writes=[("out", j)])
        P.flush()


def stage3(nc, P, NT, projT, cw1, cb1, cw2, cb2, cpos, k_gain, cvalid, kc_scr, vc_scr, ident):
    NTOK = NT * 128
    NCB = 8 * NT - 1
    CT = (NCB + 127) // 128
    with contextlib.ExitStack() as es:
        def sb(name, shape, dt=F32):
            return es.enter_context(nc.sbuf_tensor("s3_" + name, shape, dt))

        def ps(name, shape, dt=F32):
            return es.enter_context(nc.psum_tensor("s3_" + name, shape, dt))
        xT = sb("xT", [64, NTOK + 16], BF16)
        w1 = sb("w1", [64, 32, 256], BF16)
        posT = sb("posT", [64, 32], BF16)
        b1 = sb("b1", [128, 2])
        b1e = sb("b1e", [128, 2])
        w2 = sb("w2", [128, 2, 64], BF16)
        b2 = sb("b2", [128, 64])
        gk = sb("gk", [128, 64])
        hx = [sb("hx%d" % i, [128, 512]) for i in range(2)]
        hu = [sb("hu%d" % i, [128, 512]) for i in range(2)]
        hidT = sb("hidT", [128, 2, CT * 128], BF16)
        o = [sb("o%d" % i, [128, 64]) for i in range(2)]
        junk = sb("junk", [128, 64])
        st = [sb("st%d" % i, [128, 4]) for i in range(2)]
        kn = [sb("kn%d" % i, [128, 64], BF16) for i in range(2)]
        kct = [sb("kct%d" % i, [65, 128], BF16) for i in range(2)]
        vct = [sb("vct%d" % i, [128, 65], BF16) for i in range(2)]
        pb = ps("pb", [128, 512])
        ph = [ps("ph%d" % i, [128, 512]) for i in range(2)]
        po = [ps("po%d" % i, [128, 512]) for i in range(2)]
        pt = [ps("pt%d" % i, [128, 1024], BF16) for i in range(2)]
        P.dma("sp", gk[:], bass.AP(k_gain.tensor, k_gain.offset, [[0, 128], [1, 64]]), writes=["gk"])
        P.op("dve", lambda e: e.memset(hidT[:], 0.0), writes=["hidT"])
        for i in range(2):
            P.op("pool", lambda e, i=i: e.memset(vct[i][:], 1.0), writes=[("vct", i)])
        it = 0
        for kind in range(2):
            for g in range(2):
                r0 = kind * 128 + g * 64
                P.dma("pool", xT[:, :], projT[r0:r0 + 64, 16:16 + NTOK + 16] if False else projT[r0:r0 + 64, 0:NTOK + 16], writes=["xT"])
                if g == 0:
                    P.dma("pool", w1[:], cw1[kind].rearrange("(l d) h -> d l h", d=64), writes=["w1"])
                    for l in range(32):
                        pass
                    P.dma("pool", posT[:], cpos[kind].rearrange("l d -> d l"), writes=["posT"])
                    P.dma("sp", b1[:], cb1[kind].rearrange("(c p) -> p c", p=128), writes=["b1"])
                    P.dma("pool", w2[:], cw2[kind].rearrange("(c p) d -> p c d", p=128), writes=["w2"])
                    P.dma("sp", b2[:], bass.AP(cb2[kind].tensor, cb2[kind].offset, [[0, 128], [1, 64]]), writes=["b2"])
                    for c in range(2):
                        for l in range(32):
                            P.op("pe", lambda e, c=c, l=l: e.matmul(pb[:, c:c + 1], lhsT=w1[:, l, c * 128:(c + 1) * 128], rhs=posT[:, l:l + 1], start=(l == 0), stop=(l == 31)), reads=["w1", "posT"], writes=["pb"])
                    P.op("dve", lambda e: e.tensor_tensor(out=b1e[:], in0=pb[:, 0:2], in1=b1[:], op=ALU.add), reads=["pb", "b1"], writes=["b1e"])
                xv = xT[:, :].rearrange("p (n r) -> p n r", r=16)
                for n0 in range(0, NCB, 512):
                    nn = min(512, NCB - n0)
                    for c in range(2):
                        q = it % 2
                        it += 1
                        for l in range(32):
                            if l < 16:
                                rhs = xv[:, 1 + n0:1 + n0 + nn, l]
                            else:
                                rhs = xv[:, 2 + n0:2 + n0 + nn, l - 16]
                            P.op("pe", lambda e, c=c, l=l, rhs=rhs, q=q: e.matmul(ph[q][:, 0:nn], lhsT=w1[:, l, c * 128:(c + 1) * 128], rhs=rhs, start=(l == 0), stop=(l == 31)), reads=["w1", "xT"], writes=[("ph", q)])
                        P.op("dve", lambda e, c=c, q=q: e.tensor_scalar(out=hx[q][:, 0:nn], in0=ph[q][:, 0:nn], scalar1=b1e[:, c:c + 1], scalar2=None, op0=ALU.add), reads=[("ph", q), "b1e"], writes=[("hx", q)])
                        P.op("dve", lambda e, q=q: e.tensor_tensor(out=hu[q][:, 0:nn], in0=hx[q][:, 0:nn], in1=hx[q][:, 0:nn], op=ALU.mult), reads=[("hx", q)], writes=[("hu", q)])
                        P.op("dve", lambda e, q=q: e.tensor_scalar(out=hu[q][:, 0:nn], in0=hu[q][:, 0:nn], scalar1=0.044715, scalar2=1.0, op0=ALU.mult, op1=ALU.add), reads=[("hu", q)], writes=[("hu", q)])
                        P.op("dve", lambda e, q=q: e.tensor_tensor(out=hu[q][:, 0:nn], in0=hu[q][:, 0:nn], in1=hx[q][:, 0:nn], op=ALU.mult), reads=[("hu", q), ("hx", q)], writes=[("hu", q)])
                        P.op("act", lambda e, q=q: e.activation(out=hu[q][:, 0:nn], in_=hu[q][:, 0:nn], func=AF.Exp, scale=-1.5957691216057308), reads=[("hu", q)], writes=[("hu", q)])
                        P.op("dve", lambda e, q=q: e.tensor_scalar(out=hu[q][:, 0:nn], in0=hu[q][:, 0:nn], scalar1=1.0, scalar2=None, op0=ALU.add), reads=[("hu", q)], writes=[("hu", q)])
                        P.op("dve", lambda e, q=q: e.reciprocal(out=hu[q][:, 0:nn], in_=hu[q][:, 0:nn]), reads=[("hu", q)], writes=[("hu", q)])
                        P.op("dve", lambda e, q=q, c=c, n0=n0: e.tensor_tensor(out=hidT[:, c, n0:n0 + nn], in0=hx[q][:, 0:nn], in1=hu[q][:, 0:nn], op=ALU.mult), reads=[("hu", q), ("hx", q)], writes=["hidT"])
                for ct in range(CT):
                    q = ct % 2
                    for c in range(2):
                        P.op("pe", lambda e, c=c, ct=ct, q=q: e.matmul(po[q][:, 0:64], lhsT=hidT[:, c, ct * 128:(ct + 1) * 128], rhs=w2[:, c, :], start=(c == 0), stop=(c == 1)), reads=["hidT", "w2"], writes=[("po", q)])
                    P.op("dve", lambda e, q=q: e.tensor_tensor(out=o[q][:], in0=po[q][:, 0:64], in1=b2[:], op=ALU.add), reads=[("po", q), "b2"], writes=[("o", q)])
                    if kind == 0:
                        P.op("act", lambda e, q=q: e.activation(out=junk[:], in_=o[q][:], func=AF.Square, accum_out=st[q][:, 0:1]), reads=[("o", q)], writes=["junk", ("st0", q)])
                        P.op("act", lambda e, q=q: e.activation(out=st[q][:, 1:2], in_=st[q][:, 0:1], func=AF.Ln, scale=1.0 / 64, bias=1e-6), reads=[("st0", q)], writes=[("st1", q)])
                        P.op("act", lambda e, q=q: e.activation(out=st[q][:, 2:3], in_=st[q][:, 1:2], func=AF.Exp, scale=-0.5), reads=[("st1", q)], writes=[("st2", q)])
                        P.op("dve", lambda e, q=q: e.scalar_tensor_tensor(out=kn[q][:], in0=o[q][:], scalar=st[q][:, 2:3], in1=gk[:], op0=ALU.mult, op1=ALU.mult), reads=[("o", q), ("st2", q), "gk"], writes=[("kn", q)])
                        P.op("pe", lambda e, q=q: e.transpose(out=pt[q][0:64, 0:128], in_=kn[q][:], identity=ident[:]), reads=[("kn", q), "ident"], writes=[("pt", q)])
                        P.op("act", lambda e, q=q: e.activation(out=kct[q][0:64, :], in_=pt[q][0:64, 0:128], func=AF.Copy), reads=[("pt", q)], writes=[("kct", q)])
                        P.dma("pool", kct[q][64:65, :], cvalid[0:1, ct * 128:(ct + 1) * 128], reads=[("kct", q)], writes=[("kct", q)])
                        P.dma("sp", kc_scr[g, :, ct * 128:(ct + 1) * 128], kct[q][:], reads=[("kct", q)], writes=[("kc_scr", g, ct)])
                    else:
                        P.op("dve", lambda e, q=q: e.tensor_copy(out=vct[q][:, 0:64], in_=o[q][:]), reads=[("o", q)], writes=[("vct", q)])
                        P.dma("sp", vc_scr[g, ct * 128:(ct + 1) * 128, :], vct[q][:], reads=[("vct", q)], writes=[("vc_scr", g, ct)])
        P.flush()


def _brow(ap, n):
    return bass.AP(ap.tensor, ap.offset, [[0, 128], [1, n]])


def stage4(nc, P, NT, proj, kc_scr, vc_scr, q_gain, k_gain, kvalid_row, f0row, rel31, Bg, Bcg, Mc, causal_in, w4_in,
           c2s_in, onehot_in, vmB_in, amB_in, fzB_in, ya, ident, identf, bc_scr):
    NJ = NT // 2
    NTOK = NT * 128
    NCB = 8 * NT - 1
    CT = (NCB + 127) // 128
    with contextlib.ExitStack() as es:
        def sb(name, shape, dt=F32):
            return es.enter_context(nc.sbuf_tensor("s4_" + name, shape, dt))

        def ps(name, shape, dt=F32):
            return es.enter_context(nc.psum_tensor("s4_" + name, shape, dt))
        KsT = sb("KsT", [65, 2, NTOK], BF16)
        KwT = sb("KwT", [65, 2, NTOK], BF16)
        Vs = sb("Vs", [128, NT, 2, 65], BF16)
        Vw = sb("Vw", [128, NT, 2, 65], BF16)
        KcT = sb("KcT", [65, 2, CT * 128], BF16)
        Vc = sb("Vc", [128, CT, 2, 65], BF16)
        c2s = sb("c2s", [128, CT, 128], BF16)
        onehot = sb("onehot", [128, NTOK], BF16)
        B01 = sb("B01", [128, 2, 8, 128])
        W4 = sb("W4", [128, 128])
        causal = sb("causal", [128, 128])
        ch = sb("ch", [128, 8])
        gq = sb("gq", [128, 64])
        gks = sb("gks", [128, 64])
        gkw = sb("gkw", [128, 64])
        f0 = sb("f0", [128, 128])
        vmB = sb("vmB", [128, 256])
        amB = sb("amB", [128, 256])
        fzB = sb("fzB", [128, 256])
        kvt = [sb("kvt%d" % i, [128, 512]) for i in range(2)]
        sqk = [sb("sqk%d" % i, [128, 256]) for i in range(2)]
        tmpk = [sb("tmpk%d" % i, [128, 256]) for i in range(2)]
        ssk = [sb("ssk%d" % i, [128, 8]) for i in range(2)]
        kb = [sb("kb%d" % i, [128, 256], BF16) for i in range(2)]
        qt = sb("qt", [128, 512])
        sqq = sb("sqq", [128, 512])
        tmpq = sb("tmpq", [128, 512])
        ssq = sb("ssq", [128, 16])
        qb_ = sb("qb", [128, 512], BF16)
        QT = [sb("QT%d" % i, [65, 8, 128], BF16) for i in range(2)]
        gsig = [sb("gsig%d" % i, [128, 24]) for i in range(2)]
        bct = sb("bct", [128, 8, 128])
        bctH = sb("bctH", [128, 8, 128], BF16)
        bctL = sb("bctL", [128, 8, 128], BF16)
        B01H = sb("B01H", [128, 2, 8, 128], BF16)
        B01L = sb("B01L", [128, 2, 8, 128], BF16)
        W4H = sb("W4H", [128, 4, 128], BF16)
        mct = sb("mct", [128, 128])
        sfp = [sb("sfp%d" % i, [128, 512]) for i in range(2)]
        PT = [sb("PT%d" % i, [128, 512], BF16) for i in range(4)]
        PTc = [sb("PTc%d" % i, [128, 512], BF16) for i in range(2)]
        rcM = sb("rcM", [128, 16])
        rcC = sb("rcC", [128, 16])
        imp = sb("imp", [128, 128])
        sc2 = sb("sc2", [128, 128])
        m8 = sb("m8", [128, 16])
        mneg = sb("mneg", [128, 128], BF16)
        mnegT = [sb("mnegT%d" % i, [128, 4, 128], BF16) for i in range(2)]
        yat = [sb("yat%d" % i, [128, 512]) for i in range(2)]
        yab = [sb("yab%d" % i, [128, 512], BF16) for i in range(2)]
        oTsM = sb("oTsM", [65, 512])
        oTsC = sb("oTsC", [65, 512])
        iTs = sb("iTs", [128, 512])
        psc = [ps("psc%d" % i, [128, 512]) for i in range(3)]
        povT = ps("povT", [128, 512])
        pov = ps("pov", [128, 512])
        povT2 = ps("povT2", [128, 512])
        pimT = ps("pimT", [128, 512])
        ptr = ps("ptr", [128, 1024], BF16)
        pch = ptr.bitcast(F32)

        P.dma("pool", onehot[:], onehot_in[:, :], writes=["onehot"])
        P.dma("pool", c2s[:], c2s_in.rearrange("(c p) b -> p c b", p=128), writes=["c2s"])
        P.dma("sp", W4[:], w4_in[:, :], writes=["W4"])
        P.dma("sp", causal[:], causal_in[:, :], writes=["causal"])
        P.dma("sp", ch[:], _brow(rel31, 8), writes=["ch"])
        P.dma("sp", gq[:], _brow(q_gain, 64), writes=["gq"])
        P.dma("sp", gks[:], _brow(k_gain[1], 64), writes=["gks"])
        P.dma("sp", gkw[:], _brow(k_gain[2], 64), writes=["gkw"])
        P.dma("sp", f0[:], _brow(f0row, 128), writes=["f0"])
        P.dma("sp", vmB[:], vmB_in[:, :], writes=["vmB"])
        P.dma("sp", amB[:], amB_in[:, :], writes=["amB"])
        P.dma("sp", fzB[:], fzB_in[:, :], writes=["fzB"])
        for d in range(2):
            P.dma("sp", B01[:, d], Bg[d], writes=[("B01", d)])
            P.op("dve", lambda e, d=d: e.tensor_tensor(out=B01[:, d], in0=B01[:, d], in1=_bc(ch[:].rearrange("p (h o) -> p h o", o=1), [128, 8, 128]), op=ALU.subtract), reads=[("B01", d), "ch"], writes=[("B01", d)])
        P.op("dve", lambda e: e.tensor_tensor(out=B01[:, 0], in0=B01[:, 0], in1=_bc(causal[:].rearrange("p (o q) -> p o q", o=1), [128, 8, 128]), op=ALU.add), reads=[("B01", 0), "causal"], writes=[("B01", 0)])
        for d in range(2):
            P.op("dve", lambda e, d=d: e.tensor_copy(out=B01H[:, d], in_=B01[:, d]), reads=[("B01", d)], writes=[("B01H", d)])
            P.op("dve", lambda e, d=d: e.tensor_tensor(out=B01[:, d], in0=B01[:, d], in1=B01H[:, d], op=ALU.subtract), reads=[("B01", d), ("B01H", d)], writes=[("B01", d)])
            P.op("dve", lambda e, d=d: e.tensor_copy(out=B01L[:, d], in_=B01[:, d]), reads=[("B01", d)], writes=[("B01L", d)])
        P.op("dve", lambda e: e.tensor_copy(out=W4H[:], in_=_bc(W4[:].rearrange("p (o q) -> p o q", o=1), [128, 4, 128])), reads=["W4"], writes=["W4H"])
        for g in range(2):
            P.dma("pool", KsT[64:65, g, :], kvalid_row[0:1, :], writes=[("KsTv", g)])
            P.dma("pool", KwT[64:65, g, :], kvalid_row[0:1, :], writes=[("KwTv", g)])
            P.dma("sp", KcT[:, g, :], kc_scr[g], writes=[("KcT", g)])
            P.dma("sp", Vc[:, :, g, :], vc_scr[g].rearrange("(c p) e -> p c e", p=128), writes=[("Vc", g)])
        P.op("pool", lambda e: e.memset(Vs[:], 1.0), writes=["Vs_init"])
        P.op("pool", lambda e: e.memset(Vw[:], 1.0), writes=["Vw_init"])
        for i in range(2):
            P.op("pool", lambda e, i=i: e.memset(QT[i][:], 1.0), writes=[("QT", i)])

        v3 = lambda ap, b=64: ap.rearrange("p (a b) -> p a b", b=b)

        def kvprep_levels(t):
            q = t % 2
            L = [[] for _ in range(7)]
            L[0].append(lambda: P.dma("sp", kvt[q][:], proj[t * 128:(t + 1) * 128, 768:1280], writes=[("kvt", q)]))
            for ci, c0 in enumerate((0, 256)):
                L[1].append(lambda ci=ci, c0=c0: P.op("dve", lambda e: e.tensor_tensor(out=v3(sqk[q][:, ci * 128:(ci + 1) * 128]), in0=v3(kvt[q][:, c0:c0 + 128]), in1=v3(kvt[q][:, c0:c0 + 128]), op=ALU.mult), reads=[("kvt", q)], writes=[("sqk", q, ci)]))
            L[1].append(lambda: P.op("dve", lambda e: e.tensor_reduce(out=ssk[q][:, 0:4], in_=v3(sqk[q][:]), axis=AX.X, op=ALU.add), reads=[("sqk", q, 0), ("sqk", q, 1)], writes=[("ssk0", q)]))
            L[1].append(lambda: P.op("pool", lambda e: e.tensor_copy(out=Vs[:, t, :, 0:64], in_=v3(kvt[q][:, 128:256])), reads=[("kvt", q), "Vs_init"], writes=[("Vs", t)]))
            L[1].append(lambda: P.op("pool", lambda e: e.tensor_copy(out=Vw[:, t, :, 0:64], in_=v3(kvt[q][:, 384:512])), reads=[("kvt", q), "Vw_init"], writes=[("Vw", t)]))
            L[2].append(lambda: P.op("act", lambda e: e.activation(out=ssk[q][:, 0:4], in_=ssk[q][:, 0:4], func=AF.Ln, scale=1.0 / 64, bias=1e-6), reads=[("ssk0", q)], writes=[("ssk0", q)]))
            L[2].append(lambda: P.op("act", lambda e: e.activation(out=ssk[q][:, 4:8], in_=ssk[q][:, 0:4], func=AF.Exp, scale=-0.5), reads=[("ssk0", q)], writes=[("ssk4", q)]))
            for ci, (c0, gt, gk_) in enumerate(((0, gks, "gks"), (256, gkw, "gkw"))):
                L[3].append(lambda ci=ci, c0=c0: P.op("dve", lambda e: e.tensor_tensor(out=v3(tmpk[q][:, ci * 128:(ci + 1) * 128]), in0=v3(kvt[q][:, c0:c0 + 128]), in1=_bc(ssk[q][:, 4 + 2 * ci:6 + 2 * ci].rearrange("p (a o) -> p a o", o=1), [128, 2, 64]), op=ALU.mult), reads=[("kvt", q), ("ssk4", q)], writes=[("tmpk", q, ci)]))
                L[3].append(lambda ci=ci, gt=gt, gk_=gk_: P.op("dve", lambda e: e.tensor_tensor(out=v3(kb[q][:, ci * 128:(ci + 1) * 128]), in0=v3(tmpk[q][:, ci * 128:(ci + 1) * 128]), in1=_bc(gt[:].rearrange("p (o b) -> p o b", o=1), [128, 2, 64]), op=ALU.mult), reads=[("tmpk", q, ci), gk_], writes=[("kb", q, ci)]))
            for a4 in range(4):
                L[4].append(lambda a4=a4: P.op("pe", lambda e: e.transpose(out=ptr[0:64, q * 512 + a4 * 128:q * 512 + (a4 + 1) * 128], in_=kb[q][:, a4 * 64:(a4 + 1) * 64], identity=ident[:]), reads=[("kb", q, 0), ("kb", q, 1), "ident"], writes=["ptr"]))
            L[5].append(lambda: P.op("act", lambda e: e.activation(out=KsT[0:64, :, t * 128:(t + 1) * 128], in_=ptr[0:64, q * 512:q * 512 + 256].rearrange("p (g n) -> p g n", n=128), func=AF.Copy), reads=["ptr"], writes=[("KsT", t)]))
            L[5].append(lambda: P.op("act", lambda e: e.activation(out=KwT[0:64, :, t * 128:(t + 1) * 128], in_=ptr[0:64, q * 512 + 256:q * 512 + 512].rearrange("p (g n) -> p g n", n=128), func=AF.Copy), reads=["ptr"], writes=[("KwT", t)]))
            return L

        def qprep_levels(Q, qb):
            L = [[] for _ in range(7)]
            v = ((Q % 16) - 1) // 2
            L[0].append(lambda: P.dma("sp", qt[:], proj[Q * 128:(Q + 1) * 128, 0:512], writes=["qt"]))
            L[0].append(lambda: P.dma("sp", gsig[qb][:], proj[Q * 128:(Q + 1) * 128, 1280:1304], writes=[("gsig", qb)]))
            first_use = (Q // 2) < 8
            if first_use:
                L[0].append(lambda: P.dma("sp", bct[:], Bcg[v], writes=["bct"]))
                L[0].append(lambda: P.dma("sp", mct[:], Mc[v], writes=["mct"]))
            else:
                L[0].append(lambda: P.dma("sp", bctH[:].rearrange("p h q -> p (h q)"), bc_scr[v, 0], reads=[("bc_scr", v, 0)], writes=["bctH"]))
                L[0].append(lambda: P.dma("sp", bctL[:].rearrange("p h q -> p (h q)"), bc_scr[v, 1], reads=[("bc_scr", v, 1)], writes=["bctL"]))
            L[1].append(lambda: P.op("dve", lambda e: e.tensor_tensor(out=v3(sqq[:]), in0=v3(qt[:]), in1=v3(qt[:]), op=ALU.mult), reads=["qt"], writes=["sqq"]))
            L[1].append(lambda: P.op("dve", lambda e: e.tensor_reduce(out=ssq[:, 0:8], in_=v3(sqq[:]), axis=AX.X, op=ALU.add), reads=["sqq"], writes=["ssq0"]))
            if first_use:
                L[1].append(lambda: P.op("dve", lambda e: e.tensor_tensor(out=bct[:], in0=bct[:], in1=_bc(ch[:].rearrange("p (h o) -> p h o", o=1), [128, 8, 128]), op=ALU.subtract), reads=["bct", "ch"], writes=["bct"]))
            if first_use:
                L[1].append(lambda: P.op("dve", lambda e: e.tensor_tensor(out=bct[:], in0=bct[:], in1=_bc(mct[:].rearrange("p (o q) -> p o q", o=1), [128, 8, 128]), op=ALU.add), reads=["bct", "mct"], writes=["bct"]))
            if first_use:
                L[2].append(lambda: P.op("dve", lambda e: e.tensor_copy(out=bctH[:], in_=bct[:]), reads=["bct"], writes=["bctH"]))
            if first_use:
                L[2].append(lambda: P.op("dve", lambda e: e.tensor_tensor(out=bct[:], in0=bct[:], in1=bctH[:], op=ALU.subtract), reads=["bct", "bctH"], writes=["bct"]))
            if first_use:
                L[2].append(lambda: P.op("dve", lambda e: e.tensor_copy(out=bctL[:], in_=bct[:]), reads=["bct"], writes=["bctL"]))
            L[2].append(lambda: P.op("act", lambda e: e.activation(out=ssq[:, 0:8], in_=ssq[:, 0:8], func=AF.Ln, scale=1.0 / 64, bias=1e-6), reads=["ssq0"], writes=["ssq0"]))
            L[2].append(lambda: P.op("act", lambda e: e.activation(out=ssq[:, 8:16], in_=ssq[:, 0:8], func=AF.Exp, scale=-0.5), reads=["ssq0"], writes=["ssq8"]))
            L[2].append(lambda: P.op("act", lambda e: e.activation(out=gsig[qb][:], in_=gsig[qb][:], func=AF.Exp, scale=-1.0), reads=[("gsig", qb)], writes=[("gsig", qb)]))
            L[2].append(lambda: P.op("act", lambda e: e.activation(out=gsig[qb][:], in_=gsig[qb][:], func=AF.Ln, bias=1.0), reads=[("gsig", qb)], writes=[("gsig", qb)]))
            L[2].append(lambda: P.op("act", lambda e: e.activation(out=gsig[qb][:], in_=gsig[qb][:], func=AF.Exp, scale=-1.0), reads=[("gsig", qb)], writes=[("gsig", qb)]))
            if first_use:
                L[3].append(lambda: P.dma("sp", bc_scr[v, 0], bctH[:].rearrange("p h q -> p (h q)"), reads=["bctH"], writes=[("bc_scr", v, 0)]))
                L[3].append(lambda: P.dma("sp", bc_scr[v, 1], bctL[:].rearrange("p h q -> p (h q)"), reads=["bctL"], writes=[("bc_scr", v, 1)]))
            L[3].append(lambda: P.op("dve", lambda e: e.tensor_tensor(out=v3(tmpq[:]), in0=v3(qt[:]), in1=_bc(ssq[:, 8:16].rearrange("p (a o) -> p a o", o=1), [128, 8, 64]), op=ALU.mult), reads=["qt", "ssq8"], writes=["tmpq"]))
            L[3].append(lambda: P.op("dve", lambda e: e.scalar_tensor_tensor(out=v3(qb_[:]), in0=v3(tmpq[:]), scalar=0.125, in1=_bc(gq[:].rearrange("p (o b) -> p o b", o=1), [128, 8, 64]), op0=ALU.mult, op1=ALU.mult), reads=["tmpq", "gq"], writes=["qb"]))
            for h in range(8):
                L[4].append(lambda h=h: P.op("pe", lambda e: e.transpose(out=ptr[0:64, h * 128:(h + 1) * 128], in_=qb_[:, h * 64:(h + 1) * 64], identity=ident[:]), reads=["qb", "ident"], writes=["ptr"]))
            L[5].append(lambda: P.op("act", lambda e: e.activation(out=QT[qb][0:64, :, :], in_=ptr[0:64, :].rearrange("p (h n) -> p h n", n=128), func=AF.Copy), reads=["ptr"], writes=[("QT", qb)]))
            return L

        pti = [0]
        sci = [0]

        def qkm_op(QTg, qtkey, kt, kT_of, use_mask, mT, pq=None, bias=None):
            if pq is None:
                pq = sci[0]
            pscb = psc[pq]
            ksrc, kkeys = kT_of(kt)
            extra = []
            if use_mask:
                extra.append((onehot[:, kt * 128:(kt + 1) * 128], mT[0][:].rearrange("p h q -> p (h q)"), ["onehot", mT[1]]))
            if bias is not None:
                for bap in bias[0]:
                    extra.append((ident[:], bap.rearrange("p h q -> p (h q)"), ["ident"] + bias[1]))
            P.op("pe", lambda e: e.matmul(pscb[:, :], lhsT=ksrc, rhs=QTg, start=True, stop=(len(extra) == 0)), reads=kkeys + [qtkey], writes=[("psc", pq)])
            for xi, (l_, r_, k_) in enumerate(extra):
                P.op("pe", lambda e, l_=l_, r_=r_, xi=xi: e.matmul(pscb[:, :], lhsT=l_, rhs=r_, start=False, stop=(xi == len(extra) - 1)), reads=k_, writes=[("psc", pq)])
            return pq

        def exp_op(pq, has_bias=False):
            pi_ = pti[0] % 4
            pti[0] += 1
            P.op("act", lambda e: e.activation(out=PT[pi_][:], in_=psc[pq][:, :], func=AF.Exp), reads=[("psc", pq)], writes=[("PT", pi_)])
            return pi_

        def pv_op(pi_, vsrc, vkeys, acc, acckey, first, last):
            P.op("pe", lambda e: e.matmul(acc[0:65, :], lhsT=vsrc, rhs=PT[pi_][:], start=first, stop=last), reads=[("PT", pi_)] + vkeys, writes=[acckey])

        def norm_ops(po, pok, rc, rckey, gs, gskey, g, br, first_branch, yt, ytk):
            pov3 = po[:, 0:260].rearrange("p (h e) -> p h e", e=65)
            P.op("dve", lambda e: e.tensor_scalar(out=rc[:, 0:4].rearrange("p (h o) -> p h o", o=1), in0=pov3[:, :, 64:65], scalar1=1e-30, scalar2=None, op0=ALU.max), reads=[pok], writes=[rckey + "0"])
            P.op("dve", lambda e: e.reciprocal(out=rc[:, 4:8], in_=rc[:, 0:4]), reads=[rckey + "0"], writes=[rckey + "4"])
            gv = gs[:, g * 12:(g + 1) * 12].rearrange("p (h b) -> p h b", b=3)[:, :, br:br + 1]
            P.op("dve", lambda e: e.tensor_tensor(out=rc[:, 8:12].rearrange("p (h o) -> p h o", o=1), in0=rc[:, 4:8].rearrange("p (h o) -> p h o", o=1), in1=gv, op=ALU.mult), reads=[rckey + "4", gskey], writes=[rckey + "8"])
            for h in range(4):
                dst = yt[:, (g * 4 + h) * 64:(g * 4 + h + 1) * 64]
                if first_branch:
                    P.op("dve", lambda e, h=h, dst=dst: e.tensor_scalar(out=dst, in0=pov3[:, h, 0:64], scalar1=rc[:, 8 + h:9 + h], scalar2=None, op0=ALU.mult), reads=[pok, rckey + "8"], writes=[(ytk, g, h)])
                else:
                    P.op("dve", lambda e, h=h, dst=dst: e.scalar_tensor_tensor(out=dst, in0=pov3[:, h, 0:64], scalar=rc[:, 8 + h:9 + h], in1=dst, op0=ALU.mult, op1=ALU.add), reads=[pok, rckey + "8", (ytk, g, h)], writes=[(ytk, g, h)])

        def chain_levels(Q, g):
            j = Q // 2
            qb = j % 2
            QTg = QT[qb][:, 4 * g:4 * g + 4, :].rearrange("p h q -> p (h q)")
            ctb = (8 * Q + 6) // 128
            n = ctb + 1
            L = []
            st = {}
            for idx in range(n):
                kt = idx
                hb = (kt == ctb)
                def lev_a(idx=idx, kt=kt, hb=hb):
                    qkm_op(QTg, ("QT", qb), kt, lambda kt_: (KcT[:, g, kt_ * 128:(kt_ + 1) * 128], [("KcT", g)]), False, None, pq=None,
                           bias=(([bctH[:, 4 * g:4 * g + 4, :], bctL[:, 4 * g:4 * g + 4, :]], ["bctH", "bctL"]) if hb else None))
                    pqc = sci[0]
                    P.op("act", lambda e: e.activation(out=PTc[idx % 2][:], in_=psc[pqc][:, :], func=AF.Exp), reads=[("psc", pqc)], writes=[("PTc", idx % 2)])

                def lev_b(idx=idx, kt=kt):
                    P.op("pe", lambda e: e.matmul(povT2[0:65, :], lhsT=Vc[:, kt, g, :], rhs=PTc[idx % 2][:], start=(idx == 0), stop=(idx == n - 1)), reads=[("PTc", idx % 2), ("Vc", g)], writes=["povT2"])
                    P.op("pe", lambda e: e.matmul(pimT[:, :], lhsT=c2s[:, kt, :], rhs=PTc[idx % 2][:], start=(idx == 0), stop=(idx == n - 1)), reads=[("PTc", idx % 2), "c2s"], writes=["pimT"])
                L.append([lev_a])
                L.append([lev_b])
            L.append([lambda: P.op("dve", lambda e: e.tensor_copy(out=oTsC[0:65, :], in_=povT2[0:65, :]), reads=["povT2"], writes=["oTsC"]),
                      lambda: P.op("dve", lambda e: e.tensor_copy(out=iTs[:, :], in_=pimT[:, :]), reads=["pimT"], writes=["iTs"])])
            L.append([lambda h=h: P.op("pe", lambda e: e.transpose(out=pch[:, h * 65:(h + 1) * 65], in_=oTsC[0:65, h * 128:(h + 1) * 128], identity=identf[0:65, 0:65]), reads=["oTsC", "identf"], writes=["ptr"]) for h in range(4)])
            L.append([lambda: norm_ops(pch, "ptr", rcC, "rcC", gsig[qb], ("gsig", qb), g, 0, True, yat[qb], "yat%d" % qb)])
            L += [[] for _ in range(4)]
            L.append([lambda h=h: P.op("pe", lambda e: e.transpose(out=pch[:, h * 128:(h + 1) * 128], in_=iTs[:, h * 128:(h + 1) * 128], identity=identf[:, :]), reads=["iTs", "identf"], writes=["ptr"]) for h in range(4)])
            o0 = 128 - 2 * Q

            def topk():
                for h in range(4):
                    if h == 0:
                        P.op("dve", lambda e: e.tensor_scalar(out=imp[:], in0=pch[:, 0:128], scalar1=rcC[:, 4:5], scalar2=None, op0=ALU.mult), reads=["ptr", "rcC4"], writes=["imp"])
                    else:
                        P.op("dve", lambda e, h=h: e.scalar_tensor_tensor(out=imp[:], in0=pch[:, h * 128:(h + 1) * 128], scalar=rcC[:, 4 + h:5 + h], in1=imp[:], op0=ALU.mult, op1=ALU.add), reads=["ptr", "rcC4", "imp"], writes=["imp"])
                P.op("dve", lambda e: e.tensor_tensor(out=imp[:], in0=imp[:], in1=vmB[:, o0:o0 + 128], op=ALU.mult), reads=["imp", "vmB"], writes=["imp"])
                P.op("dve", lambda e: e.tensor_tensor(out=imp[:], in0=imp[:], in1=amB[:, o0:o0 + 128], op=ALU.add), reads=["imp", "amB"], writes=["imp"])
                P.op("dve", lambda e: e.tensor_tensor(out=imp[:], in0=imp[:], in1=fzB[:, o0:o0 + 128], op=ALU.max), reads=["imp", "fzB"], writes=["imp"])
                P.op("dve", lambda e: e.tensor_tensor(out=imp[:], in0=imp[:], in1=f0[:], op=ALU.max), reads=["imp", "f0"], writes=["imp"])
                P.op("dve", lambda e: e.max(out=m8[:, 0:8], in_=imp[:]), reads=["imp"], writes=["m8a"])
                P.op("dve", lambda e: e.match_replace(out=sc2[:], in_to_replace=m8[:, 0:8], in_values=imp[:], imm_value=-3.0), reads=["imp", "m8a"], writes=["sc2"])
                P.op("dve", lambda e: e.max(out=m8[:, 8:16], in_=sc2[:]), reads=["sc2"], writes=["m8b"])
                P.op("dve", lambda e: e.tensor_scalar(out=mneg[:], in0=imp[:], scalar1=m8[:, 15:16], scalar2=NEG, op0=ALU.is_lt, op1=ALU.mult), reads=["imp", "m8b"], writes=["mneg"])
            L.append([topk])
            L += [[] for _ in range(8)]
            mi = (2 * j + g) % 2
            L.append([lambda: P.op("pe", lambda e: e.transpose(out=ptr[:, 0:128], in_=mneg[:], identity=ident[:]), reads=["mneg", "ident"], writes=["ptr"])])
            L.append([lambda: P.op("act", lambda e: e.activation(out=mnegT[mi][:], in_=_bc(ptr[:, 0:128].rearrange("p (o q) -> p o q", o=1), [128, 4, 128]), func=AF.Copy), reads=["ptr"], writes=[("mnegT", mi)])])
            return L

        def zip_levels(*lists):
            n = max(len(l) for l in lists)
            out_ = []
            for i in range(n):
                lv = []
                for l in lists:
                    if i < len(l):
                        lv += l[i]
                out_.append(lv)
            return out_

        def spaced(L):
            gaps = {0: 1, 1: 2, 2: 1, 3: 2, 4: 1}
            out_ = []
            for i, lv in enumerate(L):
                out_.append(lv)
                out_ += [[] for _ in range(gaps.get(i, 0))]
            return out_

        def run_levels(levels):
            for lv in levels:
                for f in lv:
                    f()

        def main_unit(Q, g, filler, carry):
            j = Q // 2
            qb = j % 2
            mi = (2 * j + g) % 2
            QTg = QT[qb][:, 4 * g:4 * g + 4, :].rearrange("p h q -> p (h q)")
            sel_kts = list(range(Q + 1))
            win_kts = list(range(max(0, Q - 4), Q + 1))
            total_it = len(sel_kts) + len(win_kts)
            done_it = [0]
            fpos = [0]

            def pull():
                done_it[0] += 1
                rem_it = max(1, total_it - done_it[0] + 1 - 8)
                rem_f = len(filler) - fpos[0]
                k = -(-rem_f // rem_it) if rem_it > 0 else rem_f
                for _ in range(k):
                    if fpos[0] < len(filler):
                        for f in filler[fpos[0]]:
                            f()
                        fpos[0] += 1

            class Branch:
                def __init__(self, kts, kT_of, v_of, bias_of, use_mask, br):
                    self.kts, self.kT_of, self.v_of, self.bias_of, self.use_mask, self.br = kts, kT_of, v_of, bias_of, use_mask, br
                    self.slots = {}
                    self.started = False

                def qk(self, idx):
                    self.slots[idx] = qkm_op(QTg, ("QT", qb), self.kts[idx], self.kT_of, self.use_mask, (mnegT[mi], ("mnegT", mi)), pq=idx % 3, bias=self.bias_of(self.kts[idx]))

                def start(self):
                    if not self.started:
                        self.qk(0)
                        if len(self.kts) > 1:
                            self.qk(1)
                        self.started = True

                def loop(self, after=None):
                    n = len(self.kts)
                    self.start()
                    for idx in range(n):
                        if idx + 2 < n:
                            self.qk(idx + 2)
                        sci[0] = idx % 3
                        pi_ = exp_op(self.slots[idx])
                        vsrc, vkeys = self.v_of(self.kts[idx])
                        pv_op(pi_, vsrc, vkeys, povT, "povT", idx == 0, idx == n - 1)
                        if after is not None and (idx == after[0] or (idx == n - 1 and after[0] >= n)):
                            for f in after[1]:
                                f()
                        pull()

                def epi_copy(self):
                    P.op("dve", lambda e: e.tensor_copy(out=oTsM[0:65, :], in_=povT[0:65, :]), reads=["povT"], writes=["oTsM"])

                def epi_rest(self):
                    for h in range(4):
                        P.op("pe", lambda e, h=h: e.transpose(out=pov[:, h * 65:(h + 1) * 65], in_=oTsM[0:65, h * 128:(h + 1) * 128], identity=identf[0:65, 0:65]), reads=["oTsM", "identf"], writes=["pov"])
                    norm_ops(pov, "pov", rcM, "rcM", gsig[qb], ("gsig", qb), g, self.br, False, yat[qb], "yat%d" % qb)

            def sbias(kt):
                dl = Q - kt
                if dl <= 1:
                    return ([B01H[:, dl, 4 * g:4 * g + 4, :], B01L[:, dl, 4 * g:4 * g + 4, :]], [("B01H", dl), ("B01L", dl)])
                return None

            def wbias(kt):
                dl = Q - kt
                if dl <= 1:
                    return sbias(kt)
                if dl == 4:
                    return ([W4H[:]], ["W4H"])
                return None
            selB = Branch(sel_kts,
                          lambda kt: (KsT[:, g, kt * 128:(kt + 1) * 128], [("KsT", kt), ("KsTv", g)]),
                          lambda kt: (Vs[:, kt, g, :], [("Vs", kt)]), sbias, (2 * Q + 2 > 16), 1)
            winB = Branch(win_kts,
                          lambda kt: (KwT[:, g, kt * 128:(kt + 1) * 128], [("KwT", kt), ("KwTv", g)]),
                          lambda kt: (Vw[:, kt, g, :], [("Vw", kt)]), wbias, False, 2)
            selB.loop(after=(1, carry))
            winB.start()
            selB.epi_copy()
            winB.loop(after=(1, [selB.epi_rest]))
            while fpos[0] < len(filler):
                for f in filler[fpos[0]]:
                    f()
                fpos[0] += 1
            winB.epi_copy()

            def finalize():
                if g == 1:
                    P.op("dve", lambda e: e.tensor_copy(out=yab[qb][:], in_=yat[qb][:]), reads=[("yat%d" % qb, gg, h) for gg in range(2) for h in range(4)], writes=[("yab", qb)])
                    P.dma("sp", ya[j * 128:(j + 1) * 128, :], yab[qb][:], reads=[("yab", qb)], writes=[("ya", j)])
            return [winB.epi_rest, finalize]

        units = [(2 * j + 1, g) for j in range(NJ) for g in range(2)]
        run_levels(zip_levels(kvprep_levels(0), kvprep_levels(1)))
        run_levels(qprep_levels(1, 0))
        run_levels(chain_levels(1, 0))
        carry = []
        for ui, (Q, g) in enumerate(units):
            filler = []
            if ui + 1 < len(units):
                nQ, ng = units[ui + 1]
                if nQ != Q:
                    jn = nQ // 2
                    filler += zip_levels(spaced(kvprep_levels(2 * jn)), spaced(kvprep_levels(2 * jn + 1)),
                                         [[] for _ in range(4)] + spaced(qprep_levels(nQ, jn % 2)))
                filler += chain_levels(nQ, ng)
            carry = main_unit(Q, g, filler, carry)
        for f in carry:
            f()
        P.flush()


def _t5_bucket_np(dist):
    n = np.maximum(dist, 0)
    nf = np.maximum(n, 1).astype(np.float32)
    large = 16 + (np.log(nf / np.float32(16)) / np.float32(np.log(8.0)) * np.float32(16)).astype(np.int32)
    return np.where(n < 16, n, np.minimum(large, 31)).astype(np.int64)


def make_consts(NT):
    NTOK = NT * 128
    NCB = 8 * NT - 1
    CT = (NCB + 127) // 128
    c = {}
    c["c_ident"] = np.eye(128, dtype=np.float32)
    c["c_tri"] = np.triu(np.ones((128, 128), np.float32))
    k = np.arange(128)[:, None]
    q = np.arange(128)[None, :]
    c["c_causal"] = np.where(k > q, NEG, 0.0).astype(np.float32)
    c["c_w4"] = np.where(k <= q, NEG, 0.0).astype(np.float32)
    n = np.arange(CT * 128)[:, None]
    blk = np.arange(128)[None, :]
    c2s = ((16 * n < 64 * blk + 64) & (16 * n + 32 > 64 * blk) & (n < NCB)).astype(np.float32)
    c["c_c2s"] = c2s
    key = np.arange(NTOK)[None, :]
    c["c_onehot"] = (key // 64 == np.arange(128)[:, None]).astype(np.float32)
    qq = np.arange(128)[:, None]
    rb = np.arange(256)[None, :] - 128
    cur = (qq >= 64).astype(np.int64)
    vm = (rb <= cur).astype(np.float32)
    c["c_vmB"] = vm
    c["c_amB"] = vm - 1.0
    c["c_fzB"] = np.where(rb == cur, 10003.0, np.where(rb == cur - 1, 10002.0, -1.0)).astype(np.float32)
    idx = {}
    idx["Bg"] = np.stack([_t5_bucket_np(128 * d + q - k) for d in range(2)])
    r = np.arange(128)[:, None]
    dists = np.stack([16 * 8 * (2 * v + 1) + q - 16 * r - 31 for v in range(8)])
    idx["Bcg"] = _t5_bucket_np(dists)
    c["c_Mc"] = np.where(dists < 0, NEG, 0.0).astype(np.float32)
    return c, idx


def core_inputs(inputs, b, par, NT, consts, idx):
    NTOK = NT * 128
    NCB = 8 * NT - 1
    CT = (NCB + 127) // 128
    x = inputs["x"][b]
    m = dict(consts)
    if par == 1:
        m["x_loc"] = np.ascontiguousarray(x[:NTOK])
    else:
        m["x_loc"] = np.concatenate([np.zeros((128, D), np.float32), x[:NTOK - 128]], axis=0)
    kval = np.zeros(NTOK, np.float32)
    cval = np.zeros(CT * 128, np.float32)
    cval[NCB:] = NEG
    f0 = np.full(128, -1.0, np.float32)
    if par == 0:
        kval[:128] = NEG
        cval[:8] = NEG
        f0[2] = 10001.0
    else:
        f0[0] = 10001.0
    m["kvalid_row"] = kval[None, :]
    m["kvalid_tm"] = np.ascontiguousarray(kval.reshape(NT, 128).T)
    m["cvalid"] = cval[None, :]
    m["f0row"] = f0
    for k_ in ("w_in", "norm1_g", "norm2_g", "w_branch_a", "w_branch_b", "w_out", "w_ff1", "w_ff2",
               "ml_conv_w", "ml_conv_b", "ml_i_bias", "ml_f_bias", "nsa_k_gain", "nsa_q_gain"):
        m[k_] = np.ascontiguousarray(inputs[k_][0])
    for nm in ("w1", "b1", "w2", "b2", "pos"):
        m["cmp_" + nm] = np.stack([inputs["cmp_k_" + nm][0], inputs["cmp_v_" + nm][0]])
    tab = inputs["rel_table"]
    m["rel31"] = np.ascontiguousarray(tab[31])
    m["c_Bg"] = np.ascontiguousarray(tab[idx["Bg"]].transpose(0, 1, 3, 2))
    m["c_Bcg"] = np.ascontiguousarray(tab[idx["Bcg"]].transpose(0, 1, 3, 2))
    return m


_CACHE = {}


def kernel(**inputs):
    inputs = {k: np.asarray(v) for k, v in inputs.items()}
    NT = 64
    if "nc" not in _CACHE:
        _CACHE["nc"] = build_program(NT)
        _CACHE["consts"] = make_consts(NT)
    nc = _CACHE["nc"]
    consts, idx = _CACHE["consts"]
    in_maps = []
    for core in range(8):
        b, par = core // 2, core % 2
        in_maps.append(core_inputs(inputs, b, par, NT, consts, idx))
    res = run_bass_kernel_spmd(nc, in_maps, core_ids=list(range(8)))
    B, S = inputs["x"].shape[:2]
    outp = np.zeros((B, S, D), np.float32)
    for core in range(8):
        b, par = core // 2, core % 2
        o = np.asarray(res.results[core]["out"]).reshape(NT // 2, 128, D)
        for j in range(NT // 2):
            gt = 2 * j + par
            outp[b, gt * 128:(gt + 1) * 128] = o[j]
    return outp
```

```python
import contextlib
import numpy as np
import ml_dtypes
import concourse.bass as bass
import concourse.mybir as mybir
from concourse.bass_utils import run_bass_kernel_spmd

F32 = mybir.dt.float32
BF16 = mybir.dt.bfloat16
ALU = mybir.AluOpType
AF = mybir.ActivationFunctionType
AX = mybir.AxisListType

D = 1024
DPROJ = 5408
NRES = 3360
NEG = -30000.0


class Prog:
    CE = ("pe", "act", "dve", "pool")
    NDS = 12

    def __init__(self, nc, es):
        self.nc = nc
        self.ops = []
        self.sem = {e: es.enter_context(nc.semaphore("sem_" + e)) for e in self.CE}
        self.cnt = {e: 0 for e in self.CE}
        self.dsem = {q: [es.enter_context(nc.semaphore("dsem_%s%d" % (q, i))) for i in range(self.NDS)]
                     for q in ("sp", "pool", "act")}
        self.dtot = {q: [0] * self.NDS for q in self.dsem}
        self.drr = {q: 0 for q in self.dsem}
        self.waited = {e: {} for e in ("pe", "act", "dve", "pool", "sp")}
        self.lastw = {}
        self.readers = {}
        self.done = {}
        self.nops = 0
        self.stage_first = True

    def op(self, eng, fn, reads=(), writes=(), dma=False):
        deps = set()
        for k in reads:
            if k in self.lastw:
                deps.add(self.lastw[k])
        for k in writes:
            relax = (not dma) and eng in ("dve", "act", "pe")
            if k in self.lastw and not (relax and not self.ops[self.lastw[k]]["dma"] and self.ops[self.lastw[k]]["eng"] == eng):
                deps.add(self.lastw[k])
            for r in self.readers.get(k, ()):
                if not (relax and not self.ops[r]["dma"] and self.ops[r]["eng"] == eng):
                    deps.add(r)
        i = len(self.ops)
        deps.discard(i)
        if eng == "pe":
            deps = {d for d in deps if self.ops[d]["eng"] != "pe" or self.ops[d]["dma"]}
        self.ops.append(dict(eng=eng, fn=fn, deps=deps, dma=dma, flag=dma))
        for k in reads:
            self.readers.setdefault(k, []).append(i)
        for k in writes:
            self.lastw[k] = i
            self.readers[k] = []
        return i

    def dma(self, q, out, in_, reads=(), writes=()):
        return self.op(q, lambda e: e.dma_start(out=out, in_=in_), reads, writes, dma=True)

    def flush(self):
        ops = self.ops
        for o in ops:
            for d in o["deps"]:
                ops[d]["flag"] = True
        last = {}
        for i, o in enumerate(ops):
            last[o["eng"]] = i
        for e, i in last.items():
            ops[i]["flag"] = True
        comp = {}
        pre_wait = {}
        for i, o in enumerate(ops):
            e = o["eng"]
            if o["dma"]:
                s = self.drr[e] % self.NDS
                self.drr[e] += 1
                pre_wait[i] = (self.dsem[e][s], self.dtot[e][s])
                self.dtot[e][s] += 16
                comp[i] = (self.dsem[e][s], self.dtot[e][s], 16)
            elif o["flag"]:
                self.cnt[e] += 1
                comp[i] = (self.sem[e], self.cnt[e], 1)
        barrier = None
        if not self.stage_first:
            barrier = self.barrier_vals
        byeng = {}
        for i, o in enumerate(ops):
            byeng.setdefault(o["eng"], []).append(i)

        def emit(eng_name, eobj):
            w = self.waited[eng_name]

            def wait(sem, val):
                if val <= 0:
                    return
                key = id(sem)
                if w.get(key, 0) >= val:
                    return
                w[key] = val
                eobj.wait_ge(sem, val)
            if barrier is not None:
                for sem, val in barrier:
                    wait(sem, val)
            for i in byeng.get(eng_name, []):
                o = ops[i]
                for d in sorted(o["deps"]):
                    sem, val, _ = comp[d]
                    wait(sem, val)
                if i in pre_wait:
                    wait(*pre_wait[i])
                inst = o["fn"](eobj)
                if i in comp:
                    sem, val, inc = comp[i]
                    inst.then_inc(sem, inc)
            if getattr(self, "final", False):
                for q in self.dsem:
                    for s in range(self.NDS):
                        wait(self.dsem[q][s], self.dtot[q][s])

        with self.nc.Block() as block:
            block.sync(lambda e: emit("sp", e))
            block.tensor(lambda e: emit("pe", e))
            block.scalar(lambda e: emit("act", e))
            block.vector(lambda e: emit("dve", e))
            block.gpsimd(lambda e: emit("pool", e))
        bv = [(self.sem[e], self.cnt[e]) for e in self.CE]
        for q in self.dsem:
            for s in range(self.NDS):
                bv.append((self.dsem[q][s], self.dtot[q][s]))
        self.barrier_vals = bv
        self.stage_first = False
        self.nops += len(ops)
        self.ops = []
        self.lastw = {}
        self.readers = {}


def _bc(ap, shape):
    return ap.to_broadcast(shape)


def build_program(NT, stages=("s1", "s2", "s3", "s4", "s5"), debug=False):
    NJ = NT // 2
    NTOK = NT * 128
    NOWN = NJ * 128
    nc = bass.Bass("TRN2", target_bir_lowering=False)

    def din(name, shape, dt=F32):
        return nc.dram_tensor(name, list(shape), dt, kind="ExternalInput").ap()

    def dscr(name, shape, dt=F32):
        return nc.dram_tensor(name, list(shape), dt, kind="ExternalOutput" if debug else "Internal").ap()

    x_loc = din("x_loc", [NTOK, D])
    w_in = din("w_in", [D, DPROJ])
    norm1_g = din("norm1_g", [D])
    ident_in = din("c_ident", [128, 128])
    out = nc.dram_tensor("out", [NOWN, D], F32, kind="ExternalOutput").ap()

    proj = dscr("proj", [NTOK, NRES])
    projT = dscr("projT", [1280, NTOK + 16])
    conv_w = din("ml_conv_w", [4, 1024])
    conv_b = din("ml_conv_b", [1024])
    i_bias = din("ml_i_bias", [4])
    f_bias = din("ml_f_bias", [4])
    kvalid_tm = din("kvalid_tm", [128, NT])
    tri_in = din("c_tri", [128, 128])
    yb = dscr("yb", [NOWN, 512], BF16)
    ya = dscr("ya", [NOWN, 512], BF16)
    norm2_g = din("norm2_g", [D])
    w_pa = din("w_branch_a", [512, D])
    w_pb = din("w_branch_b", [512, D])
    w_out = din("w_out", [D, D])
    w_ff1 = din("w_ff1", [D, 4 * D])
    w_ff2 = din("w_ff2", [4 * D, D])
    NCB = 8 * NT - 1
    CT = (NCB + 127) // 128
    cw1 = din("cmp_w1", [2, 2048, 256])
    cb1 = din("cmp_b1", [2, 256])
    cw2 = din("cmp_w2", [2, 256, 64])
    cb2 = din("cmp_b2", [2, 64])
    cpos = din("cmp_pos", [2, 32, 64])
    k_gain = din("nsa_k_gain", [3, 64])
    q_gain = din("nsa_q_gain", [64])
    cvalid = din("cvalid", [1, CT * 128])
    kvalid_row = din("kvalid_row", [1, NTOK])
    f0row = din("f0row", [128])
    rel31 = din("rel31", [8])
    Bg = din("c_Bg", [2, 128, 8, 128])
    Bcg = din("c_Bcg", [8, 128, 8, 128])
    Mc = din("c_Mc", [8, 128, 128])
    causal_in = din("c_causal", [128, 128])
    w4_in = din("c_w4", [128, 128])
    c2s_in = din("c_c2s", [CT * 128, 128])
    onehot_in = din("c_onehot", [128, NTOK])
    vmB_in = din("c_vmB", [128, 256])
    amB_in = din("c_amB", [128, 256])
    fzB_in = din("c_fzB", [128, 256])
    bc_scr = dscr("bc_scr", [8, 2, 128, 1024], BF16)
    kc_scr = dscr("kc_scr", [2, 65, CT * 128], BF16)
    vc_scr = dscr("vc_scr", [2, CT * 128, 65], BF16)

    es = contextlib.ExitStack()
    with es:
        es.enter_context(nc.allow_non_contiguous_dma(reason="small parameter vectors / layout loads"))
        P = Prog(nc, es)
        ident = es.enter_context(nc.sbuf_tensor("ident", [128, 128], BF16))
        identf = es.enter_context(nc.sbuf_tensor("identf", [128, 128], F32))
        P.dma("sp", identf[:], ident_in[:, :], writes=["identf"])
        P.dma("pool", ident[:], ident_in[:, :], writes=["ident"])

        if "s1" in stages:
            stage1(nc, P, NT, x_loc, w_in, norm1_g, proj, projT, ident)
        if "s2" in stages:
            stage2(nc, P, NT, proj, projT, conv_w, conv_b, i_bias, f_bias, kvalid_tm, yb, ident, identf, tri_in, tri_in)
        if "s3" in stages:
            stage3(nc, P, NT, projT, cw1, cb1, cw2, cb2, cpos, k_gain[0], cvalid, kc_scr, vc_scr, ident)
        if "s4" in stages:
            stage4(nc, P, NT, proj, kc_scr, vc_scr, q_gain, k_gain, kvalid_row, f0row, rel31, Bg, Bcg, Mc, causal_in, w4_in,
                   c2s_in, onehot_in, vmB_in, amB_in, fzB_in, ya, ident, identf, bc_scr)
        if "s5" in stages:
            stage5(nc, P, NT, x_loc, ya, yb, w_in, norm1_g, norm2_g, w_pa, w_pb, w_out, w_ff1, w_ff2, out, ident)
        P.final = True
        P.flush()
    return nc


def stage1(nc, P, NT, x_loc, w_in, norm1_g, proj, projT, ident):
    NB = NT // 4
    with contextlib.ExitStack() as es:
        W = es.enter_context(nc.sbuf_tensor("s1_W", [128, 8, NRES], BF16))
        g1T = es.enter_context(nc.sbuf_tensor("s1_g1T", [128, 8], F32))
        xt = [es.enter_context(nc.sbuf_tensor("s1_xt%d" % i, [128, D], F32)) for i in range(4)]
        junk = es.enter_context(nc.sbuf_tensor("s1_junk", [128, D], BF16))
        xn = [es.enter_context(nc.sbuf_tensor("s1_xn%d" % i, [128, D], BF16)) for i in range(4)]
        st = [es.enter_context(nc.sbuf_tensor("s1_st%d" % i, [128, 4], F32)) for i in range(4)]
        hT = [es.enter_context(nc.sbuf_tensor("s1_hT%d" % i, [128, 8, 512], BF16)) for i in range(2)]
        stg = [es.enter_context(nc.sbuf_tensor("s1_stg%d" % i, [128, 2080], F32)) for i in range(2)]
        stgT = [es.enter_context(nc.sbuf_tensor("s1_stgT%d" % i, [128, 512], F32)) for i in range(3)]
        zt = es.enter_context(nc.sbuf_tensor("s1_z", [128, 16], F32))
        pT = [es.enter_context(nc.psum_tensor("s1_pT%d" % i, [128, 8, 128], BF16)) for i in range(2)]
        pp = [es.enter_context(nc.psum_tensor("s1_pp%d" % i, [128, 512], F32)) for i in range(4)]

        for k in range(8):
            P.dma("pool", W[:, k, :], w_in[k * 128:(k + 1) * 128, 0:NRES], writes=[("W", k)])
        P.dma("sp", g1T[:], norm1_g.rearrange("(c p) -> p c", p=128), writes=["g1T"])
        P.op("dve", lambda e: e.memset(zt[:], 0.0), writes=["zt"])
        for r in range(10):
            P.dma("sp", projT[r * 128:(r + 1) * 128, 0:16], zt[:], reads=["zt"], writes=[("projT_z", r)])

        TM = [(0, 512), (768, 512), (1280, 24), (2328, 512), (2840, 8), (2848, 512)]
        tm_off = []
        o = 0
        for c0, wd in TM:
            tm_off.append(o)
            o += wd
        CM = [(512, 64, 0), (576, 64, 64), (640, 64, 128), (704, 64, 192)]
        for h in range(4):
            CM.append((1304 + 128 * h, 128, 256 + 128 * h))
        for h in range(4):
            CM.append((1816 + 128 * h, 128, 768 + 128 * h))

        ppi = 0
        evi = 0
        def prep_a(b):
            for i in range(4):
                t = b * 4 + i
                xb, xnb, stb = xt[i], xn[i], st[i]
                P.dma("sp", xb[:], x_loc[t * 128:(t + 1) * 128, :], writes=[("xt", i)])
                P.op("act", lambda e, xb=xb, stb=stb: e.activation(out=junk[:], in_=xb[:], func=AF.Square, accum_out=stb[:, 0:1]),
                     reads=[("xt", i)], writes=["junk", ("st0", i)])
                P.op("act", lambda e, stb=stb: e.activation(out=stb[:, 1:2], in_=stb[:, 0:1], func=AF.Ln, scale=1.0 / D, bias=1e-6),
                     reads=[("st0", i)], writes=[("st1", i)])
                P.op("act", lambda e, stb=stb: e.activation(out=stb[:, 2:3], in_=stb[:, 1:2], func=AF.Exp, scale=-0.5),
                     reads=[("st1", i)], writes=[("st2", i)])
                P.op("dve", lambda e, xb=xb, xnb=xnb, stb=stb: e.tensor_scalar(out=xnb[:], in0=xb[:], scalar1=stb[:, 2:3], scalar2=None, op0=ALU.mult),
                     reads=[("xt", i), ("st2", i)], writes=[("xn", i)])

        def prep_b(b):
            hb = hT[b % 2]
            for i in range(4):
                xnb, ptb = xn[i], pT[i % 2]
                for c in range(8):
                    P.op("pe", lambda e, c=c, xnb=xnb, ptb=ptb: e.transpose(out=ptb[:, c, :], in_=xnb[:, c * 128:(c + 1) * 128], identity=ident[:]),
                         reads=[("xn", i), "ident"], writes=[("pT", i % 2)])
                P.op("dve", lambda e, hb=hb, ptb=ptb, i=i: e.tensor_tensor(out=hb[:, :, i * 128:(i + 1) * 128], in0=ptb[:], in1=_bc(g1T[:].rearrange("p (c o) -> p c o", o=1), [128, 8, 128]), op=ALU.mult),
                     reads=[("pT", i % 2), "g1T"], writes=[("hT", b % 2, i)])

        def proj_tok(b):
            nonlocal ppi, evi
            hb = hT[b % 2]
            for i in range(4):
                t = b * 4 + i
                sg = stg[t % 2]
                for gi, (c0, wd) in enumerate(TM):
                    if t % 2 == 0 and gi in (0, 5):
                        continue
                    pb = pp[ppi % 4]
                    pk = ("pp", ppi % 4)
                    ppi += 1
                    for k in range(8):
                        P.op("pe", lambda e, pb=pb, hb=hb, i=i, k=k, c0=c0, wd=wd: e.matmul(pb[:, 0:wd], lhsT=hb[:, k, i * 128:(i + 1) * 128], rhs=W[:, k, c0:c0 + wd], start=(k == 0), stop=(k == 7)),
                             reads=[("hT", b % 2, i), ("W", k)], writes=[pk])
                    eng = "dve" if evi % 2 == 0 else "act"
                    evi += 1
                    so = tm_off[gi]
                    if eng == "dve":
                        P.op("dve", lambda e, sg=sg, pb=pb, so=so, wd=wd: e.tensor_copy(out=sg[:, so:so + wd], in_=pb[:, 0:wd]),
                             reads=[pk], writes=[("stg", t % 2, gi)])
                    else:
                        P.op("act", lambda e, sg=sg, pb=pb, so=so, wd=wd: e.activation(out=sg[:, so:so + wd], in_=pb[:, 0:wd], func=AF.Copy),
                             reads=[pk], writes=[("stg", t % 2, gi)])
                    P.dma("sp", proj[t * 128:(t + 1) * 128, c0:c0 + wd], sg[:, so:so + wd], reads=[("stg", t % 2, gi)], writes=[("proj", t, gi)])

        def proj_chan(b):
            nonlocal ppi, evi
            hb = hT[b % 2]
            for ci, (c0, M, r0) in enumerate(CM):
                pb = pp[ppi % 4]
                pk = ("pp", ppi % 4)
                ppi += 1
                sT = stgT[ci % 3]
                for k in range(8):
                    P.op("pe", lambda e, pb=pb, hb=hb, k=k, c0=c0, M=M: e.matmul(pb[0:M, :], lhsT=W[:, k, c0:c0 + M], rhs=hb[:, k, :], start=(k == 0), stop=(k == 7)),
                         reads=[("hT", b % 2, 0), ("hT", b % 2, 1), ("hT", b % 2, 2), ("hT", b % 2, 3), ("W", k)], writes=[pk])
                eng = "dve" if evi % 2 == 0 else "act"
                evi += 1
                if eng == "dve":
                    P.op("dve", lambda e, sT=sT, pb=pb, M=M: e.tensor_copy(out=sT[0:M, :], in_=pb[0:M, :]), reads=[pk], writes=[("stgT", ci % 3)])
                else:
                    P.op("act", lambda e, sT=sT, pb=pb, M=M: e.activation(out=sT[0:M, :], in_=pb[0:M, :], func=AF.Copy), reads=[pk], writes=[("stgT", ci % 3)])
                P.dma("sp", projT[r0:r0 + M, 16 + b * 512:16 + (b + 1) * 512], sT[0:M, :], reads=[("stgT", ci % 3)], writes=[("projT", ci, b)])

        prep_a(0)
        prep_b(0)
        for b in range(NB):
            if b + 1 < NB:
                prep_a(b + 1)
            proj_tok(b)
            if b + 1 < NB:
                prep_b(b + 1)
            proj_chan(b)
        P.flush()


def stage2(nc, P, NT, proj, projT, conv_w, conv_b, i_bias, f_bias, kvalid_tm, yb, ident, identf, tri_in, mask_in):
    NB = NT // 4
    with contextlib.ExitStack() as es:
        def sb(name, shape, dt=F32):
            return es.enter_context(nc.sbuf_tensor("s2_" + name, shape, dt))

        def ps(name, shape, dt=F32):
            return es.enter_context(nc.psum_tensor("s2_" + name, shape, dt))
        cw = sb("cw", [128, 4, 8])
        cb = sb("cb", [128, 8])
        ib = sb("ib", [128, 4])
        fb = sb("fb", [128, 4])
        kv = sb("kv", [128, NT])
        tri = sb("tri", [128, 128])
        maskT = sb("maskT", [128, 128])
        ctmp = sb("ctmp", [128, 512])
        qkT = [sb("qkT%d" % i, [128, 8, 515]) for i in range(2)]
        acc = [sb("acc%d" % i, [128, 8, 512]) for i in range(2)]
        sg = sb("sg", [128, 8, 512])
        gl = [sb("gl%d" % i, [128, 4, 8]) for i in range(2)]
        ga = [sb("ga%d" % i, [128, 3, 4, 4]) for i in range(2)]
        mv = sb("mv", [128, 4, 512])
        mo = [sb("mo%d" % i, [128, 2, 512]) for i in range(2)]
        vaug = [sb("vaug%d" % i, [128, 4, 4, 129], BF16) for i in range(2)]
        U4 = [sb("U4%d" % i, [128, 4, 128]) for i in range(2)]
        R4 = [sb("R4%d" % i, [128, 4, 128]) for i in range(2)]
        qp4 = [sb("qp4%d" % i, [128, 4, 128], BF16) for i in range(2)]
        kpT4 = [sb("kpT4%d" % i, [128, 4, 128], BF16) for i in range(2)]
        kp4 = [sb("kp4%d" % i, [128, 4, 128], BF16) for i in range(2)]
        wT4 = [sb("wT4%d" % i, [128, 4, 128], BF16) for i in range(2)]
        dn4 = sb("dn4", [128, 4, 2])
        ybt = [sb("ybt%d" % i, [128, 512], BF16) for i in range(2)]
        Cf4 = sb("Cf4", [128, 4, 129])
        Cb4 = sb("Cb4", [128, 4, 129], BF16)
        PC = ps("PC", [128, 512])
        PR = ps("PR", [128, 512])
        PS = ps("PS", [128, 512])
        pD = [ps("pD%d" % i, [128, 512]) for i in range(2)]
        pOb = [ps("pOb%d" % i, [128, 512]) for i in range(2)]
        pk = ps("pk", [128, 1024], BF16)

        for jj in range(4):
            P.dma("sp", cw[:, jj, :], conv_w[jj, :].rearrange("(c p) -> p c", p=128), writes=[("cw", jj)])
        cwk = [("cw", jj) for jj in range(4)]
        P.dma("sp", cb[:], conv_b.rearrange("(c p) -> p c", p=128), writes=["cb"])
        P.dma("sp", ib[:], bass.AP(i_bias.tensor, 0, [[0, 128], [1, 4]]), writes=["ib"])
        P.dma("sp", fb[:], bass.AP(f_bias.tensor, 0, [[0, 128], [1, 4]]), writes=["fb"])
        P.dma("sp", kv[:], kvalid_tm[:, :], writes=["kv"])
        P.dma("sp", tri[:], tri_in[:, :], writes=["tri"])
        P.dma("sp", maskT[:], mask_in[:, :], writes=["maskT"])
        P.op("dve", lambda e: e.memset(Cf4[:], 0.0), writes=["Cf4"])
        P.op("dve", lambda e: e.memset(Cb4[:], 0.0), writes=["Cb4"])
        for i in range(2):
            P.op("pool", lambda e, i=i: e.memset(vaug[i][:], 1.0), writes=[("vaug", i, ii) for ii in range(4)])

        def front(b):
            pb = b % 2
            c0 = 16 + b * 512 - 3
            rows = slice(b * 512, (b + 1) * 512)
            P.dma("sp", qkT[pb][:], projT[256:1280, c0:c0 + 515].rearrange("(c p) n -> p c n", p=128), writes=[("qkT", pb)])
            P.dma("sp", gl[pb][:], proj[rows, 2840:2848].rearrange("(i p) c -> p i c", p=128), writes=[("gl", pb)])
            P.dma("sp", mv[:], proj[rows, 2328:2840].rearrange("(i p) c -> p i c", p=128), writes=["mv"])
            for oi in range(2):
                t = b * 4 + 2 * oi + 1
                P.dma("sp", mo[pb][:, oi, :], proj[t * 128:(t + 1) * 128, 2848:3360], writes=[("mo", pb)])
            for c in range(8):
                eng = "dve"
                P.op("act", lambda e, c=c, pb=pb: e.activation(out=acc[pb][:, c, :], in_=qkT[pb][:, c, 0:512], func=AF.Identity, scale=cw[:, 0, c:c + 1], bias=cb[:, c:c + 1]),
                     reads=[("qkT", pb), "cb"] + cwk, writes=[("acc", pb, c)])
                for jj in range(1, 4):
                    if eng == "dve":
                        P.op(eng, lambda e, c=c, pb=pb, jj=jj: e.scalar_tensor_tensor(out=acc[pb][:, c, :], in0=qkT[pb][:, c, jj:jj + 512], scalar=cw[:, jj, c:c + 1], in1=acc[pb][:, c, :], op0=ALU.mult, op1=ALU.add),
                             reads=[("qkT", pb), ("acc", pb, c)] + cwk, writes=[("acc", pb, c)])
                    else:
                        P.op(eng, lambda e, c=c, pb=pb, jj=jj: e.tensor_scalar(out=ctmp[:], in0=qkT[pb][:, c, jj:jj + 512], scalar1=cw[:, jj, c:c + 1], scalar2=None, op0=ALU.mult),
                             reads=[("qkT", pb)] + cwk, writes=["ctmp"])
                        P.op(eng, lambda e, c=c, pb=pb: e.tensor_tensor(out=acc[pb][:, c, :], in0=acc[pb][:, c, :], in1=ctmp[:], op=ALU.add),
                             reads=["ctmp", ("acc", pb, c)], writes=[("acc", pb, c)])
            acck = [("acc", pb, c) for c in range(8)]
            for half in range(2):
                hs = slice(half * 4, half * 4 + 4)
                hk = acck[half * 4:half * 4 + 4]
                P.op("act", lambda e, pb=pb, hs=hs: e.activation(out=sg[:, hs, :], in_=acc[pb][:, hs, :], func=AF.Exp, scale=-1.0), reads=hk, writes=[("sg", half)])
                P.op("act", lambda e, hs=hs: e.activation(out=sg[:, hs, :], in_=sg[:, hs, :], func=AF.Ln, bias=1.0), reads=[("sg", half)], writes=[("sg", half)])
                P.op("act", lambda e, hs=hs: e.activation(out=sg[:, hs, :], in_=sg[:, hs, :], func=AF.Exp, scale=-1.0), reads=[("sg", half)], writes=[("sg", half)])
                P.op("dve", lambda e, pb=pb, hs=hs: e.tensor_tensor(out=acc[pb][:, hs, :], in0=acc[pb][:, hs, :], in1=sg[:, hs, :], op=ALU.mult), reads=hk + [("sg", half)], writes=hk)
            P.op("dve", lambda e, pb=pb: e.tensor_tensor(out=ga[pb][:, 2], in0=gl[pb][:, :, 4:8], in1=_bc(fb[:].rearrange("p (o h) -> p o h", o=1), [128, 4, 4]), op=ALU.add), reads=[("gl", pb), "fb"], writes=[("ga2", pb)])
            P.op("act", lambda e, pb=pb: e.activation(out=ga[pb][:, 2], in_=ga[pb][:, 2], func=AF.Exp, scale=-1.0), reads=[("ga2", pb)], writes=[("ga2", pb)])
            P.op("act", lambda e, pb=pb: e.activation(out=ga[pb][:, 0], in_=ga[pb][:, 2], func=AF.Ln, bias=1.0), reads=[("ga2", pb)], writes=[("ga0", pb)])
            P.op("dve", lambda e, pb=pb: e.tensor_tensor(out=ga[pb][:, 1], in0=gl[pb][:, :, 0:4], in1=_bc(ib[:].rearrange("p (o h) -> p o h", o=1), [128, 4, 4]), op=ALU.add), reads=[("gl", pb), "ib"], writes=[("ga1", pb)])
            P.op("dve", lambda e, pb=pb, b=b: e.tensor_tensor(out=ga[pb][:, 1], in0=ga[pb][:, 1], in1=_bc(kv[:, 4 * b:4 * b + 4].rearrange("p (i o) -> p i o", o=1), [128, 4, 4]), op=ALU.add), reads=[("ga1", pb), "kv"], writes=[("ga1", pb)])
            for i in range(4):
                P.op("act", lambda e, pb=pb, i=i: e.activation(out=vaug[pb][:, i, :, 0:128], in_=mv[:, i, :].rearrange("p (h d) -> p h d", d=128), func=AF.Copy), reads=["mv"], writes=[("vaug", pb, i)])
            P.op("act", lambda e, pb=pb: e.activation(out=mo[pb][:], in_=mo[pb][:], func=AF.Exp, scale=-1.0), reads=[("mo", pb)], writes=[("mo", pb)])
            P.op("act", lambda e, pb=pb: e.activation(out=mo[pb][:], in_=mo[pb][:], func=AF.Ln, bias=1.0), reads=[("mo", pb)], writes=[("mo", pb)])
            P.op("act", lambda e, pb=pb: e.activation(out=mo[pb][:], in_=mo[pb][:], func=AF.Exp, scale=-1.0), reads=[("mo", pb)], writes=[("mo", pb)])

        s_ = 128.0 ** -0.5

        def levels(t):
            b, i = t // 4, t % 4
            pb = b % 2
            tp = t % 2
            own = (t % 2 == 1)
            j = t // 2
            oi = i // 2
            yp = j % 2
            ts_ = slice(i * 128, (i + 1) * 128)
            A = {}

            def A1():
                for h in range(4):
                    a_col = ga[pb][:, 0, i, h:h + 1]
                    li_col = ga[pb][:, 1, i, h:h + 1]
                    hs = slice(h * 128, (h + 1) * 128)
                    P.op("pe", lambda e, a_col=a_col, hs=hs: e.matmul(PC[:, hs], lhsT=_bc(a_col, [128, 128]), rhs=tri[:], start=True, stop=True), reads=[("ga0", pb), "tri"], writes=["PC"])
                    P.op("pe", lambda e, a_col=a_col, hs=hs: e.matmul(PR[:, hs], lhsT=_bc(a_col, [128, 128]), rhs=tri[:], start=True, stop=False), reads=[("ga0", pb), "tri"], writes=["PR"])
                    P.op("pe", lambda e, li_col=li_col, hs=hs: e.matmul(PR[:, hs], lhsT=_bc(li_col, [128, 128]), rhs=identf[:], start=False, stop=True), reads=[("ga1", pb), "identf"], writes=["PR"])

            def A2():
                P.op("act", lambda e: e.activation(out=U4[tp][:].rearrange("p h n -> p (h n)"), in_=PC[:, :], func=AF.Exp, scale=-1.0), reads=["PC"], writes=[("U4", tp)])
                P.op("act", lambda e: e.activation(out=R4[tp][:].rearrange("p h n -> p (h n)"), in_=PR[:, :], func=AF.Exp), reads=["PR"], writes=[("R4", tp)])

            def A3():
                P.op("dve", lambda e: e.scalar_tensor_tensor(out=qp4[tp][:], in0=acc[pb][:, 0:4, ts_], scalar=s_, in1=U4[tp][:], op0=ALU.mult, op1=ALU.mult),
                     reads=[("acc", pb, c) for c in range(4)] + [("U4", tp)], writes=[("qp4", tp)])
                P.op("dve", lambda e: e.tensor_tensor(out=kpT4[tp][:], in0=acc[pb][:, 4:8, ts_], in1=R4[tp][:], op=ALU.mult),
                     reads=[("acc", pb, c) for c in range(4, 8)] + [("R4", tp)], writes=[("kpT4", tp)])

            def A4():
                for h in range(4):
                    P.op("pe", lambda e, h=h: e.transpose(out=pk[:, h * 128:(h + 1) * 128], in_=kpT4[tp][:, h, :], identity=ident[:]), reads=[("kpT4", tp), "ident"], writes=["pk"])

            def A5():
                P.op("act", lambda e: e.activation(out=kp4[tp][:].rearrange("p h n -> p (h n)"), in_=pk[:, 0:512], func=AF.Copy), reads=["pk"], writes=[("kp4", tp)])

            def B6():
                for h in range(4):
                    pd = pD[h // 2][:, (h % 2) * 129:(h % 2) * 129 + 129]
                    P.op("pe", lambda e, h=h, pd=pd: e.matmul(pd, lhsT=kp4[tp][:, h, :], rhs=vaug[pb][:, i, h, :], start=True, stop=True), reads=[("kp4", tp), ("vaug", pb, i)], writes=[("pD", h // 2)])
                if own:
                    for h in range(4):
                        P.op("pe", lambda e, h=h: e.matmul(PS[:, h * 128:(h + 1) * 128], lhsT=kpT4[tp][:, h, :], rhs=qp4[tp][:, h, :], start=True, stop=True), reads=[("kpT4", tp), ("qp4", tp)], writes=["PS"])

            def B7():
                if own:
                    P.op("dve", lambda e: e.tensor_tensor(out=wT4[tp][:], in0=PS[:, :].rearrange("p (h n) -> p h n", n=128), in1=_bc(maskT[:].rearrange("p (o n) -> p o n", o=1), [128, 4, 128]), op=ALU.mult), reads=["PS", "maskT"], writes=[("wT4", tp)])

            def B8():
                if own:
                    for h in range(4):
                        pO = pOb[h // 2][:, (h % 2) * 129:(h % 2) * 129 + 129]
                        P.op("pe", lambda e, h=h, pO=pO: e.matmul(pO, lhsT=wT4[tp][:, h, :], rhs=vaug[pb][:, i, h, :], start=True, stop=False), reads=[("wT4", tp), ("vaug", pb, i)], writes=[("pO", h // 2)])
                        P.op("pe", lambda e, h=h, pO=pO: e.matmul(pO, lhsT=qp4[tp][:, h, :], rhs=Cb4[:, h, :], start=False, stop=True), reads=[("qp4", tp), "Cb4"], writes=[("pO", h // 2)])

            def B9():
                for k in range(2):
                    P.op("dve", lambda e, k=k: e.tensor_tensor(out=Cf4[:, 2 * k:2 * k + 2, :], in0=pD[k][:, 0:258].rearrange("p (h e) -> p h e", e=129), in1=Cf4[:, 2 * k:2 * k + 2, :], op=ALU.add), reads=[("pD", k), "Cf4"], writes=["Cf4"])
                P.op("dve", lambda e: e.tensor_tensor(out=Cf4[:], in0=Cf4[:], in1=_bc(U4[tp][:, :, 127:128], [128, 4, 129]), op=ALU.mult), reads=["Cf4", ("U4", tp)], writes=["Cf4"])
                if own:
                    for k in range(2):
                        den = pOb[k][:, 0:258].rearrange("p (h e) -> p h e", e=129)[:, :, 128:129]
                        dk_ = dn4[:, 2 * k:2 * k + 2, :]
                        P.op("dve", lambda e, den=den, dk_=dk_: e.tensor_scalar(out=dk_[:, :, 1:2], in0=den, scalar1=-1.0, scalar2=1.0, op0=ALU.mult, op1=ALU.max), reads=[("pO", k)], writes=[("dn1", k)])
                        P.op("dve", lambda e, den=den, dk_=dk_: e.scalar_tensor_tensor(out=dk_[:, :, 0:1], in0=den, scalar=1.0, in1=dk_[:, :, 1:2], op0=ALU.max, op1=ALU.max), reads=[("pO", k), ("dn1", k)], writes=[("dn0", k)])
                        P.op("dve", lambda e, dk_=dk_: e.reciprocal(out=dk_[:, :, 1:2], in_=dk_[:, :, 0:1]), reads=[("dn0", k)], writes=[("dn1", k)])
                    for h in range(4):
                        pO = pOb[h // 2][:, (h % 2) * 129:(h % 2) * 129 + 128]
                        P.op("dve", lambda e, h=h, pO=pO: e.scalar_tensor_tensor(out=ybt[yp][:, h * 128:(h + 1) * 128], in0=pO, scalar=dn4[:, h, 1:2], in1=mo[pb][:, oi, h * 128:(h + 1) * 128], op0=ALU.mult, op1=ALU.mult),
                             reads=[("pO", h // 2), ("dn1", h // 2), ("mo", pb)], writes=[("ybt", yp, h)])
                P.op("act", lambda e: e.activation(out=Cb4[:].rearrange("p h e -> p (h e)"), in_=Cf4[:].rearrange("p h e -> p (h e)"), func=AF.Copy), reads=["Cf4"], writes=["Cb4"])
                if own:
                    P.dma("sp", yb[j * 128:(j + 1) * 128, :], ybt[yp][:], reads=[("ybt", yp, h) for h in range(4)], writes=[("yb", j)])
            return [A1, A2, A3, A4, A5], [B6, B7, B8, B9]

        front(0)
        if NB > 1:
            front(1)
        lv = {0: levels(0)}
        for f in lv[0][0]:
            f()
        for t in range(NT):
            if t % 4 == 0 and t // 4 + 2 < NB and t > 0:
                pass
            if t + 1 < NT:
                lv[t + 1] = levels(t + 1)
                An = lv[t + 1][0]
            else:
                An = []
            Bc = lv[t][1]
            for k in range(5):
                if k < len(An):
                    An[k]()
                if k < len(Bc):
                    Bc[k]()
            if t % 4 == 3:
                nb_ = t // 4 + 2
                if nb_ < NB:
                    front(nb_)
            del lv[t]
        P.flush()


def stage5(nc, P, NT, x_loc, ya, yb, w_in, norm1_g, norm2_g, w_pa, w_pb, w_out, w_ff1, w_ff2, out, ident):
    NJ = NT // 2
    NBK = (NJ + 3) // 4
    with contextlib.ExitStack() as es:
        def sb(name, shape, dt=F32):
            return es.enter_context(nc.sbuf_tensor("s5_" + name, shape, dt))

        def ps(name, shape, dt=F32):
            return es.enter_context(nc.psum_tensor("s5_" + name, shape, dt))
        Wg = sb("Wg", [128, 8, 2048], BF16)
        PA = sb("PA", [128, 4, 1024], BF16)
        PB = sb("PB", [128, 4, 1024], BF16)
        Wo = sb("Wo", [128, 8, 1024], BF16)
        g1T = sb("g1T", [128, 8])
        g2T = sb("g2T", [128, 8])
        xk = [sb("xk%d" % i, [128, D]) for i in range(4)]
        junk = sb("junk", [128, D], BF16)
        xn = [sb("xn%d" % i, [128, D], BF16) for i in range(2)]
        st = [sb("st%d" % i, [128, 4]) for i in range(2)]
        yat = [sb("yat%d" % i, [128, 512], BF16) for i in range(2)]
        ybt = [sb("ybt%d" % i, [128, 512], BF16) for i in range(2)]
        hT = sb("hT", [128, 8, 512], BF16)
        yaT = sb("yaT", [128, 4, 512], BF16)
        ybT = sb("ybT", [128, 4, 512], BF16)
        mixT = sb("mixT", [128, 8, 512], BF16)
        sa = [sb("sa%d" % i, [128, 512]) for i in range(2)]
        sbb = [sb("sbb%d" % i, [128, 512]) for i in range(2)]
        W1c = [sb("W1c%d" % i, [128, 8, 512], BF16) for i in range(2)]
        W2c = [sb("W2c%d" % i, [128, 4, 1024], BF16) for i in range(2)]
        rl = [sb("rl%d" % i, [128, 512]) for i in range(2)]
        aT = [sb("aT%d" % i, [128, 4, 512], BF16) for i in range(2)]
        pT = [ps("pT%d" % i, [128, 8, 128], BF16) for i in range(2)]
        pq = [ps("pq%d" % i, [128, 512]) for i in range(6)]
        pqi = [0]

        def nextp():
            i = pqi[0] % 6
            pqi[0] += 1
            return pq[i], ("pq", i)

        for k in range(8):
            P.dma("pool", Wg[:, k, :], w_in[k * 128:(k + 1) * 128, NRES:DPROJ], writes=[("Wg", k)])
            P.dma("pool", Wo[:, k, :], w_out[k * 128:(k + 1) * 128, :], writes=[("Wo", k)])
        for k in range(4):
            P.dma("pool", PA[:, k, :], w_pa[k * 128:(k + 1) * 128, :], writes=[("PA", k)])
            P.dma("pool", PB[:, k, :], w_pb[k * 128:(k + 1) * 128, :], writes=[("PB", k)])
        P.dma("sp", g1T[:], norm1_g.rearrange("(c p) -> p c", p=128), writes=["g1T"])
        P.dma("sp", g2T[:], norm2_g.rearrange("(c p) -> p c", p=128), writes=["g2T"])
        Wgk = [("Wg", k) for k in range(8)]
        Wok = [("Wo", k) for k in range(8)]

        def norm_T(src, srckey, gT, gkey, dstT, dkey, i, q):
            P.op("act", lambda e: e.activation(out=junk[:], in_=src[:], func=AF.Square, accum_out=st[q][:, 0:1]), reads=[srckey], writes=["junk", ("st0", q)])
            P.op("act", lambda e: e.activation(out=st[q][:, 1:2], in_=st[q][:, 0:1], func=AF.Ln, scale=1.0 / D, bias=1e-6), reads=[("st0", q)], writes=[("st1", q)])
            P.op("act", lambda e: e.activation(out=st[q][:, 2:3], in_=st[q][:, 1:2], func=AF.Exp, scale=-0.5), reads=[("st1", q)], writes=[("st2", q)])
            P.op("dve", lambda e: e.tensor_scalar(out=xn[q][:], in0=src[:], scalar1=st[q][:, 2:3], scalar2=None, op0=ALU.mult), reads=[srckey, ("st2", q)], writes=[("xn", q)])
            for c in range(8):
                P.op("pe", lambda e, c=c: e.transpose(out=pT[q][:, c, :], in_=xn[q][:, c * 128:(c + 1) * 128], identity=ident[:]), reads=[("xn", q), "ident"], writes=[("pT", q)])
            P.op("dve", lambda e: e.tensor_tensor(out=dstT[:, :, i * 128:(i + 1) * 128], in0=pT[q][:], in1=_bc(gT[:].rearrange("p (c o) -> p c o", o=1), [128, 8, 128]), op=ALU.mult),
                 reads=[("pT", q), gkey], writes=[(dkey, i)])

        for bk in range(NBK):
            nti = min(4, NJ - bk * 4)
            NTK = nti * 128
            for i in range(nti):
                j = bk * 4 + i
                t = 2 * j + 1
                q = i % 2
                P.dma("sp", xk[i][:], x_loc[t * 128:(t + 1) * 128, :], writes=[("xk", i)])
                P.dma("sp", yat[q][:], ya[j * 128:(j + 1) * 128, :], writes=[("yat", q)])
                P.dma("sp", ybt[q][:], yb[j * 128:(j + 1) * 128, :], writes=[("ybt", q)])
                norm_T(xk[i], ("xk", i), g1T, "g1T", hT, "hT", i, q)
                for (srct, skey, dst, dkey) in ((yat[q], ("yat", q), yaT, "yaT"), (ybt[q], ("ybt", q), ybT, "ybT")):
                    for c in range(4):
                        P.op("pe", lambda e, c=c, srct=srct: e.transpose(out=pT[q][:, c, :], in_=srct[:, c * 128:(c + 1) * 128], identity=ident[:]), reads=[skey, "ident"], writes=[("pT", q)])
                    P.op("act", lambda e, dst=dst, i=i: e.activation(out=dst[:, :, i * 128:(i + 1) * 128], in_=pT[q][:, 0:4, :], func=AF.Copy), reads=[("pT", q)], writes=[(dkey, i)])
            hTk = [("hT", i) for i in range(nti)]
            yaTk = [("yaT", i) for i in range(nti)]
            ybTk = [("ybT", i) for i in range(nti)]
            for f in range(8):
                q = f % 2
                pga, kga = nextp()
                for k in range(8):
                    P.op("pe", lambda e, pga=pga, k=k, f=f: e.matmul(pga[:, 0:NTK], lhsT=Wg[:, k, f * 128:(f + 1) * 128], rhs=hT[:, k, 0:NTK], start=(k == 0), stop=(k == 7)), reads=hTk + Wgk, writes=[kga])
                pgb, kgb = nextp()
                for k in range(8):
                    P.op("pe", lambda e, pgb=pgb, k=k, f=f: e.matmul(pgb[:, 0:NTK], lhsT=Wg[:, k, 1024 + f * 128:1024 + (f + 1) * 128], rhs=hT[:, k, 0:NTK], start=(k == 0), stop=(k == 7)), reads=hTk + Wgk, writes=[kgb])
                P.op("act", lambda e, pga=pga, q=q: e.activation(out=sa[q][:, 0:NTK], in_=pga[:, 0:NTK], func=AF.Exp, scale=-1.0), reads=[kga], writes=[("sa", q)])
                P.op("act", lambda e, pgb=pgb, q=q: e.activation(out=sbb[q][:, 0:NTK], in_=pgb[:, 0:NTK], func=AF.Exp, scale=-1.0), reads=[kgb], writes=[("sbb", q)])
                pa_, kpa = nextp()
                for k in range(4):
                    P.op("pe", lambda e, pa_=pa_, k=k, f=f: e.matmul(pa_[:, 0:NTK], lhsT=PA[:, k, f * 128:(f + 1) * 128], rhs=yaT[:, k, 0:NTK], start=(k == 0), stop=(k == 3)), reads=yaTk + [("PA", kk) for kk in range(4)], writes=[kpa])
                pb_, kpb = nextp()
                for k in range(4):
                    P.op("pe", lambda e, pb_=pb_, k=k, f=f: e.matmul(pb_[:, 0:NTK], lhsT=PB[:, k, f * 128:(f + 1) * 128], rhs=ybT[:, k, 0:NTK], start=(k == 0), stop=(k == 3)), reads=ybTk + [("PB", kk) for kk in range(4)], writes=[kpb])
                for (sx, skey) in ((sa[q], ("sa", q)), (sbb[q], ("sbb", q))):
                    P.op("act", lambda e, sx=sx: e.activation(out=sx[:, 0:NTK], in_=sx[:, 0:NTK], func=AF.Ln, bias=1.0), reads=[skey], writes=[skey])
                    P.op("act", lambda e, sx=sx: e.activation(out=sx[:, 0:NTK], in_=sx[:, 0:NTK], func=AF.Exp, scale=-1.0), reads=[skey], writes=[skey])
                P.op("dve", lambda e, q=q, pa_=pa_: e.tensor_tensor(out=sa[q][:, 0:NTK], in0=pa_[:, 0:NTK], in1=sa[q][:, 0:NTK], op=ALU.mult), reads=[kpa, ("sa", q)], writes=[("sa", q)])
                P.op("dve", lambda e, q=q, pb_=pb_: e.tensor_tensor(out=sbb[q][:, 0:NTK], in0=pb_[:, 0:NTK], in1=sbb[q][:, 0:NTK], op=ALU.mult), reads=[kpb, ("sbb", q)], writes=[("sbb", q)])
                P.op("dve", lambda e, q=q, f=f: e.tensor_tensor(out=mixT[:, f, 0:NTK], in0=sa[q][:, 0:NTK], in1=sbb[q][:, 0:NTK], op=ALU.add), reads=[("sa", q), ("sbb", q)], writes=[("mixT", f)])
            mixk = [("mixT", f) for f in range(8)]
            def wout(i):
                for hh in range(2):
                    po, kpo = nextp()
                    for f in range(8):
                        P.op("pe", lambda e, po=po, f=f, i=i, hh=hh: e.matmul(po[:, :], lhsT=mixT[:, f, i * 128:(i + 1) * 128], rhs=Wo[:, f, hh * 512:(hh + 1) * 512], start=(f == 0), stop=(f == 7)), reads=mixk + Wok, writes=[kpo])
                    P.op("dve", lambda e, po=po, i=i, hh=hh: e.tensor_tensor(out=xk[i][:, hh * 512:(hh + 1) * 512], in0=po[:, :], in1=xk[i][:, hh * 512:(hh + 1) * 512], op=ALU.add), reads=[kpo, ("xk", i)], writes=[("xk", i)])
            wout(0)
            for i in range(nti):
                if i + 1 < nti:
                    wout(i + 1)
                norm_T(xk[i], ("xk", i), g2T, "g2T", hT, "hT", i, i % 2)
            for fc in range(8):
                wq = fc % 2
                P.dma("pool", W1c[wq][:], w_ff1[:, fc * 512:(fc + 1) * 512].rearrange("(k p) n -> p k n", p=128), writes=[("W1c", wq)])
                P.dma("pool", W2c[wq][:], w_ff2[fc * 512:(fc + 1) * 512, :].rearrange("(k p) n -> p k n", p=128), writes=[("W2c", wq)])
                for m in range(4):
                    pf, kpf = nextp()
                    for k in range(8):
                        P.op("pe", lambda e, pf=pf, k=k, m=m, wq=wq: e.matmul(pf[:, 0:NTK], lhsT=W1c[wq][:, k, m * 128:(m + 1) * 128], rhs=hT[:, k, 0:NTK], start=(k == 0), stop=(k == 7)), reads=hTk + [("W1c", wq)], writes=[kpf])
                    rq = m % 2
                    P.op("act", lambda e, pf=pf, rq=rq: e.activation(out=rl[rq][:, 0:NTK], in_=pf[:, 0:NTK], func=AF.Relu), reads=[kpf], writes=[("rl", rq)])
                    P.op("dve", lambda e, rq=rq, m=m, wq=wq: e.tensor_tensor(out=aT[wq][:, m, 0:NTK], in0=rl[rq][:, 0:NTK], in1=rl[rq][:, 0:NTK], op=ALU.mult), reads=[("rl", rq)], writes=[("aT", wq, m)])
                aTk = [("aT", wq, m) for m in range(4)]
                for i in range(nti):
                    for hh in range(2):
                        po, kpo = nextp()
                        for m in range(4):
                            P.op("pe", lambda e, po=po, m=m, i=i, hh=hh, wq=wq: e.matmul(po[:, :], lhsT=aT[wq][:, m, i * 128:(i + 1) * 128], rhs=W2c[wq][:, m, hh * 512:(hh + 1) * 512], start=(m == 0), stop=(m == 3)), reads=aTk + [("W2c", wq)], writes=[kpo])
                        P.op("dve", lambda e, po=po, i=i, hh=hh: e.tensor_tensor(out=xk[i][:, hh * 512:(hh + 1) * 512], in0=po[:, :], in1=xk[i][:, hh * 512:(hh + 1) * 512], op=ALU.add), reads=[kpo, ("xk", i)], writes=[("xk", i)])
            for i in range(nti):
                j = bk * 4 + i
                P.dma("sp", out[j * 128:(j + 1) * 128, :], xk[i][:], reads=[("xk", i)], writes=[("out", j)])
        P.flush()


def stage3(nc, P, NT, projT, cw1, cb1, cw2, cb2, cpos, k_gain, cvalid, kc_scr, vc_scr, ident):
    NTOK = NT * 128
    NCB = 8 * NT - 1
    CT = (NCB + 127) // 128
    with contextlib.ExitStack() as es:
        def sb(name, shape, dt=F32):
            return es.enter_context(nc.sbuf_tensor("s3_" + name, shape, dt))

        def ps(name, shape, dt=F32):
            return es.enter_context(nc.psum_tensor("s3_" + name, shape, dt))
        xT = sb("xT", [64, NTOK + 16], BF16)
        w1 = sb("w1", [64, 32, 256], BF16)
        posT = sb("posT", [64, 32], BF16)
        b1 = sb("b1", [128, 2])
        b1e = sb("b1e", [128, 2])
        w2 = sb("w2", [128, 2, 64], BF16)
        b2 = sb("b2", [128, 64])
        gk = sb("gk", [128, 64])
        hx = [sb("hx%d" % i, [128, 512]) for i in range(2)]
        hu = [sb("hu%d" % i, [128, 512]) for i in range(2)]
        hidT = sb("hidT", [128, 2, CT * 128], BF16)
        o = [sb("o%d" % i, [128, 64]) for i in range(2)]
        junk = sb("junk", [128, 64])
        st = [sb("st%d" % i, [128, 4]) for i in range(2)]
        kn = [sb("kn%d" % i, [128, 64], BF16) for i in range(2)]
        kct = [sb("kct%d" % i, [65, 128], BF16) for i in range(2)]
        vct = [sb("vct%d" % i, [128, 65], BF16) for i in range(2)]
        pb = ps("pb", [128, 512])
        ph = [ps("ph%d" % i, [128, 512]) for i in range(2)]
        po = [ps("po%d" % i, [128, 512]) for i in range(2)]
        pt = [ps("pt%d" % i, [128, 1024], BF16) for i in range(2)]
        P.dma("sp", gk[:], bass.AP(k_gain.tensor, k_gain.offset, [[0, 128], [1, 64]]), writes=["gk"])
        P.op("dve", lambda e: e.memset(hidT[:], 0.0), writes=["hidT"])
        for i in range(2):
            P.op("pool", lambda e, i=i: e.memset(vct[i][:], 1.0), writes=[("vct", i)])
        it = 0
        for kind in range(2):
            for g in range(2):
                r0 = kind * 128 + g * 64
                P.dma("pool", xT[:, :], projT[r0:r0 + 64, 16:16 + NTOK + 16] if False else projT[r0:r0 + 64, 0:NTOK + 16], writes=["xT"])
                if g == 0:
                    P.dma("pool", w1[:], cw1[kind].rearrange("(l d) h -> d l h", d=64), writes=["w1"])
                    for l in range(32):
                        pass
                    P.dma("pool", posT[:], cpos[kind].rearrange("l d -> d l"), writes=["posT"])
                    P.dma("sp", b1[:], cb1[kind].rearrange("(c p) -> p c", p=128), writes=["b1"])
                    P.dma("pool", w2[:], cw2[kind].rearrange("(c p) d -> p c d", p=128), writes=["w2"])
                    P.dma("sp", b2[:], bass.AP(cb2[kind].tensor, cb2[kind].offset, [[0, 128], [1, 64]]), writes=["b2"])
                    for c in range(2):
                        for l in range(32):
                            P.op("pe", lambda e, c=c, l=l: e.matmul(pb[:, c:c + 1], lhsT=w1[:, l, c * 128:(c + 1) * 128], rhs=posT[:, l:l + 1], start=(l == 0), stop=(l == 31)), reads=["w1", "posT"], writes=["pb"])
                    P.op("dve", lambda e: e.tensor_tensor(out=b1e[:], in0=pb[:, 0:2], in1=b1[:], op=ALU.add), reads=["pb", "b1"], writes=["b1e"])
                xv = xT[:, :].rearrange("p (n r) -> p n r", r=16)
                for n0 in range(0, NCB, 512):
                    nn = min(512, NCB - n0)
                    for c in range(2):
                        q = it % 2
                        it += 1
                        for l in range(32):
                            if l < 16:
                                rhs = xv[:, 1 + n0:1 + n0 + nn, l]
                            else:
                                rhs = xv[:, 2 + n0:2 + n0 + nn, l - 16]
                            P.op("pe", lambda e, c=c, l=l, rhs=rhs, q=q: e.matmul(ph[q][:, 0:nn], lhsT=w1[:, l, c * 128:(c + 1) * 128], rhs=rhs, start=(l == 0), stop=(l == 31)), reads=["w1", "xT"], writes=[("ph", q)])
                        P.op("dve", lambda e, c=c, q=q: e.tensor_scalar(out=hx[q][:, 0:nn], in0=ph[q][:, 0:nn], scalar1=b1e[:, c:c + 1], scalar2=None, op0=ALU.add), reads=[("ph", q), "b1e"], writes=[("hx", q)])
                        P.op("dve", lambda e, q=q: e.tensor_tensor(out=hu[q][:, 0:nn], in0=hx[q][:, 0:nn], in1=hx[q][:, 0:nn], op=ALU.mult), reads=[("hx", q)], writes=[("hu", q)])
                        P.op("dve", lambda e, q=q: e.tensor_scalar(out=hu[q][:, 0:nn], in0=hu[q][:, 0:nn], scalar1=0.044715, scalar2=1.0, op0=ALU.mult, op1=ALU.add), reads=[("hu", q)], writes=[("hu", q)])
                        P.op("dve", lambda e, q=q: e.tensor_tensor(out=hu[q][:, 0:nn], in0=hu[q][:, 0:nn], in1=hx[q][:, 0:nn], op=ALU.mult), reads=[("hu", q), ("hx", q)], writes=[("hu", q)])
                        P.op("act", lambda e, q=q: e.activation(out=hu[q][:, 0:nn], in_=hu[q][:, 0:nn], func=AF.Exp, scale=-1.5957691216057308), reads=[("hu", q)], writes=[("hu", q)])
                        P.op("dve", lambda e, q=q: e.tensor_scalar(out=hu[q][:, 0:nn], in0=hu[q][:, 0:nn], scalar1=1.0, scalar2=None, op0=ALU.add), reads=[("hu", q)], writes=[("hu", q)])
                        P.op("dve", lambda e, q=q: e.reciprocal(out=hu[q][:, 0:nn], in_=hu[q][:, 0:nn]), reads=[("hu", q)], writes=[("hu", q)])
                        P.op("dve", lambda e, q=q, c=c, n0=n0: e.tensor_tensor(out=hidT[:, c, n0:n0 + nn], in0=hx[q][:, 0:nn], in1=hu[q][:, 0:nn], op=ALU.mult), reads=[("hu", q), ("hx", q)], writes=["hidT"])
                for ct in range(CT):
                    q = ct % 2
                    for c in range(2):
                        P.op("pe", lambda e, c=c, ct=ct, q=q: e.matmul(po[q][:, 0:64], lhsT=hidT[:, c, ct * 128:(ct + 1) * 128], rhs=w2[:, c, :], start=(c == 0), stop=(c == 1)), reads=["hidT", "w2"], writes=[("po", q)])
                    P.op("dve", lambda e, q=q: e.tensor_tensor(out=o[q][:], in0=po[q][:, 0:64], in1=b2[:], op=ALU.add), reads=[("po", q), "b2"], writes=[("o", q)])
                    if kind == 0:
                        P.op("act", lambda e, q=q: e.activation(out=junk[:], in_=o[q][:], func=AF.Square, accum_out=st[q][:, 0:1]), reads=[("o", q)], writes=["junk", ("st0", q)])
                        P.op("act", lambda e, q=q: e.activation(out=st[q][:, 1:2], in_=st[q][:, 0:1], func=AF.Ln, scale=1.0 / 64, bias=1e-6), reads=[("st0", q)], writes=[("st1", q)])
                        P.op("act", lambda e, q=q: e.activation(out=st[q][:, 2:3], in_=st[q][:, 1:2], func=AF.Exp, scale=-0.5), reads=[("st1", q)], writes=[("st2", q)])
                        P.op("dve", lambda e, q=q: e.scalar_tensor_tensor(out=kn[q][:], in0=o[q][:], scalar=st[q][:, 2:3], in1=gk[:], op0=ALU.mult, op1=ALU.mult), reads=[("o", q), ("st2", q), "gk"], writes=[("kn", q)])
                        P.op("pe", lambda e, q=q: e.transpose(out=pt[q][0:64, 0:128], in_=kn[q][:], identity=ident[:]), reads=[("kn", q), "ident"], writes=[("pt", q)])
                        P.op("act", lambda e, q=q: e.activation(out=kct[q][0:64, :], in_=pt[q][0:64, 0:128], func=AF.Copy), reads=[("pt", q)], writes=[("kct", q)])
                        P.dma("pool", kct[q][64:65, :], cvalid[0:1, ct * 128:(ct + 1) * 128], reads=[("kct", q)], writes=[("kct", q)])
                        P.dma("sp", kc_scr[g, :, ct * 128:(ct + 1) * 128], kct[q][:], reads=[("kct", q)], writes=[("kc_scr", g, ct)])
                    else:
                        P.op("dve", lambda e, q=q: e.tensor_copy(out=vct[q][:, 0:64], in_=o[q][:]), reads=[("o", q)], writes=[("vct", q)])
                        P.dma("sp", vc_scr[g, ct * 128:(ct + 1) * 128, :], vct[q][:], reads=[("vct", q)], writes=[("vc_scr", g, ct)])
        P.flush()


def _brow(ap, n):
    return bass.AP(ap.tensor, ap.offset, [[0, 128], [1, n]])


def stage4(nc, P, NT, proj, kc_scr, vc_scr, q_gain, k_gain, kvalid_row, f0row, rel31, Bg, Bcg, Mc, causal_in, w4_in,
           c2s_in, onehot_in, vmB_in, amB_in, fzB_in, ya, ident, identf, bc_scr):
    NJ = NT // 2
    NTOK = NT * 128
    NCB = 8 * NT - 1
    CT = (NCB + 127) // 128
    with contextlib.ExitStack() as es:
        def sb(name, shape, dt=F32):
            return es.enter_context(nc.sbuf_tensor("s4_" + name, shape, dt))

        def ps(name, shape, dt=F32):
            return es.enter_context(nc.psum_tensor("s4_" + name, shape, dt))
        KsT = sb("KsT", [65, 2, NTOK], BF16)
        KwT = sb("KwT", [65, 2, NTOK], BF16)
        Vs = sb("Vs", [128, NT, 2, 65], BF16)
        Vw = sb("Vw", [128, NT, 2, 65], BF16)
        KcT = sb("KcT", [65, 2, CT * 128], BF16)
        Vc = sb("Vc", [128, CT, 2, 65], BF16)
        c2s = sb("c2s", [128, CT, 128], BF16)
        onehot = sb("onehot", [128, NTOK], BF16)
        B01 = sb("B01", [128, 2, 8, 128])
        W4 = sb("W4", [128, 128])
        causal = sb("causal", [128, 128])
        ch = sb("ch", [128, 8])
        gq = sb("gq", [128, 64])
        gks = sb("gks", [128, 64])
        gkw = sb("gkw", [128, 64])
        f0 = sb("f0", [128, 128])
        vmB = sb("vmB", [128, 256])
        amB = sb("amB", [128, 256])
        fzB = sb("fzB", [128, 256])
        kvt = [sb("kvt%d" % i, [128, 512]) for i in range(2)]
        sqk = [sb("sqk%d" % i, [128, 256]) for i in range(2)]
        tmpk = [sb("tmpk%d" % i, [128, 256]) for i in range(2)]
        ssk = [sb("ssk%d" % i, [128, 8]) for i in range(2)]
        kb = [sb("kb%d" % i, [128, 256], BF16) for i in range(2)]
        qt = sb("qt", [128, 512])
        sqq = sb("sqq", [128, 512])
        tmpq = sb("tmpq", [128, 512])
        ssq = sb("ssq", [128, 16])
        qb_ = sb("qb", [128, 512], BF16)
        QT = [sb("QT%d" % i, [65, 8, 128], BF16) for i in range(2)]
        gsig = [sb("gsig%d" % i, [128, 24]) for i in range(2)]
        bct = sb("bct", [128, 8, 128])
        bctH = sb("bctH", [128, 8, 128], BF16)
        bctL = sb("bctL", [128, 8, 128], BF16)
        B01H = sb("B01H", [128, 2, 8, 128], BF16)
        B01L = sb("B01L", [128, 2, 8, 128], BF16)
        W4H = sb("W4H", [128, 4, 128], BF16)
        mct = sb("mct", [128, 128])
        sfp = [sb("sfp%d" % i, [128, 512]) for i in range(2)]
        PT = [sb("PT%d" % i, [128, 512], BF16) for i in range(4)]
        PTc = [sb("PTc%d" % i, [128, 512], BF16) for i in range(2)]
        rcM = sb("rcM", [128, 16])
        rcC = sb("rcC", [128, 16])
        imp = sb("imp", [128, 128])
        sc2 = sb("sc2", [128, 128])
        m8 = sb("m8", [128, 16])
        mneg = sb("mneg", [128, 128], BF16)
        mnegT = [sb("mnegT%d" % i, [128, 4, 128], BF16) for i in range(2)]
        yat = [sb("yat%d" % i, [128, 512]) for i in range(2)]
        yab = [sb("yab%d" % i, [128, 512], BF16) for i in range(2)]
        oTsM = sb("oTsM", [65, 512])
        oTsC = sb("oTsC", [65, 512])
        iTs = sb("iTs", [128, 512])
        psc = [ps("psc%d" % i, [128, 512]) for i in range(3)]
        povT = ps("povT", [128, 512])
        pov = ps("pov", [128, 512])
        povT2 = ps("povT2", [128, 512])
        pimT = ps("pimT", [128, 512])
        ptr = ps("ptr", [128, 1024], BF16)
        pch = ptr.bitcast(F32)

        P.dma("pool", onehot[:], onehot_in[:, :], writes=["onehot"])
        P.dma("pool", c2s[:], c2s_in.rearrange("(c p) b -> p c b", p=128), writes=["c2s"])
        P.dma("sp", W4[:], w4_in[:, :], writes=["W4"])
        P.dma("sp", causal[:], causal_in[:, :], writes=["causal"])
        P.dma("sp", ch[:], _brow(rel31, 8), writes=["ch"])
        P.dma("sp", gq[:], _brow(q_gain, 64), writes=["gq"])
        P.dma("sp", gks[:], _brow(k_gain[1], 64), writes=["gks"])
        P.dma("sp", gkw[:], _brow(k_gain[2], 64), writes=["gkw"])
        P.dma("sp", f0[:], _brow(f0row, 128), writes=["f0"])
        P.dma("sp", vmB[:], vmB_in[:, :], writes=["vmB"])
        P.dma("sp", amB[:], amB_in[:, :], writes=["amB"])
        P.dma("sp", fzB[:], fzB_in[:, :], writes=["fzB"])
        for d in range(2):
            P.dma("sp", B01[:, d], Bg[d], writes=[("B01", d)])
            P.op("dve", lambda e, d=d: e.tensor_tensor(out=B01[:, d], in0=B01[:, d], in1=_bc(ch[:].rearrange("p (h o) -> p h o", o=1), [128, 8, 128]), op=ALU.subtract), reads=[("B01", d), "ch"], writes=[("B01", d)])
        P.op("dve", lambda e: e.tensor_tensor(out=B01[:, 0], in0=B01[:, 0], in1=_bc(causal[:].rearrange("p (o q) -> p o q", o=1), [128, 8, 128]), op=ALU.add), reads=[("B01", 0), "causal"], writes=[("B01", 0)])
        for d in range(2):
            P.op("dve", lambda e, d=d: e.tensor_copy(out=B01H[:, d], in_=B01[:, d]), reads=[("B01", d)], writes=[("B01H", d)])
            P.op("dve", lambda e, d=d: e.tensor_tensor(out=B01[:, d], in0=B01[:, d], in1=B01H[:, d], op=ALU.subtract), reads=[("B01", d), ("B01H", d)], writes=[("B01", d)])
            P.op("dve", lambda e, d=d: e.tensor_copy(out=B01L[:, d], in_=B01[:, d]), reads=[("B01", d)], writes=[("B01L", d)])
        P.op("dve", lambda e: e.tensor_copy(out=W4H[:], in_=_bc(W4[:].rearrange("p (o q) -> p o q", o=1), [128, 4, 128])), reads=["W4"], writes=["W4H"])
        for g in range(2):
            P.dma("pool", KsT[64:65, g, :], kvalid_row[0:1, :], writes=[("KsTv", g)])
            P.dma("pool", KwT[64:65, g, :], kvalid_row[0:1, :], writes=[("KwTv", g)])
            P.dma("sp", KcT[:, g, :], kc_scr[g], writes=[("KcT", g)])
            P.dma("sp", Vc[:, :, g, :], vc_scr[g].rearrange("(c p) e -> p c e", p=128), writes=[("Vc", g)])
        P.op("pool", lambda e: e.memset(Vs[:], 1.0), writes=["Vs_init"])
        P.op("pool", lambda e: e.memset(Vw[:], 1.0), writes=["Vw_init"])
        for i in range(2):
            P.op("pool", lambda e, i=i: e.memset(QT[i][:], 1.0), writes=[("QT", i)])

        v3 = lambda ap, b=64: ap.rearrange("p (a b) -> p a b", b=b)

        def kvprep_levels(t):
            q = t % 2
            L = [[] for _ in range(7)]
            L[0].append(lambda: P.dma("sp", kvt[q][:], proj[t * 128:(t + 1) * 128, 768:1280], writes=[("kvt", q)]))
            for ci, c0 in enumerate((0, 256)):
                L[1].append(lambda ci=ci, c0=c0: P.op("dve", lambda e: e.tensor_tensor(out=v3(sqk[q][:, ci * 128:(ci + 1) * 128]), in0=v3(kvt[q][:, c0:c0 + 128]), in1=v3(kvt[q][:, c0:c0 + 128]), op=ALU.mult), reads=[("kvt", q)], writes=[("sqk", q, ci)]))
            L[1].append(lambda: P.op("dve", lambda e: e.tensor_reduce(out=ssk[q][:, 0:4], in_=v3(sqk[q][:]), axis=AX.X, op=ALU.add), reads=[("sqk", q, 0), ("sqk", q, 1)], writes=[("ssk0", q)]))
            L[1].append(lambda: P.op("pool", lambda e: e.tensor_copy(out=Vs[:, t, :, 0:64], in_=v3(kvt[q][:, 128:256])), reads=[("kvt", q), "Vs_init"], writes=[("Vs", t)]))
            L[1].append(lambda: P.op("pool", lambda e: e.tensor_copy(out=Vw[:, t, :, 0:64], in_=v3(kvt[q][:, 384:512])), reads=[("kvt", q), "Vw_init"], writes=[("Vw", t)]))
            L[2].append(lambda: P.op("act", lambda e: e.activation(out=ssk[q][:, 0:4], in_=ssk[q][:, 0:4], func=AF.Ln, scale=1.0 / 64, bias=1e-6), reads=[("ssk0", q)], writes=[("ssk0", q)]))
            L[2].append(lambda: P.op("act", lambda e: e.activation(out=ssk[q][:, 4:8], in_=ssk[q][:, 0:4], func=AF.Exp, scale=-0.5), reads=[("ssk0", q)], writes=[("ssk4", q)]))
            for ci, (c0, gt, gk_) in enumerate(((0, gks, "gks"), (256, gkw, "gkw"))):
                L[3].append(lambda ci=ci, c0=c0: P.op("dve", lambda e: e.tensor_tensor(out=v3(tmpk[q][:, ci * 128:(ci + 1) * 128]), in0=v3(kvt[q][:, c0:c0 + 128]), in1=_bc(ssk[q][:, 4 + 2 * ci:6 + 2 * ci].rearrange("p (a o) -> p a o", o=1), [128, 2, 64]), op=ALU.mult), reads=[("kvt", q), ("ssk4", q)], writes=[("tmpk", q, ci)]))
                L[3].append(lambda ci=ci, gt=gt, gk_=gk_: P.op("dve", lambda e: e.tensor_tensor(out=v3(kb[q][:, ci * 128:(ci + 1) * 128]), in0=v3(tmpk[q][:, ci * 128:(ci + 1) * 128]), in1=_bc(gt[:].rearrange("p (o b) -> p o b", o=1), [128, 2, 64]), op=ALU.mult), reads=[("tmpk", q, ci), gk_], writes=[("kb", q, ci)]))
            for a4 in range(4):
                L[4].append(lambda a4=a4: P.op("pe", lambda e: e.transpose(out=ptr[0:64, q * 512 + a4 * 128:q * 512 + (a4 + 1) * 128], in_=kb[q][:, a4 * 64:(a4 + 1) * 64], identity=ident[:]), reads=[("kb", q, 0), ("kb", q, 1), "ident"], writes=["ptr"]))
            L[5].append(lambda: P.op("act", lambda e: e.activation(out=KsT[0:64, :, t * 128:(t + 1) * 128], in_=ptr[0:64, q * 512:q * 512 + 256].rearrange("p (g n) -> p g n", n=128), func=AF.Copy), reads=["ptr"], writes=[("KsT", t)]))
            L[5].append(lambda: P.op("act", lambda e: e.activation(out=KwT[0:64, :, t * 128:(t + 1) * 128], in_=ptr[0:64, q * 512 + 256:q * 512 + 512].rearrange("p (g n) -> p g n", n=128), func=AF.Copy), reads=["ptr"], writes=[("KwT", t)]))
            return L

        def qprep_levels(Q, qb):
            L = [[] for _ in range(7)]
            v = ((Q % 16) - 1) // 2
            L[0].append(lambda: P.dma("sp", qt[:], proj[Q * 128:(Q + 1) * 128, 0:512], writes=["qt"]))
            L[0].append(lambda: P.dma("sp", gsig[qb][:], proj[Q * 128:(Q + 1) * 128, 1280:1304], writes=[("gsig", qb)]))
            first_use = (Q // 2) < 8
            if first_use:
                L[0].append(lambda: P.dma("sp", bct[:], Bcg[v], writes=["bct"]))
                L[0].append(lambda: P.dma("sp", mct[:], Mc[v], writes=["mct"]))
            else:
                L[0].append(lambda: P.dma("sp", bctH[:].rearrange("p h q -> p (h q)"), bc_scr[v, 0], reads=[("bc_scr", v, 0)], writes=["bctH"]))
                L[0].append(lambda: P.dma("sp", bctL[:].rearrange("p h q -> p (h q)"), bc_scr[v, 1], reads=[("bc_scr", v, 1)], writes=["bctL"]))
            L[1].append(lambda: P.op("dve", lambda e: e.tensor_tensor(out=v3(sqq[:]), in0=v3(qt[:]), in1=v3(qt[:]), op=ALU.mult), reads=["qt"], writes=["sqq"]))
            L[1].append(lambda: P.op("dve", lambda e: e.tensor_reduce(out=ssq[:, 0:8], in_=v3(sqq[:]), axis=AX.X, op=ALU.add), reads=["sqq"], writes=["ssq0"]))
            if first_use:
                L[1].append(lambda: P.op("dve", lambda e: e.tensor_tensor(out=bct[:], in0=bct[:], in1=_bc(ch[:].rearrange("p (h o) -> p h o", o=1), [128, 8, 128]), op=ALU.subtract), reads=["bct", "ch"], writes=["bct"]))
            if first_use:
                L[1].append(lambda: P.op("dve", lambda e: e.tensor_tensor(out=bct[:], in0=bct[:], in1=_bc(mct[:].rearrange("p (o q) -> p o q", o=1), [128, 8, 128]), op=ALU.add), reads=["bct", "mct"], writes=["bct"]))
            if first_use:
                L[2].append(lambda: P.op("dve", lambda e: e.tensor_copy(out=bctH[:], in_=bct[:]), reads=["bct"], writes=["bctH"]))
            if first_use:
                L[2].append(lambda: P.op("dve", lambda e: e.tensor_tensor(out=bct[:], in0=bct[:], in1=bctH[:], op=ALU.subtract), reads=["bct", "bctH"], writes=["bct"]))
            if first_use:
                L[2].append(lambda: P.op("dve", lambda e: e.tensor_copy(out=bctL[:], in_=bct[:]), reads=["bct"], writes=["bctL"]))
            L[2].append(lambda: P.op("act", lambda e: e.activation(out=ssq[:, 0:8], in_=ssq[:, 0:8], func=AF.Ln, scale=1.0 / 64, bias=1e-6), reads=["ssq0"], writes=["ssq0"]))
            L[2].append(lambda: P.op("act", lambda e: e.activation(out=ssq[:, 8:16], in_=ssq[:, 0:8], func=AF.Exp, scale=-0.5), reads=["ssq0"], writes=["ssq8"]))
            L[2].append(lambda: P.op("act", lambda e: e.activation(out=gsig[qb][:], in_=gsig[qb][:], func=AF.Exp, scale=-1.0), reads=[("gsig", qb)], writes=[("gsig", qb)]))
            L[2].append(lambda: P.op("act", lambda e: e.activation(out=gsig[qb][:], in_=gsig[qb][:], func=AF.Ln, bias=1.0), reads=[("gsig", qb)], writes=[("gsig", qb)]))
            L[2].append(lambda: P.op("act", lambda e: e.activation(out=gsig[qb][:], in_=gsig[qb][:], func=AF.Exp, scale=-1.0), reads=[("gsig", qb)], writes=[("gsig", qb)]))
            if first_use:
                L[3].append(lambda: P.dma("sp", bc_scr[v, 0], bctH[:].rearrange("p h q -> p (h q)"), reads=["bctH"], writes=[("bc_scr", v, 0)]))
                L[3].append(lambda: P.dma("sp", bc_scr[v, 1], bctL[:].rearrange("p h q -> p (h q)"), reads=["bctL"], writes=[("bc_scr", v, 1)]))
            L[3].append(lambda: P.op("dve", lambda e: e.tensor_tensor(out=v3(tmpq[:]), in0=v3(qt[:]), in1=_bc(ssq[:, 8:16].rearrange("p (a o) -> p a o", o=1), [128, 8, 64]), op=ALU.mult), reads=["qt", "ssq8"], writes=["tmpq"]))
            L[3].append(lambda: P.op("dve", lambda e: e.scalar_tensor_tensor(out=v3(qb_[:]), in0=v3(tmpq[:]), scalar=0.125, in1=_bc(gq[:].rearrange("p (o b) -> p o b", o=1), [128, 8, 64]), op0=ALU.mult, op1=ALU.mult), reads=["tmpq", "gq"], writes=["qb"]))
            for h in range(8):
                L[4].append(lambda h=h: P.op("pe", lambda e: e.transpose(out=ptr[0:64, h * 128:(h + 1) * 128], in_=qb_[:, h * 64:(h + 1) * 64], identity=ident[:]), reads=["qb", "ident"], writes=["ptr"]))
            L[5].append(lambda: P.op("act", lambda e: e.activation(out=QT[qb][0:64, :, :], in_=ptr[0:64, :].rearrange("p (h n) -> p h n", n=128), func=AF.Copy), reads=["ptr"], writes=[("QT", qb)]))
            return L

        pti = [0]
        sci = [0]

        def qkm_op(QTg, qtkey, kt, kT_of, use_mask, mT, pq=None, bias=None):
            if pq is None:
                pq = sci[0]
            pscb = psc[pq]
            ksrc, kkeys = kT_of(kt)
            extra = []
            if use_mask:
                extra.append((onehot[:, kt * 128:(kt + 1) * 128], mT[0][:].rearrange("p h q -> p (h q)"), ["onehot", mT[1]]))
            if bias is not None:
                for bap in bias[0]:
                    extra.append((ident[:], bap.rearrange("p h q -> p (h q)"), ["ident"] + bias[1]))
            P.op("pe", lambda e: e.matmul(pscb[:, :], lhsT=ksrc, rhs=QTg, start=True, stop=(len(extra) == 0)), reads=kkeys + [qtkey], writes=[("psc", pq)])
            for xi, (l_, r_, k_) in enumerate(extra):
                P.op("pe", lambda e, l_=l_, r_=r_, xi=xi: e.matmul(pscb[:, :], lhsT=l_, rhs=r_, start=False, stop=(xi == len(extra) - 1)), reads=k_, writes=[("psc", pq)])
            return pq

        def exp_op(pq, has_bias=False):
            pi_ = pti[0] % 4
            pti[0] += 1
            P.op("act", lambda e: e.activation(out=PT[pi_][:], in_=psc[pq][:, :], func=AF.Exp), reads=[("psc", pq)], writes=[("PT", pi_)])
            return pi_

        def pv_op(pi_, vsrc, vkeys, acc, acckey, first, last):
            P.op("pe", lambda e: e.matmul(acc[0:65, :], lhsT=vsrc, rhs=PT[pi_][:], start=first, stop=last), reads=[("PT", pi_)] + vkeys, writes=[acckey])

        def norm_ops(po, pok, rc, rckey, gs, gskey, g, br, first_branch, yt, ytk):
            pov3 = po[:, 0:260].rearrange("p (h e) -> p h e", e=65)
            P.op("dve", lambda e: e.tensor_scalar(out=rc[:, 0:4].rearrange("p (h o) -> p h o", o=1), in0=pov3[:, :, 64:65], scalar1=1e-30, scalar2=None, op0=ALU.max), reads=[pok], writes=[rckey + "0"])
            P.op("dve", lambda e: e.reciprocal(out=rc[:, 4:8], in_=rc[:, 0:4]), reads=[rckey + "0"], writes=[rckey + "4"])
            gv = gs[:, g * 12:(g + 1) * 12].rearrange("p (h b) -> p h b", b=3)[:, :, br:br + 1]
            P.op("dve", lambda e: e.tensor_tensor(out=rc[:, 8:12].rearrange("p (h o) -> p h o", o=1), in0=rc[:, 4:8].rearrange("p (h o) -> p h o", o=1), in1=gv, op=ALU.mult), reads=[rckey + "4", gskey], writes=[rckey + "8"])
            for h in range(4):
                dst = yt[:, (g * 4 + h) * 64:(g * 4 + h + 1) * 64]
                if first_branch:
                    P.op("dve", lambda e, h=h, dst=dst: e.tensor_scalar(out=dst, in0=pov3[:, h, 0:64], scalar1=rc[:, 8 + h:9 + h], scalar2=None, op0=ALU.mult), reads=[pok, rckey + "8"], writes=[(ytk, g, h)])
                else:
                    P.op("dve", lambda e, h=h, dst=dst: e.scalar_tensor_tensor(out=dst, in0=pov3[:, h, 0:64], scalar=rc[:, 8 + h:9 + h], in1=dst, op0=ALU.mult, op1=ALU.add), reads=[pok, rckey + "8", (ytk, g, h)], writes=[(ytk, g, h)])

        def chain_levels(Q, g):
            j = Q // 2
            qb = j % 2
            QTg = QT[qb][:, 4 * g:4 * g + 4, :].rearrange("p h q -> p (h q)")
            ctb = (8 * Q + 6) // 128
            n = ctb + 1
            L = []
            st = {}
            for idx in range(n):
                kt = idx
                hb = (kt == ctb)
                def lev_a(idx=idx, kt=kt, hb=hb):
                    qkm_op(QTg, ("QT", qb), kt, lambda kt_: (KcT[:, g, kt_ * 128:(kt_ + 1) * 128], [("KcT", g)]), False, None, pq=None,
                           bias=(([bctH[:, 4 * g:4 * g + 4, :], bctL[:, 4 * g:4 * g + 4, :]], ["bctH", "bctL"]) if hb else None))
                    pqc = sci[0]
                    P.op("act", lambda e: e.activation(out=PTc[idx % 2][:], in_=psc[pqc][:, :], func=AF.Exp), reads=[("psc", pqc)], writes=[("PTc", idx % 2)])

                def lev_b(idx=idx, kt=kt):
                    P.op("pe", lambda e: e.matmul(povT2[0:65, :], lhsT=Vc[:, kt, g, :], rhs=PTc[idx % 2][:], start=(idx == 0), stop=(idx == n - 1)), reads=[("PTc", idx % 2), ("Vc", g)], writes=["povT2"])
                    P.op("pe", lambda e: e.matmul(pimT[:, :], lhsT=c2s[:, kt, :], rhs=PTc[idx % 2][:], start=(idx == 0), stop=(idx == n - 1)), reads=[("PTc", idx % 2), "c2s"], writes=["pimT"])
                L.append([lev_a])
                L.append([lev_b])
            L.append([lambda: P.op("dve", lambda e: e.tensor_copy(out=oTsC[0:65, :], in_=povT2[0:65, :]), reads=["povT2"], writes=["oTsC"]),
                      lambda: P.op("dve", lambda e: e.tensor_copy(out=iTs[:, :], in_=pimT[:, :]), reads=["pimT"], writes=["iTs"])])
            L.append([lambda h=h: P.op("pe", lambda e: e.transpose(out=pch[:, h * 65:(h + 1) * 65], in_=oTsC[0:65, h * 128:(h + 1) * 128], identity=identf[0:65, 0:65]), reads=["oTsC", "identf"], writes=["ptr"]) for h in range(4)])
            L.append([lambda: norm_ops(pch, "ptr", rcC, "rcC", gsig[qb], ("gsig", qb), g, 0, True, yat[qb], "yat%d" % qb)])
            L += [[] for _ in range(4)]
            L.append([lambda h=h: P.op("pe", lambda e: e.transpose(out=pch[:, h * 128:(h + 1) * 128], in_=iTs[:, h * 128:(h + 1) * 128], identity=identf[:, :]), reads=["iTs", "identf"], writes=["ptr"]) for h in range(4)])
            o0 = 128 - 2 * Q

            def topk():
                for h in range(4):
                    if h == 0:
                        P.op("dve", lambda e: e.tensor_scalar(out=imp[:], in0=pch[:, 0:128], scalar1=rcC[:, 4:5], scalar2=None, op0=ALU.mult), reads=["ptr", "rcC4"], writes=["imp"])
                    else:
                        P.op("dve", lambda e, h=h: e.scalar_tensor_tensor(out=imp[:], in0=pch[:, h * 128:(h + 1) * 128], scalar=rcC[:, 4 + h:5 + h], in1=imp[:], op0=ALU.mult, op1=ALU.add), reads=["ptr", "rcC4", "imp"], writes=["imp"])
                P.op("dve", lambda e: e.tensor_tensor(out=imp[:], in0=imp[:], in1=vmB[:, o0:o0 + 128], op=ALU.mult), reads=["imp", "vmB"], writes=["imp"])
                P.op("dve", lambda e: e.tensor_tensor(out=imp[:], in0=imp[:], in1=amB[:, o0:o0 + 128], op=ALU.add), reads=["imp", "amB"], writes=["imp"])
                P.op("dve", lambda e: e.tensor_tensor(out=imp[:], in0=imp[:], in1=fzB[:, o0:o0 + 128], op=ALU.max), reads=["imp", "fzB"], writes=["imp"])
                P.op("dve", lambda e: e.tensor_tensor(out=imp[:], in0=imp[:], in1=f0[:], op=ALU.max), reads=["imp", "f0"], writes=["imp"])
                P.op("dve", lambda e: e.max(out=m8[:, 0:8], in_=imp[:]), reads=["imp"], writes=["m8a"])
                P.op("dve", lambda e: e.match_replace(out=sc2[:], in_to_replace=m8[:, 0:8], in_values=imp[:], imm_value=-3.0), reads=["imp", "m8a"], writes=["sc2"])
                P.op("dve", lambda e: e.max(out=m8[:, 8:16], in_=sc2[:]), reads=["sc2"], writes=["m8b"])
                P.op("dve", lambda e: e.tensor_scalar(out=mneg[:], in0=imp[:], scalar1=m8[:, 15:16], scalar2=NEG, op0=ALU.is_lt, op1=ALU.mult), reads=["imp", "m8b"], writes=["mneg"])
            L.append([topk])
            L += [[] for _ in range(8)]
            mi = (2 * j + g) % 2
            L.append([lambda: P.op("pe", lambda e: e.transpose(out=ptr[:, 0:128], in_=mneg[:], identity=ident[:]), reads=["mneg", "ident"], writes=["ptr"])])
            L.append([lambda: P.op("act", lambda e: e.activation(out=mnegT[mi][:], in_=_bc(ptr[:, 0:128].rearrange("p (o q) -> p o q", o=1), [128, 4, 128]), func=AF.Copy), reads=["ptr"], writes=[("mnegT", mi)])])
            return L

        def zip_levels(*lists):
            n = max(len(l) for l in lists)
            out_ = []
            for i in range(n):
                lv = []
                for l in lists:
                    if i < len(l):
                        lv += l[i]
                out_.append(lv)
            return out_

        def spaced(L):
            gaps = {0: 1, 1: 2, 2: 1, 3: 2, 4: 1}
            out_ = []
            for i, lv in enumerate(L):
                out_.append(lv)
                out_ += [[] for _ in range(gaps.get(i, 0))]
            return out_

        def run_levels(levels):
            for lv in levels:
                for f in lv:
                    f()

        def main_unit(Q, g, filler, carry):
            j = Q // 2
            qb = j % 2
            mi = (2 * j + g) % 2
            QTg = QT[qb][:, 4 * g:4 * g + 4, :].rearrange("p h q -> p (h q)")
            sel_kts = list(range(Q + 1))
            win_kts = list(range(max(0, Q - 4), Q + 1))
            total_it = len(sel_kts) + len(win_kts)
            done_it = [0]
            fpos = [0]

            def pull():
                done_it[0] += 1
                rem_it = max(1, total_it - done_it[0] + 1 - 8)
                rem_f = len(filler) - fpos[0]
                k = -(-rem_f // rem_it) if rem_it > 0 else rem_f
                for _ in range(k):
                    if fpos[0] < len(filler):
                        for f in filler[fpos[0]]:
                            f()
                        fpos[0] += 1

            class Branch:
                def __init__(self, kts, kT_of, v_of, bias_of, use_mask, br):
                    self.kts, self.kT_of, self.v_of, self.bias_of, self.use_mask, self.br = kts, kT_of, v_of, bias_of, use_mask, br
                    self.slots = {}
                    self.started = False

                def qk(self, idx):
                    self.slots[idx] = qkm_op(QTg, ("QT", qb), self.kts[idx], self.kT_of, self.use_mask, (mnegT[mi], ("mnegT", mi)), pq=idx % 3, bias=self.bias_of(self.kts[idx]))

                def start(self):
                    if not self.started:
                        self.qk(0)
                        if len(self.kts) > 1:
                            self.qk(1)
                        self.started = True

                def loop(self, after=None):
                    n = len(self.kts)
                    self.start()
                    for idx in range(n):
                        if idx + 2 < n:
                            self.qk(idx + 2)
                        sci[0] = idx % 3
                        pi_ = exp_op(self.slots[idx])
                        vsrc, vkeys = self.v_of(self.kts[idx])
                        pv_op(pi_, vsrc, vkeys, povT, "povT", idx == 0, idx == n - 1)
                        if after is not None and (idx == after[0] or (idx == n - 1 and after[0] >= n)):
                            for f in after[1]:
                                f()
                        pull()

                def epi_copy(self):
                    P.op("dve", lambda e: e.tensor_copy(out=oTsM[0:65, :], in_=povT[0:65, :]), reads=["povT"], writes=["oTsM"])

                def epi_rest(self):
                    for h in range(4):
                        P.op("pe", lambda e, h=h: e.transpose(out=pov[:, h * 65:(h + 1) * 65], in_=oTsM[0:65, h * 128:(h + 1) * 128], identity=identf[0:65, 0:65]), reads=["oTsM", "identf"], writes=["pov"])
                    norm_ops(pov, "pov", rcM, "rcM", gsig[qb], ("gsig", qb), g, self.br, False, yat[qb], "yat%d" % qb)

            def sbias(kt):
                dl = Q - kt
                if dl <= 1:
                    return ([B01H[:, dl, 4 * g:4 * g + 4, :], B01L[:, dl, 4 * g:4 * g + 4, :]], [("B01H", dl), ("B01L", dl)])
                return None

            def wbias(kt):
                dl = Q - kt
                if dl <= 1:
                    return sbias(kt)
                if dl == 4:
                    return ([W4H[:]], ["W4H"])
                return None
            selB = Branch(sel_kts,
                          lambda kt: (KsT[:, g, kt * 128:(kt + 1) * 128], [("KsT", kt), ("KsTv", g)]),
                          lambda kt: (Vs[:, kt, g, :], [("Vs", kt)]), sbias, (2 * Q + 2 > 16), 1)
            winB = Branch(win_kts,
                          lambda kt: (KwT[:, g, kt * 128:(kt + 1) * 128], [("KwT", kt), ("KwTv", g)]),
                          lambda kt: (Vw[:, kt, g, :], [("Vw", kt)]), wbias, False, 2)
            selB.loop(after=(1, carry))
            winB.start()
            selB.epi_copy()
            winB.loop(after=(1, [selB.epi_rest]))
            while fpos[0] < len(filler):
                for f in filler[fpos[0]]:
                    f()
                fpos[0] += 1
            winB.epi_copy()

            def finalize():
                if g == 1:
                    P.op("dve", lambda e: e.tensor_copy(out=yab[qb][:], in_=yat[qb][:]), reads=[("yat%d" % qb, gg, h) for gg in range(2) for h in range(4)], writes=[("yab", qb)])
                    P.dma("sp", ya[j * 128:(j + 1) * 128, :], yab[qb][:], reads=[("yab", qb)], writes=[("ya", j)])
            return [winB.epi_rest, finalize]

        units = [(2 * j + 1, g) for j in range(NJ) for g in range(2)]
        run_levels(zip_levels(kvprep_levels(0), kvprep_levels(1)))
        run_levels(qprep_levels(1, 0))
        run_levels(chain_levels(1, 0))
        carry = []
        for ui, (Q, g) in enumerate(units):
            filler = []
            if ui + 1 < len(units):
                nQ, ng = units[ui + 1]
                if nQ != Q:
                    jn = nQ // 2
                    filler += zip_levels(spaced(kvprep_levels(2 * jn)), spaced(kvprep_levels(2 * jn + 1)),
                                         [[] for _ in range(4)] + spaced(qprep_levels(nQ, jn % 2)))
                filler += chain_levels(nQ, ng)
            carry = main_unit(Q, g, filler, carry)
        for f in carry:
            f()
        P.flush()


def _t5_bucket_np(dist):
    n = np.maximum(dist, 0)
    nf = np.maximum(n, 1).astype(np.float32)
    large = 16 + (np.log(nf / np.float32(16)) / np.float32(np.log(8.0)) * np.float32(16)).astype(np.int32)
    return np.where(n < 16, n, np.minimum(large, 31)).astype(np.int64)


def make_consts(NT):
    NTOK = NT * 128
    NCB = 8 * NT - 1
    CT = (NCB + 127) // 128
    c = {}
    c["c_ident"] = np.eye(128, dtype=np.float32)
    c["c_tri"] = np.triu(np.ones((128, 128), np.float32))
    k = np.arange(128)[:, None]
    q = np.arange(128)[None, :]
    c["c_causal"] = np.where(k > q, NEG, 0.0).astype(np.float32)
    c["c_w4"] = np.where(k <= q, NEG, 0.0).astype(np.float32)
    n = np.arange(CT * 128)[:, None]
    blk = np.arange(128)[None, :]
    c2s = ((16 * n < 64 * blk + 64) & (16 * n + 32 > 64 * blk) & (n < NCB)).astype(np.float32)
    c["c_c2s"] = c2s
    key = np.arange(NTOK)[None, :]
    c["c_onehot"] = (key // 64 == np.arange(128)[:, None]).astype(np.float32)
    qq = np.arange(128)[:, None]
    rb = np.arange(256)[None, :] - 128
    cur = (qq >= 64).astype(np.int64)
    vm = (rb <= cur).astype(np.float32)
    c["c_vmB"] = vm
    c["c_amB"] = vm - 1.0
    c["c_fzB"] = np.where(rb == cur, 10003.0, np.where(rb == cur - 1, 10002.0, -1.0)).astype(np.float32)
    idx = {}
    idx["Bg"] = np.stack([_t5_bucket_np(128 * d + q - k) for d in range(2)])
    r = np.arange(128)[:, None]
    dists = np.stack([16 * 8 * (2 * v + 1) + q - 16 * r - 31 for v in range(8)])
    idx["Bcg"] = _t5_bucket_np(dists)
    c["c_Mc"] = np.where(dists < 0, NEG, 0.0).astype(np.float32)
    return c, idx


def core_inputs(inputs, b, par, NT, consts, idx):
    NTOK = NT * 128
    NCB = 8 * NT - 1
    CT = (NCB + 127) // 128
    x = inputs["x"][b]
    m = dict(consts)
    if par == 1:
        m["x_loc"] = np.ascontiguousarray(x[:NTOK])
    else:
        m["x_loc"] = np.concatenate([np.zeros((128, D), np.float32), x[:NTOK - 128]], axis=0)
    kval = np.zeros(NTOK, np.float32)
    cval = np.zeros(CT * 128, np.float32)
    cval[NCB:] = NEG
    f0 = np.full(128, -1.0, np.float32)
    if par == 0:
        kval[:128] = NEG
        cval[:8] = NEG
        f0[2] = 10001.0
    else:
        f0[0] = 10001.0
    m["kvalid_row"] = kval[None, :]
    m["kvalid_tm"] = np.ascontiguousarray(kval.reshape(NT, 128).T)
    m["cvalid"] = cval[None, :]
    m["f0row"] = f0
    for k_ in ("w_in", "norm1_g", "norm2_g", "w_branch_a", "w_branch_b", "w_out", "w_ff1", "w_ff2",
               "ml_conv_w", "ml_conv_b", "ml_i_bias", "ml_f_bias", "nsa_k_gain", "nsa_q_gain"):
        m[k_] = np.ascontiguousarray(inputs[k_][0])
    for nm in ("w1", "b1", "w2", "b2", "pos"):
        m["cmp_" + nm] = np.stack([inputs["cmp_k_" + nm][0], inputs["cmp_v_" + nm][0]])
    tab = inputs["rel_table"]
    m["rel31"] = np.ascontiguousarray(tab[31])
    m["c_Bg"] = np.ascontiguousarray(tab[idx["Bg"]].transpose(0, 1, 3, 2))
    m["c_Bcg"] = np.ascontiguousarray(tab[idx["Bcg"]].transpose(0, 1, 3, 2))
    return m


_CACHE = {}


def kernel(**inputs):
    inputs = {k: np.asarray(v) for k, v in inputs.items()}
    NT = 64
    if "nc" not in _CACHE:
        _CACHE["nc"] = build_program(NT)
        _CACHE["consts"] = make_consts(NT)
    nc = _CACHE["nc"]
    consts, idx = _CACHE["consts"]
    in_maps = []
    for core in range(8):
        b, par = core // 2, core % 2
        in_maps.append(core_inputs(inputs, b, par, NT, consts, idx))
    res = run_bass_kernel_spmd(nc, in_maps, core_ids=list(range(8)))
    B, S = inputs["x"].shape[:2]
    outp = np.zeros((B, S, D), np.float32)
    for core in range(8):
        b, par = core // 2, core % 2
        o = np.asarray(res.results[core]["out"]).reshape(NT // 2, 128, D)
        for j in range(NT // 2):
            gt = 2 * j + par
            outp[b, gt * 128:(gt + 1) * 128] = o[j]
    return outp
```
